# Optimizing a Trainium2 kernel written in Bass

```python
import jax, jax.numpy as jnp
from jax import lax
import numpy as np

D_MODEL = 1024
BATCH = 2
SEQ = 8192
DEPTH = 1
DEC_BATCH = 128
DEC_SEQ = 4
PAST_LEN = 8192
PAGE_SIZE = 128

HEAD_DIM = 64
N_HEADS_A = 6
N_HEADS_B = 6
N_HEADS_M = 4
WIDTH_A = N_HEADS_A * HEAD_DIM
WIDTH_B = N_HEADS_B * HEAD_DIM
WIDTH_M = N_HEADS_M * HEAD_DIM
MIX_WIDTH = WIDTH_A + WIDTH_B + WIDTH_M
IN_WIDTH = 2 * WIDTH_A + 3 * WIDTH_B + WIDTH_M
SPLITS = (WIDTH_A, 2 * WIDTH_A, 2 * WIDTH_A + WIDTH_B, 2 * WIDTH_A + 2 * WIDTH_B, 2 * WIDTH_A + 3 * WIDTH_B)
CHUNK = 128
DILATIONS = ((128, 1), (512, 4), (2048, 16))
MAX_WINDOW = 2048
BLOCK = 128
N_MEM = 256
ROPE_THETA = 500000.0
ROT_DIM = HEAD_DIM // 4
SCALE = HEAD_DIM ** -0.5
N_EXPERTS = 32
TOP_K = 4
D_EXPERT = D_MODEL
SWIGLU_LIMIT = 7.0
SWIGLU_ALPHA = 1.702
EPS = 1e-6

kernel_name = 'hymba_gmlp_longnet_memxattn_moe_step'


def rmsnorm(x, g):
    xf = x.astype(jnp.float32)
    y = xf * lax.rsqrt(jnp.mean(xf * xf, axis=-1, keepdims=True) + EPS)
    return (y * g.astype(jnp.float32)).astype(x.dtype)


def partial_rope(x, pos):
    half = ROT_DIM // 2
    inv_freq = jnp.power(ROPE_THETA, -jnp.arange(half, dtype=jnp.float32) / half)
    ang = pos.astype(jnp.float32)[:, None] * inv_freq[None, :]
    cos = jnp.cos(ang)[:, None, :]
    sin = jnp.sin(ang)[:, None, :]
    xr = x[..., :ROT_DIM].astype(jnp.float32)
    x1, x2 = xr[..., :half], xr[..., half:]
    rot = jnp.concatenate([x1 * cos - x2 * sin, x2 * cos + x1 * sin], axis=-1)
    return jnp.concatenate([rot.astype(x.dtype), x[..., ROT_DIM:]], axis=-1)


def pre_mix(x, pos, norm1_g, w_in, gv_a, gq_b, gk_b, gq_m):
    B, S, _ = x.shape
    z = rmsnorm(x, norm1_g) @ w_in
    u_a, v_a, q_b, k_b, v_b, q_m = jnp.split(z, SPLITS, axis=-1)
    v_a = rmsnorm(v_a, gv_a)
    q_b = partial_rope(rmsnorm(q_b.reshape(B, S, N_HEADS_B, HEAD_DIM), gq_b), pos)
    k_b = partial_rope(rmsnorm(k_b.reshape(B, S, N_HEADS_B, HEAD_DIM), gk_b), pos)
    v_b = v_b.reshape(B, S, N_HEADS_B, HEAD_DIM)
    q_m = rmsnorm(q_m.reshape(B, S, N_HEADS_M, HEAD_DIM), gq_m)
    return u_a, v_a, q_b, k_b, v_b, q_m


def spatial_gate(u, v_n, w_s, b_s):
    B, S, _ = u.shape
    T = min(CHUNK, S)
    vc = v_n.reshape(B, S // T, T, N_HEADS_A, HEAD_DIM)
    w = jnp.where(jnp.tril(jnp.ones((T, T), dtype=bool)), w_s[:, :T, :T], 0)
    mixed = jnp.einsum('gts,bnsgc->bntgc', w, vc) + jnp.transpose(b_s[:, :T])[None, None, :, :, None]
    return u * mixed.reshape(B, S, WIDTH_A)


def dilated_branch_prompt(q, k, v, window, dil):
    B, S, H, Dh = q.shape
    n_sub = window // dil
    L = -(-S // dil)
    nb = -(-L // BLOCK)

    def to_blocks(t):
        t = jnp.pad(t, ((0, 0), (0, L * dil - S), (0, 0), (0, 0)))
        t = t.reshape(B, L, dil, H, Dh).transpose(0, 2, 1, 3, 4)
        t = jnp.pad(t, ((0, 0), (0, 0), (0, nb * BLOCK - L), (0, 0), (0, 0)))
        return t.reshape(B, dil, nb, BLOCK, H, Dh)

    def with_prev(t):
        prev = jnp.pad(t, ((0, 0), (0, 0), (1, 0), (0, 0), (0, 0), (0, 0)))[:, :, :-1]
        return jnp.concatenate([prev, t], axis=3)

    def from_blocks(t):
        rest = t.shape[4:]
        t = t.reshape((B, dil, nb * BLOCK) + rest)[:, :, :L]
        t = jnp.swapaxes(t, 1, 2).reshape((B, L * dil) + rest)
        return t[:, :S]

    qb = to_blocks(q)
    kk = with_prev(to_blocks(k))
    vv = with_prev(to_blocks(v))
    qi = jnp.arange(BLOCK)[:, None]
    kj = jnp.arange(2 * BLOCK)[None, :]
    dist = BLOCK + qi - kj
    band = (dist >= 0) & (dist <= n_sub)
    mask = band[None] & ((jnp.arange(nb)[:, None, None] > 0) | (kj[None] >= BLOCK))
    s = jnp.einsum('brnqhd,brnkhd->brnhqk', qb, kk).astype(jnp.float32) * SCALE
    s = jnp.where(mask[None, None, :, None], s, -jnp.inf)
    m = jnp.max(s, axis=-1, keepdims=True)
    p = jnp.exp(s - m)
    l = jnp.sum(p, axis=-1, keepdims=True)
    o = jnp.einsum('brnhqk,brnkhd->brnqhd', p, vv.astype(jnp.float32)) / jnp.swapaxes(l, 3, 4)
    lse = jnp.swapaxes((m + jnp.log(l))[..., 0], 3, 4)
    return from_blocks(o), from_blocks(lse)


def combine_by_denominator(outs, lses):
    w = jax.nn.softmax(jnp.stack(lses), axis=0)
    return jnp.sum(w[..., None] * jnp.stack(outs), axis=0)


def dilated_attention_prompt(q, k, v):
    outs, lses = [], []
    for window, dil in DILATIONS:
        o, lse = dilated_branch_prompt(q, k, v, window, dil)
        outs.append(o)
        lses.append(lse)
    return combine_by_denominator(outs, lses)


def dilated_attention_sample(q, k_all, v_all, n_past):
    T = q.shape[1]
    outs, lses = [], []
    for window, dil in DILATIONS:
        offs = dil * jnp.arange(window // dil + 1)
        idx = n_past + jnp.arange(T)[:, None] - offs[None, :]
        valid = idx >= 0
        idx = jnp.maximum(idx, 0)
        kg = k_all[:, idx]
        vg = v_all[:, idx]
        s = jnp.einsum('bthd,btkhd->bthk', q, kg).astype(jnp.float32) * SCALE
        s = jnp.where(valid[None, :, None, :], s, -jnp.inf)
        m = jnp.max(s, axis=-1, keepdims=True)
        p = jnp.exp(s - m)
        l = jnp.sum(p, axis=-1, keepdims=True)
        outs.append(jnp.einsum('bthk,btkhd->bthd', p, vg.astype(jnp.float32)) / l)
        lses.append((m + jnp.log(l))[..., 0])
    return combine_by_denominator(outs, lses)


def memory_kv(mem, mem_norm_g, w_mem_kv, gk_m):
    B, M, _ = mem.shape
    kv = rmsnorm(mem, mem_norm_g) @ w_mem_kv
    k = rmsnorm(kv[..., :WIDTH_M].reshape(B, M, N_HEADS_M, HEAD_DIM), gk_m)
    v = kv[..., WIDTH_M:].reshape(B, M, N_HEADS_M, HEAD_DIM)
    return k, v


def memory_attend(q_m, k_m, v_m):
    s = jnp.einsum('bshd,bmhd->bhsm', q_m, k_m).astype(jnp.float32) * SCALE
    p = jax.nn.softmax(s, axis=-1)
    return jnp.einsum('bhsm,bmhd->bshd', p, v_m.astype(jnp.float32))


def moe_ffn(h, w_router, b_router, w_gate_up, b_gate_up, w_down, b_down):
    shp = h.shape
    t = h.reshape(-1, D_MODEL)
    logits = (t @ w_router + b_router).astype(jnp.float32)
    top_val, top_idx = lax.top_k(logits, TOP_K)
    gates = jax.nn.softmax(top_val, axis=-1)
    comb = jnp.einsum('nk,nke->ne', gates, jax.nn.one_hot(top_idx, N_EXPERTS, dtype=jnp.float32))
    y = jnp.zeros(t.shape, jnp.float32)
    for e in range(N_EXPERTS):
        gu = t @ w_gate_up[e] + b_gate_up[e]
        gate = jnp.minimum(gu[:, :D_EXPERT], SWIGLU_LIMIT)
        up = jnp.clip(gu[:, D_EXPERT:], -SWIGLU_LIMIT, SWIGLU_LIMIT)
        act = (up + 1) * (gate * jax.nn.sigmoid(SWIGLU_ALPHA * gate))
        y = y + comb[:, e:e + 1] * (act @ w_down[e] + b_down[e]).astype(jnp.float32)
    return y.reshape(shp).astype(h.dtype)


def post_mix(x, o_a, o_b, o_m, w_out, norm2_g, w_router, b_router, w_gate_up, b_gate_up, w_down, b_down):
    B, S, _ = x.shape
    mixed = jnp.concatenate([o_a.astype(x.dtype),
                             o_b.reshape(B, S, WIDTH_B).astype(x.dtype),
                             o_m.reshape(B, S, WIDTH_M).astype(x.dtype)], axis=-1)
    x = x + mixed @ w_out
    return x + moe_ffn(rmsnorm(x, norm2_g), w_router, b_router, w_gate_up, b_gate_up, w_down, b_down)


def setup_inputs(seed: int = 0) -> dict:
    key = jax.random.key(seed)
    ks = jax.random.split(key, 32)
    f32 = jnp.float32
    w_buf = min(MAX_WINDOW, PAST_LEN)
    L = DEPTH

    def nrm(k, shape, scale):
        return scale * jax.random.normal(k, shape, f32)

    def gain(k, shape):
        return 1.0 + 0.05 * jax.random.normal(k, shape, f32)

    return {
        'x_prompt': nrm(ks[0], (BATCH, SEQ, D_MODEL), 1.0),
        'x_sample': nrm(ks[1], (DEC_BATCH, DEC_SEQ, D_MODEL), 1.0),
        'mem_prompt': nrm(ks[2], (BATCH, N_MEM, D_MODEL), 1.0),
        'cache_win_k': nrm(ks[3], (L, DEC_BATCH, w_buf, N_HEADS_B, HEAD_DIM), 1.0),
        'cache_win_v': nrm(ks[4], (L, DEC_BATCH, w_buf, N_HEADS_B, HEAD_DIM), 1.0),
        'cache_mem_k': nrm(ks[5], (L, DEC_BATCH, N_MEM, N_HEADS_M, HEAD_DIM), 1.0),
        'cache_mem_v': nrm(ks[6], (L, DEC_BATCH, N_MEM, N_HEADS_M, HEAD_DIM), 1.0),
        'norm1_g': gain(ks[7], (L, D_MODEL)),
        'w_in': nrm(ks[8], (L, D_MODEL, IN_WIDTH), D_MODEL ** -0.5),
        'gv_a': gain(ks[9], (L, WIDTH_A)),
        'w_s': nrm(ks[10], (L, N_HEADS_A, CHUNK, CHUNK), CHUNK ** -0.5),
        'b_s': gain(ks[11], (L, N_HEADS_A, CHUNK)),
        'gq_b': gain(ks[12], (L, HEAD_DIM)),
        'gk_b': gain(ks[13], (L, HEAD_DIM)),
        'gq_m': gain(ks[14], (L, HEAD_DIM)),
        'gk_m': gain(ks[15], (L, HEAD_DIM)),
        'mem_norm_g': gain(ks[16], (L, D_MODEL)),
        'w_mem_kv': nrm(ks[17], (L, D_MODEL, 2 * WIDTH_M), D_MODEL ** -0.5),
        'w_out': nrm(ks[18], (L, MIX_WIDTH, D_MODEL), MIX_WIDTH ** -0.5),
        'norm2_g': gain(ks[19], (L, D_MODEL)),
        'w_router': nrm(ks[20], (L, D_MODEL, N_EXPERTS), D_MODEL ** -0.5),
        'b_router': nrm(ks[21], (L, N_EXPERTS), 0.01),
        'w_gate_up': nrm(ks[22], (L, N_EXPERTS, D_MODEL, 2 * D_EXPERT), D_MODEL ** -0.5),
        'b_gate_up': nrm(ks[23], (L, N_EXPERTS, 2 * D_EXPERT), 0.01),
        'w_down': nrm(ks[24], (L, N_EXPERTS, D_EXPERT, D_MODEL), D_EXPERT ** -0.5),
        'b_down': nrm(ks[25], (L, N_EXPERTS, D_MODEL), 0.01),
    }


def reference(x_prompt, x_sample, mem_prompt, cache_win_k, cache_win_v, cache_mem_k, cache_mem_v,
              norm1_g, w_in, gv_a, w_s, b_s, gq_b, gk_b, gq_m, gk_m, mem_norm_g, w_mem_kv, w_out,
              norm2_g, w_router, b_router, w_gate_up, b_gate_up, w_down, b_down):
    S = x_prompt.shape[1]
    T = x_sample.shape[1]
    pos_p = jnp.arange(S)
    pos_s = PAST_LEN + jnp.arange(T)
    n_keep = min(MAX_WINDOW, S)
    n_past = cache_win_k.shape[2]
    xp, xs = x_prompt, x_sample
    win_k_p, win_v_p, mem_k_p, mem_v_p, win_k_s, win_v_s, gate_v_s = [], [], [], [], [], [], []
    for l in range(DEPTH):
        ffn = (w_router[l], b_router[l], w_gate_up[l], b_gate_up[l], w_down[l], b_down[l])
        u_a, v_a, q_b, k_b, v_b, q_m = pre_mix(xp, pos_p, norm1_g[l], w_in[l], gv_a[l], gq_b[l], gk_b[l], gq_m[l])
        o_a = spatial_gate(u_a, v_a, w_s[l], b_s[l])
        o_b = dilated_attention_prompt(q_b, k_b, v_b)
        k_m, v_m = memory_kv(mem_prompt, mem_norm_g[l], w_mem_kv[l], gk_m[l])
        o_m = memory_attend(q_m, k_m, v_m)
        xp = post_mix(xp, o_a, o_b, o_m, w_out[l], norm2_g[l], *ffn)
        win_k_p.append(k_b[:, S - n_keep:])
        win_v_p.append(v_b[:, S - n_keep:])
        mem_k_p.append(k_m)
        mem_v_p.append(v_m)
        u_a, v_a, q_b, k_b, v_b, q_m = pre_mix(xs, pos_s, norm1_g[l], w_in[l], gv_a[l], gq_b[l], gk_b[l], gq_m[l])
        o_a = spatial_gate(u_a, v_a, w_s[l], b_s[l])
        k_all = jnp.concatenate([cache_win_k[l].astype(k_b.dtype), k_b], axis=1)
        v_all = jnp.concatenate([cache_win_v[l].astype(v_b.dtype), v_b], axis=1)
        o_b = dilated_attention_sample(q_b, k_all, v_all, n_past)
        o_m = memory_attend(q_m, cache_mem_k[l], cache_mem_v[l])
        xs = post_mix(xs, o_a, o_b, o_m, w_out[l], norm2_g[l], *ffn)
        win_k_s.append(k_b)
        win_v_s.append(v_b)
        gate_v_s.append(v_a)
    y_prompt = xp
    y_sample = xs
    new_win_k_prompt = jnp.stack(win_k_p)
    new_win_v_prompt = jnp.stack(win_v_p)
    new_mem_k_prompt = jnp.stack(mem_k_p)
    new_mem_v_prompt = jnp.stack(mem_v_p)
    new_win_k_sample = jnp.stack(win_k_s)
    new_win_v_sample = jnp.stack(win_v_s)
    new_gate_v_sample = jnp.stack(gate_v_s)
    return (y_prompt, y_sample, new_win_k_prompt, new_win_v_prompt, new_mem_k_prompt, new_mem_v_prompt,
            new_win_k_sample, new_win_v_sample, new_gate_v_sample)
```

```python
import contextlib
import numpy as np
import concourse.bass as bass
import concourse.mybir as mybir
from concourse.bass_utils import run_bass_kernel_spmd

F32 = mybir.dt.float32
BF16 = mybir.dt.bfloat16
I32 = mybir.dt.int32
AF = mybir.ActivationFunctionType
ALU = mybir.AluOpType
AX = mybir.AxisListType

NCORES = 8
D = 1024
SEG = 2048
NS = 64
NTOK = SEG + NS
NEXP = 32
EPS = 1e-6
SCALE = 0.125
KSTOP = 9


class Res:
    __slots__ = ("name", "w", "r", "excl")

    def __init__(self, name="", excl=False):
        self.name = name
        self.w = None
        self.r = []
        self.excl = excl


class Prog:
    ENGS = ("pe", "act", "dve", "pool", "sp")

    def __init__(self, nc):
        self.nc = nc
        self.ops = {e: [] for e in self.ENGS}
        self.cnt = {e: 0 for e in self.ENGS}
        self.sems = {}
        self.semval = {}
        self.known = {e: {} for e in self.ENGS}
        self.sealed = {}
        self.out_tokens = []

    def add_sem(self, key, handle):
        self.sems[key] = handle
        self.semval[key] = 0

    def _need(self, eng, tok, same_ok):
        if tok is None:
            return
        key, val = tok
        if key in self.cnt:
            if key == eng and same_ok:
                return
        else:
            if self.known[eng].get(key, 0) >= val:
                return
            val = max(val, self.semval[key])
            self.sealed[key] = True
        if self.known[eng].get(key, 0) >= val:
            return
        self.known[eng][key] = val
        sem = self.sems[key]
        self.ops[eng].append(lambda h, sem=sem, val=val: h.wait_ge(sem, val))

    def _deps(self, eng, reads, writes, is_dma=False, semkey=None):
        same_ok = (not is_dma) and eng != "pool"
        need = {}

        def add(tok, ok):
            if tok is None:
                return
            key, val = tok
            if ok and key == eng:
                return
            if need.get(key, 0) < val:
                need[key] = val
        for r in reads:
            add(r.w, False)
        for w in writes:
            if not (is_dma and w.w is not None and w.w[0] == semkey):
                add(w.w, same_ok)
            for t in w.r:
                add(t, same_ok)
        for key, val in need.items():
            self._need(eng, (key, val), same_ok=False)

    def _commit(self, tok, reads, writes):
        for r in reads:
            r.r.append(tok)
        for w in writes:
            w.w = tok
            w.r = []

    @staticmethod
    def _split(reads, writes):
        writes = list(writes) + [r for r in reads if r.excl and r not in writes]
        reads = [r for r in reads if not r.excl]
        return reads, writes

    def op(self, eng, fn, reads=(), writes=()):
        reads, writes = self._split(reads, writes)
        self._deps(eng, reads, writes)
        self.cnt[eng] += 1
        tok = (eng, self.cnt[eng])
        sem = self.sems[eng]
        self.ops[eng].append(lambda h, fn=fn, sem=sem: fn(h).then_inc(sem, 1))
        self._commit(tok, reads, writes)
        return tok

    def dma(self, eng, semkey, out, in_, reads=(), writes=(), is_output=False, **kw):
        self._deps(eng, reads, writes, is_dma=True, semkey=semkey)
        if self.sealed.get(semkey) and self.semval[semkey] > 0:
            self._need(eng, (semkey, self.semval[semkey]), same_ok=False)
        self.sealed[semkey] = False
        self.semval[semkey] += 16
        tok = (semkey, self.semval[semkey])
        sem = self.sems[semkey]
        self.ops[eng].append(
            lambda h, out=out, in_=in_, sem=sem, kw=kw: h.dma_start(out=out, in_=in_, **kw).then_inc(sem, 16))
        self._commit(tok, reads, writes)
        if is_output:
            self.out_tokens.append(tok)
        return tok

    def dmaf(self, eng, semkey, fn, reads=(), writes=(), is_output=False):
        self._deps(eng, reads, writes, is_dma=True, semkey=semkey)
        if self.sealed.get(semkey) and self.semval[semkey] > 0:
            self._need(eng, (semkey, self.semval[semkey]), same_ok=False)
        self.sealed[semkey] = False
        self.semval[semkey] += 16
        tok = (semkey, self.semval[semkey])
        sem = self.sems[semkey]
        self.ops[eng].append(lambda h, fn=fn, sem=sem: fn(h).then_inc(sem, 16))
        self._commit(tok, reads, writes)
        if is_output:
            self.out_tokens.append(tok)
        return tok

    def barrier(self):
        for e in self.ENGS:
            for f in self.ENGS:
                if (f != e or e == "pool") and self.cnt[f] > 0:
                    self._need(e, (f, self.cnt[f]), same_ok=False)
            for k, v in self.semval.items():
                if v > 0:
                    self._need(e, (k, v), same_ok=False)

    def finish(self, eng="sp"):
        last = {}
        for key, val in self.out_tokens:
            last[key] = max(last.get(key, 0), val)
        for key, val in last.items():
            self._need(eng, (key, val), same_ok=False)

    def emit(self, block):
        hmap = {"pe": "tensor", "act": "scalar", "dve": "vector", "pool": "gpsimd", "sp": "sync"}
        for e in self.ENGS:
            ops = self.ops[e]
            if not ops:
                continue

            def body(h, ops=ops):
                for f in ops:
                    f(h)
            getattr(block, hmap[e])(body)


class B:
    def __init__(self, t, name, excl=False):
        self.t = t
        self.r = Res(name, excl)

    def __getitem__(self, k):
        return self.t[k]


def build_program():
    nc = bass.Bass("TRN2", target_bir_lowering=False)

    def din(name, shape):
        return nc.dram_tensor(name, list(shape), F32, kind="ExternalInput").ap()

    def dout(name, shape):
        return nc.dram_tensor(name, list(shape), F32, kind="ExternalOutput").ap()

    xp = din("xp", [SEG, D]); xh = din("xh", [SEG, D]); xs_d = din("xs", [NS, D]); mem_d = din("mem", [256, D])
    cwkT = din("cwkT", [16, 128, 3, 1152]); cwv = din("cwv", [16, 128, 9, 384])
    cmkT = din("cmkT", [128, 16, 2, 256]); cmv = din("cmv", [128, 16, 2, 256])
    norm1_g = din("norm1_gT", [128, 8]); norm2_g = din("norm2_gT", [128, 8]); memn_g = din("memn_gT", [128, 8])
    w_in = din("w_in", [D, 2176]); w_mem = din("w_mem_kv", [D, 512]); w_out = din("w_out", [D, D])
    gv_a = din("gv_a", [1, 384]); gqk = din("gqk", [1, 768]); gqm = din("gqm", [1, 256]); gkm = din("gkm", [1, 256])
    w_sT = din("w_sT", [128, 6, 128]); bsB_d = din("bsB", [128, 384])
    bd_ws = din("bd_ws", [64, 6, 64]); bsS_d = din("bsS", [64, 384]); mask_bd = din("mask_bd", [64, 6, 64])
    rope_cs = din("rope_cs", [2 * SEG + NS, 16]); rope_sn = din("rope_sn", [2 * SEG + NS, 16])
    maskA_d = din("maskA", [128, 512]); maskH_d = din("maskH", [128, 512])
    maskS_d = din("maskS", [128, 216]); maskN_d = din("maskN", [64, 384])
    ident_d = din("ident", [128, 128]); onesAB_d = din("onesAB", [128, 192])
    w_router = din("w_router", [D, 32]); b_router = din("b_router", [1, 32])
    w_gu = din("w_gate_up", [NEXP, D, 2048]); w_dn = din("w_down", [NEXP, D, D])
    bguT_d = din("bguT", [128, NEXP, 16]); b_dn = din("b_down", [NEXP, D])

    mconst_d = din("mconst", [128, 111]); triu_d = din("triu", [128, 128]); norm2_row = din("norm2_row", [1, D])

    vscr = nc.dram_tensor("vscr", [2 * SEG, 384], F32, kind="Internal").ap()
    x1scr = nc.dram_tensor("x1scr", [NTOK, D], F32, kind="Internal").ap()
    hn_d = nc.dram_tensor("hn_d", [NTOK + 128, D], BF16, kind="Internal").ap()
    comb_d = nc.dram_tensor("comb_d", [NTOK + 128, 32], F32, kind="Internal").ap()
    inv_d = nc.dram_tensor("inv_d", [54 * 384, 1], I32, kind="Internal").ap()
    res_d = nc.dram_tensor("res_d", [54 * 384, D], F32, kind="Internal").ap()

    y_p = dout("y_p", [SEG, D]); y_s = dout("y_s", [NS, D])
    wk_p = dout("wk_p", [SEG, 384]); wv_p = dout("wv_p", [SEG, 384])
    mk_p = dout("mk_p", [256, 256]); mv_p = dout("mv_p", [256, 256])
    wk_s = dout("wk_s", [NS, 384]); wv_s = dout("wv_s", [NS, 384]); gv_s = dout("gv_s", [NS, 384])

    with contextlib.ExitStack() as top:
        P = Prog(nc)
        for e in Prog.ENGS:
            P.add_sem(e, top.enter_context(nc.semaphore("s_" + e)))
        for k in ("c", "cp", "x0", "x1", "o0", "o1", "v", "kv0", "kv1", "w0", "w1", "wd0", "wd1", "g", "r0", "r1", "r2", "m", "y"):
            P.add_sem(k, top.enter_context(nc.semaphore("d_" + k)))
        block_cm = nc.Block()

        def sbuf(es, name, shape, dt=F32):
            return B(es.enter_context(nc.sbuf_tensor("sb_" + name, list(shape), dt)), name)

        PS = [B(top.enter_context(nc.psum_tensor(f"ps{i}", [128, 512], F32)), f"ps{i}", excl=True) for i in range(8)]

        def act(out, in_, func, reads, writes, **kw):
            P.op("act", lambda h: h.activation(out=out, in_=in_, func=func, **kw),
                 [b.r for b in reads], [b.r for b in writes])

        def tt(eng, out, in0, in1, op, reads, writes):
            P.op(eng, lambda h: h.tensor_tensor(out=out, in0=in0, in1=in1, op=op),
                 [b.r for b in reads], [b.r for b in writes])

        def ts(eng, out, in0, s1, s2, op0, op1, reads, writes, **kw):
            if op1 is None:
                P.op(eng, lambda h: h.tensor_scalar(out=out, in0=in0, scalar1=s1, scalar2=None, op0=op0, **kw),
                     [b.r for b in reads], [b.r for b in writes])
            else:
                P.op(eng, lambda h: h.tensor_scalar(out=out, in0=in0, scalar1=s1, scalar2=s2, op0=op0, op1=op1, **kw),
                     [b.r for b in reads], [b.r for b in writes])

        def stt(out, in0, scalar, in1, op0, op1, reads, writes, **kw):
            P.op("dve", lambda h: h.scalar_tensor_tensor(out=out, in0=in0, scalar=scalar, in1=in1, op0=op0, op1=op1, **kw),
                 [b.r for b in reads], [b.r for b in writes])

        def cp(eng, out, in_, reads, writes):
            if eng == "act":
                act(out, in_, AF.Copy, reads, writes)
            else:
                P.op(eng, lambda h: h.tensor_copy(out, in_), [b.r for b in reads], [b.r for b in writes])

        def mm(out, lhsT, rhs, start, stop, reads, writes):
            P.op("pe", lambda h: h.matmul(out, lhsT=lhsT, rhs=rhs, start=start, stop=stop),
                 [b.r for b in reads], [b.r for b in writes])

        def tr(out, in_, ident, reads, writes):
            P.op("pe", lambda h: h.transpose(out=out, in_=in_, identity=ident),
                 [b.r for b in reads], [b.r for b in writes])

        def recip(out, in_, reads, writes):
            P.op("dve", lambda h: h.reciprocal(out, in_), [b.r for b in reads], [b.r for b in writes])

        def red(out, in_, reads, writes):
            P.op("dve", lambda h: h.tensor_reduce(out=out, in_=in_, axis=AX.X, op=ALU.add),
                 [b.r for b in reads], [b.r for b in writes])

        def dma(eng, sem, out, in_, reads=(), writes=(), **kw):
            P.dma(eng, sem, out, in_, [b.r for b in reads], [b.r for b in writes], **kw)

        def rstd_from_ssq(ssq, std, rstd, n, width, rows):
            act(std[0:rows, 0:n], ssq[0:rows, 0:n], AF.Sqrt, [ssq], [std], scale=1.0 / width, bias=EPS)
            recip(rstd[0:rows, 0:n], std[0:rows, 0:n], [std], [rstd])

        ident_f = sbuf(top, "ident_f", [128, 128]); ident_b = sbuf(top, "ident_b", [128, 128], BF16)
        dma("sp", "c", ident_f[:], ident_d[:, :], writes=[ident_f])
        cp("dve", ident_b[:], ident_f[:], [ident_f], [ident_b])

        with contextlib.ExitStack() as sA:
            oaT = sbuf(sA, "oaT", [128, 3, NTOK], BF16)
            mixB = sbuf(sA, "mixB", [128, 3, NTOK], BF16)
            mixM = sbuf(sA, "mixM", [128, 2, NTOK], BF16)
            with contextlib.ExitStack() as sB:
                qT = sbuf(sB, "qT", [128, 3, SEG], BF16)
                kT = sbuf(sB, "kT", [128, 3, 2 * SEG], BF16)
                qmT = sbuf(sB, "qmT", [128, 2, NTOK], BF16)
                kmT = sbuf(sB, "kmT", [128, 2, 256], BF16)
                vmp = sbuf(sB, "vmp", [128, 2, 2, 192], BF16)
                qTs = sbuf(sB, "qTs", [128, 3, NS], BF16)
                kTs = sbuf(sB, "kTs", [128, 3, NS], BF16)
                vsp = sbuf(sB, "vsp", [64, 3, 192], BF16)
                onesAB = sbuf(sB, "onesAB", [128, 192], BF16)
                dma("pool", "cp", onesAB[:], onesAB_d[:, :], writes=[onesAB])
                P.op("pool", lambda h: h.memset(vmp[:], 0.0), [], [vmp.r])
                P.op("pool", lambda h: h.memset(vsp[:], 0.0), [], [vsp.r])

                with contextlib.ExitStack() as s1:
                    w_in_b = sbuf(s1, "w_in_b", [128, 8, 2176], BF16)
                    w_mem_b = sbuf(s1, "w_mem_b", [128, 8, 512], BF16)
                    for hh in range(2):
                        dma("pool", "cp", w_in_b[:, :, hh * 1088:(hh + 1) * 1088],
                            w_in.rearrange("(c p) n -> p c n", p=128)[:, :, hh * 1088:(hh + 1) * 1088], writes=[w_in_b])
                    dma("pool", "cp", w_mem_b[:], w_mem.rearrange("(c p) n -> p c n", p=128), writes=[w_mem_b])
                    wsT_f = sbuf(s1, "wsT_f", [128, 6, 128]); wsT_b = sbuf(s1, "wsT_b", [128, 6, 128], BF16)
                    mcur_f = sbuf(s1, "mcur_f", [128, 128])
                    dma("sp", "c", wsT_f[:], w_sT[:, :, :], writes=[wsT_f])
                    dma("sp", "c", mcur_f[:], maskA_d[:, 384:512], writes=[mcur_f])
                    tt("dve", wsT_b[:], wsT_f[:], mcur_f[:].unsqueeze(1).to_broadcast([128, 6, 128]), ALU.mult,
                       [wsT_f, mcur_f], [wsT_b])
                    bd_f = sbuf(s1, "bd_f", [64, 6, 64]); mbd_f = sbuf(s1, "mbd_f", [64, 6, 64]); bd_b = sbuf(s1, "bd_b", [64, 6, 64], BF16)
                    dma("sp", "c", bd_f[:], bd_ws[:, :, :], writes=[bd_f])
                    dma("sp", "c", mbd_f[:], mask_bd[:, :, :], writes=[mbd_f])
                    tt("dve", bd_b[:], bd_f[:], mbd_f[:], ALU.mult, [bd_f, mbd_f], [bd_b])
                    bsB = sbuf(s1, "bsB", [128, 384]); bsS = sbuf(s1, "bsS", [64, 384])
                    dma("sp", "c", bsB[:], bsB_d[:, :], writes=[bsB])
                    dma("sp", "c", bsS[:], bsS_d[:, :], writes=[bsS])
                    g1c = sbuf(s1, "g1c", [128, 8]); gmc = sbuf(s1, "gmc", [128, 8])
                    dma("sp", "c", g1c[:], norm1_g[:, :], writes=[g1c])
                    dma("sp", "c", gmc[:], memn_g[:, :], writes=[gmc])
                    gvB = sbuf(s1, "gvB", [128, 384]); gqkB = sbuf(s1, "gqkB", [128, 768])
                    gqmB = sbuf(s1, "gqmB", [128, 256]); gkmB = sbuf(s1, "gkmB", [128, 256])
                    dma("sp", "c", gvB[:], gv_a.partition_broadcast(128), writes=[gvB])
                    dma("sp", "c", gqkB[:], gqk.partition_broadcast(128), writes=[gqkB])
                    dma("sp", "c", gqmB[:], gqm.partition_broadcast(128), writes=[gqmB])
                    dma("sp", "c", gkmB[:], gkm.partition_broadcast(128), writes=[gkmB])

                    class WS:
                        pass
                    wsets = []
                    for par in range(2):
                        w = WS()
                        sfx = f"_{par}"
                        w.xt = sbuf(s1, "xt" + sfx, [128, D]); w.junk = sbuf(s1, "junk" + sfx, [128, D], BF16)
                        w.ssq = sbuf(s1, "ssq" + sfx, [128, 1]); w.std = sbuf(s1, "std" + sfx, [128, 1]); w.rstd = sbuf(s1, "rstd" + sfx, [128, 1])
                        w.xs = sbuf(s1, "xs" + sfx, [128, D], BF16); w.hT = sbuf(s1, "hT" + sfx, [128, 8, 128], BF16)
                        w.u32 = sbuf(s1, "u32" + sfx, [128, 384])
                        w.ssqv = sbuf(s1, "ssqv" + sfx, [128, 1]); w.stdv = sbuf(s1, "stdv" + sfx, [128, 1]); w.rstdv = sbuf(s1, "rstdv" + sfx, [128, 1])
                        w.vn32 = sbuf(s1, "vn32" + sfx, [128, 384]); w.vnb = sbuf(s1, "vnb" + sfx, [128, 384], BF16)
                        w.sqt = sbuf(s1, "sqt" + sfx, [128, 768])
                        w.ssqk = sbuf(s1, "ssqk" + sfx, [128, 12]); w.stdk = sbuf(s1, "stdk" + sfx, [128, 12]); w.rstdk = sbuf(s1, "rstdk" + sfx, [128, 12])
                        w.qk32 = sbuf(s1, "qk32" + sfx, [128, 12, 64])
                        w.rt1 = sbuf(s1, "rt1" + sfx, [128, 12, 16]); w.rt2 = sbuf(s1, "rt2" + sfx, [128, 12, 16])
                        w.qkb = sbuf(s1, "qkb" + sfx, [128, 768], BF16)
                        w.v32 = sbuf(s1, "v32" + sfx, [128, 384])
                        w.ssqm = sbuf(s1, "ssqm" + sfx, [128, 4]); w.stdm = sbuf(s1, "stdm" + sfx, [128, 4]); w.rstdm = sbuf(s1, "rstdm" + sfx, [128, 4])
                        w.qm32 = sbuf(s1, "qm32" + sfx, [128, 4, 64]); w.qmb = sbuf(s1, "qmb" + sfx, [128, 256], BF16)
                        w.g1 = sbuf(s1, "g1" + sfx, [128, 384]); w.oab = sbuf(s1, "oab" + sfx, [128, 384], BF16)
                        w.cs = sbuf(s1, "cs" + sfx, [128, 16]); w.sn = sbuf(s1, "sn" + sfx, [128, 16])
                        wsets.append(w)

                    PSxT = PS[0]
                    psxT_b = PS[0][:].bitcast(BF16)
                    ps7_b = PS[7][:].bitcast(BF16)

                    def load_x(idx):
                        kind, it, nt, src_ap, ropebase = tiles1[idx]
                        par = idx % 2
                        w = wsets[par]
                        dma("sp", f"x{par}", w.xt[0:nt, :], src_ap, writes=[w.xt])
                        if ropebase is not None:
                            dma("sp", f"x{par}", w.cs[0:nt, :], rope_cs[ropebase:ropebase + nt, :], writes=[w.cs])
                            dma("sp", f"x{par}", w.sn[0:nt, :], rope_sn[ropebase:ropebase + nt, :], writes=[w.sn])

                    def norm_hT(w, src_ap, nt, gcol, par):
                        act(w.junk[0:nt, :], w.xt[0:nt, :], AF.Square, [w.xt], [w.junk, w.ssq], accum_out=w.ssq[0:nt, :])
                        rstd_from_ssq(w.ssq, w.std, w.rstd, 1, D, nt)
                        act(w.xs[0:nt, :], w.xt[0:nt, :], AF.Copy, [w.xt, w.rstd], [w.xs], scale=w.rstd[0:nt, :])
                        for c in range(8):
                            tr(psxT_b[:, c * 128:c * 128 + nt], w.xs[0:nt, c * 128:(c + 1) * 128], ident_b[0:nt, 0:nt],
                               [w.xs, ident_b], [PSxT])
                        tt("dve", w.hT[:, :, 0:nt], psxT_b.rearrange("p (c t) -> p c t", c=8)[:, :, 0:nt],
                           gcol[:, 0:8].unsqueeze(2).to_broadcast([128, 8, nt]), ALU.mult, [PSxT, gcol], [w.hT])

                    def proj(w, bank, wt, c0, width, nt):
                        for k in range(8):
                            mm(bank[0:nt, 0:width], w.hT[:, k, 0:nt], wt[:, k, c0:c0 + width], k == 0, k == 7,
                               [w.hT, wt], [bank])

                    def head_norm(w, bank, nh, h0, sq, ssq, std, rstd, dst32, nt, sq_off):
                        act(sq[0:nt, sq_off:sq_off + nh * 64], bank[0:nt, 0:nh * 64], AF.Square, [bank], [sq])

                    def rope(w, h0, nh, nt):
                        q = w.qk32
                        csb = w.cs[0:nt, :].unsqueeze(1).to_broadcast([nt, nh, 16])
                        tt("pool", w.rt1[0:nt, h0:h0 + nh, :], q[0:nt, h0:h0 + nh, 0:16], csb, ALU.mult, [w.qk32, w.cs], [w.rt1])
                        tt("pool", w.rt2[0:nt, h0:h0 + nh, 0:8], q[0:nt, h0:h0 + nh, 8:16],
                           w.sn[0:nt, 0:8].unsqueeze(1).to_broadcast([nt, nh, 8]), ALU.mult, [w.qk32, w.sn], [w.rt2])
                        tt("pool", w.rt2[0:nt, h0:h0 + nh, 8:16], q[0:nt, h0:h0 + nh, 0:8],
                           w.sn[0:nt, 8:16].unsqueeze(1).to_broadcast([nt, nh, 8]), ALU.mult, [w.qk32, w.sn], [w.rt2])
                        tt("pool", q[0:nt, h0:h0 + nh, 0:16], w.rt1[0:nt, h0:h0 + nh, :], w.rt2[0:nt, h0:h0 + nh, :], ALU.add,
                           [w.rt1, w.rt2], [w.qk32])

                    def qk_norm_rope(w, nt, with_q, ropebase):
                        h0 = 0 if with_q else 6
                        nh = 12 - h0
                        if with_q:
                            act(w.sqt[0:nt, 0:384], PS[3][0:nt, 0:384], AF.Square, [PS[3]], [w.sqt])
                        act(w.sqt[0:nt, 384:768], PS[4][0:nt, 0:384], AF.Square, [PS[4]], [w.sqt])
                        red(w.ssqk[0:nt, h0:12], w.sqt[0:nt, h0 * 64:768].rearrange("p (h d) -> p h d", d=64), [w.sqt], [w.ssqk])
                        act(w.stdk[0:nt, h0:12], w.ssqk[0:nt, h0:12], AF.Sqrt, [w.ssqk], [w.stdk], scale=1.0 / 64, bias=EPS)
                        recip(w.rstdk[0:nt, h0:12], w.stdk[0:nt, h0:12], [w.stdk], [w.rstdk])
                        if with_q:
                            tt("dve", w.qk32[0:nt, 0:6, :], PS[3][0:nt, 0:384].rearrange("p (h d) -> p h d", d=64),
                               w.rstdk[0:nt, 0:6].unsqueeze(2).to_broadcast([nt, 6, 64]), ALU.mult, [PS[3], w.rstdk], [w.qk32])
                        tt("dve", w.qk32[0:nt, 6:12, :], PS[4][0:nt, 0:384].rearrange("p (h d) -> p h d", d=64),
                           w.rstdk[0:nt, 6:12].unsqueeze(2).to_broadcast([nt, 6, 64]), ALU.mult, [PS[4], w.rstdk], [w.qk32])
                        tt("pool", w.qk32[0:nt, h0:12, :], w.qk32[0:nt, h0:12, :],
                           gqkB[0:nt, h0 * 64:768].rearrange("p (h d) -> p h d", d=64), ALU.mult, [w.qk32, gqkB], [w.qk32])
                        rope(w, h0, nh, nt)
                        cp("act", w.qkb[0:nt, h0 * 64:768], w.qk32[0:nt, h0:12, :].rearrange("p h d -> p (h d)"), [w.qk32], [w.qkb])

                    front_done = set()

                    def do_front(idx):
                        if idx in front_done or idx >= len(tiles1):
                            return
                        kind_, it_, nt_, src_, rb_ = tiles1[idx]
                        norm_hT(wsets[idx % 2], src_, nt_, gmc if kind_ == "mem" else g1c, idx % 2)
                        front_done.add(idx)

                    tiles1 = [("mem", mt, 128, mem_d[mt * 128:(mt + 1) * 128, :], None) for mt in range(2)]
                    tiles1 += [("halo", it, 128, xh[it * 128:(it + 1) * 128, :], SEG + it * 128) for it in range(16)]
                    tiles1 += [("own", it, 128, xp[it * 128:(it + 1) * 128, :], it * 128) for it in range(16)]
                    tiles1 += [("samp", 0, NS, xs_d[0:NS, :], 2 * SEG)]
                    load_x(0)

                    tcount = 0
                    for mt in range(2):
                        par = tcount % 2
                        load_x(tcount + 1)
                        cur = tcount
                        tcount += 1
                        w = wsets[par]
                        do_front(cur)
                        proj(w, PS[1], w_mem_b, 0, 512, 128)
                        act(w.sqt[:, 0:256], PS[1][:, 0:256], AF.Square, [PS[1]], [w.sqt])
                        red(w.ssqm[:, 0:4], w.sqt[:, 0:256].rearrange("p (h d) -> p h d", d=64), [w.sqt], [w.ssqm])
                        act(w.stdm[:, 0:4], w.ssqm[:, 0:4], AF.Sqrt, [w.ssqm], [w.stdm], scale=1.0 / 64, bias=EPS)
                        recip(w.rstdm[:, 0:4], w.stdm[:, 0:4], [w.stdm], [w.rstdm])
                        tt("dve", w.qm32[:, :, :], PS[1][:, 0:256].rearrange("p (h d) -> p h d", d=64),
                           w.rstdm[:, 0:4].unsqueeze(2).to_broadcast([128, 4, 64]), ALU.mult, [PS[1], w.rstdm], [w.qm32])
                        tt("pool", w.qm32[:, :, :], w.qm32[:, :, :], gkmB[:, :].rearrange("p (h d) -> p h d", d=64), ALU.mult,
                           [w.qm32, gkmB], [w.qm32])
                        dma("sp", f"o{par}", mk_p[mt * 128:(mt + 1) * 128, :], w.qm32[:, :, :].rearrange("p h d -> p (h d)"),
                            reads=[w.qm32], is_output=True)
                        cp("act", w.qmb[:, :], w.qm32[:, :, :].rearrange("p h d -> p (h d)"), [w.qm32], [w.qmb])
                        for pp in range(2):
                            tr(ps7_b[:, pp * 128:(pp + 1) * 128], w.qmb[:, pp * 128:(pp + 1) * 128], ident_b[:, :], [w.qmb, ident_b], [PS[7]])
                        cp("dve", kmT[:, :, mt * 128:(mt + 1) * 128], ps7_b[:, 0:256].rearrange("p (c t) -> p c t", c=2), [PS[7]], [kmT])
                        cp("act", w.v32[:, 0:256], PS[1][:, 256:512], [PS[1]], [w.v32])
                        do_front(cur + 1)
                        dma("sp", f"o{par}", mv_p[mt * 128:(mt + 1) * 128, :], w.v32[:, 0:256], reads=[w.v32], is_output=True)
                        for i in range(2):
                            cp("pool", vmp[:, mt, :, i * 128:i * 128 + 64],
                               w.v32[:, 0:256].rearrange("p (c i d) -> p c i d", c=2, i=2)[:, :, i, :], [w.v32], [vmp])

                    for it in range(16):
                        par = tcount % 2
                        load_x(tcount + 1)
                        cur = tcount
                        tcount += 1
                        w = wsets[par]
                        do_front(cur)
                        proj(w, PS[4], w_in_b, 1152, 384, 128)
                        proj(w, PS[5], w_in_b, 1536, 384, 128)
                        qk_norm_rope(w, 128, False, SEG + it * 128)
                        cp("act", w.v32[:, :], PS[5][:, 0:384], [PS[5]], [w.v32])
                        do_front(cur + 1)
                        for pp in range(3):
                            tr(ps7_b[:, (3 + pp) * 128:(4 + pp) * 128], w.qkb[:, 384 + pp * 128:384 + (pp + 1) * 128], ident_b[:, :],
                               [w.qkb, ident_b], [PS[7]])
                        cp("act", kT[:, :, it * 128:(it + 1) * 128], ps7_b[:, 384:768].rearrange("p (c t) -> p c t", c=3), [PS[7]], [kT])
                        dma("sp", f"o{par}", vscr[it * 128:(it + 1) * 128, :], w.v32[:, :], reads=[w.v32])

                    tiles = [("own", it, 128) for it in range(16)] + [("samp", 0, NS)]
                    for kind, it, nt in tiles:
                        par = tcount % 2
                        if tcount + 1 < len(tiles1):
                            load_x(tcount + 1)
                        cur = tcount
                        tcount += 1
                        w = wsets[par]
                        samp = kind == "samp"
                        tok0 = SEG if samp else it * 128
                        src = xs_d[0:NS, :] if samp else xp[it * 128:(it + 1) * 128, :]
                        do_front(cur)
                        for j in range(6):
                            proj(w, PS[1 + j], w_in_b, j * 384, 384 if j < 5 else 256, nt)
                        cp("act", w.u32[0:nt, :], PS[1][0:nt, 0:384], [PS[1]], [w.u32])
                        act(w.junk[0:nt, 0:384], PS[2][0:nt, 0:384], AF.Square, [PS[2]], [w.junk, w.ssqv], accum_out=w.ssqv[0:nt, :])
                        rstd_from_ssq(w.ssqv, w.stdv, w.rstdv, 1, 384, nt)
                        stt(w.vn32[0:nt, :], PS[2][0:nt, 0:384], w.rstdv[0:nt, :], gvB[0:nt, :], ALU.mult, ALU.mult,
                            [PS[2], w.rstdv, gvB], [w.vn32])
                        cp("pool", w.vnb[0:nt, :], w.vn32[0:nt, :], [w.vn32], [w.vnb])
                        if samp:
                            dma("sp", f"o{par}", gv_s[:, :], w.vn32[0:nt, :], reads=[w.vn32], is_output=True)
                        ropebase = 2 * SEG if samp else it * 128
                        qk_norm_rope(w, nt, True, ropebase)
                        if samp:
                            dma("sp", f"o{par}", wk_s[:, :], w.qk32[0:nt, 6:12, :].rearrange("p h d -> p (h d)"), reads=[w.qk32], is_output=True)
                        else:
                            dma("sp", f"o{par}", wk_p[it * 128:(it + 1) * 128, :], w.qk32[0:nt, 6:12, :].rearrange("p h d -> p (h d)"),
                                reads=[w.qk32], is_output=True)
                        cp("act", w.v32[0:nt, :], PS[5][0:nt, 0:384], [PS[5]], [w.v32])
                        if samp:
                            dma("sp", f"o{par}", wv_s[:, :], w.v32[0:nt, :], reads=[w.v32], is_output=True)
                            for i in range(2):
                                cp("pool", vsp[0:nt, :, i * 128:i * 128 + 64],
                                   w.v32[0:nt, :].rearrange("p (c i d) -> p c i d", c=3, i=2)[:, :, i, :], [w.v32], [vsp])
                        else:
                            dma("sp", f"o{par}", wv_p[it * 128:(it + 1) * 128, :], w.v32[0:nt, :], reads=[w.v32], is_output=True)
                            dma("sp", f"o{par}", vscr[SEG + it * 128:SEG + (it + 1) * 128, :], w.v32[0:nt, :], reads=[w.v32])
                        act(w.sqt[0:nt, 0:256], PS[6][0:nt, 0:256], AF.Square, [PS[6]], [w.sqt])
                        red(w.ssqm[0:nt, 0:4], w.sqt[0:nt, 0:256].rearrange("p (h d) -> p h d", d=64), [w.sqt], [w.ssqm])
                        act(w.stdm[0:nt, 0:4], w.ssqm[0:nt, 0:4], AF.Sqrt, [w.ssqm], [w.stdm], scale=1.0 / 64, bias=EPS)
                        recip(w.rstdm[0:nt, 0:4], w.stdm[0:nt, 0:4], [w.stdm], [w.rstdm])
                        tt("dve", w.qm32[0:nt, :, :], PS[6][0:nt, 0:256].rearrange("p (h d) -> p h d", d=64),
                           w.rstdm[0:nt, 0:4].unsqueeze(2).to_broadcast([nt, 4, 64]), ALU.mult, [PS[6], w.rstdm], [w.qm32])
                        tt("pool", w.qmb[0:nt, :].rearrange("p (h d) -> p h d", d=64), w.qm32[0:nt, :, :],
                           gqmB[0:nt, :].rearrange("p (h d) -> p h d", d=64), ALU.mult, [w.qm32, gqmB], [w.qmb])
                        do_front(cur + 1)
                        for pp in range(3):
                            tr(ps7_b[:, pp * 128:pp * 128 + nt], w.qkb[0:nt, pp * 128:(pp + 1) * 128], ident_b[0:nt, 0:nt], [w.qkb, ident_b], [PS[7]])
                        for pp in range(3):
                            tr(ps7_b[:, (3 + pp) * 128:(3 + pp) * 128 + nt], w.qkb[0:nt, 384 + pp * 128:384 + (pp + 1) * 128], ident_b[0:nt, 0:nt],
                               [w.qkb, ident_b], [PS[7]])
                        for pp in range(2):
                            tr(ps7_b[:, (6 + pp) * 128:(6 + pp) * 128 + nt], w.qmb[0:nt, pp * 128:(pp + 1) * 128], ident_b[0:nt, 0:nt],
                               [w.qmb, ident_b], [PS[7]])
                        p7v = ps7_b.rearrange("p (c t) -> p c t", c=8)
                        if samp:
                            cp("dve", qTs[:, :, :], p7v[:, 0:3, 0:nt], [PS[7]], [qTs])
                            cp("act", kTs[:, :, :], p7v[:, 3:6, 0:nt], [PS[7]], [kTs])
                        else:
                            cp("dve", qT[:, :, it * 128:(it + 1) * 128], p7v[:, 0:3, :], [PS[7]], [qT])
                            cp("act", kT[:, :, SEG + it * 128:SEG + (it + 1) * 128], p7v[:, 3:6, :], [PS[7]], [kT])
                        cp("dve", qmT[:, :, tok0:tok0 + nt], p7v[:, 6:8, 0:nt], [PS[7]], [qmT])
                        for gi in range(6):
                            lhs = bd_b[0:nt, gi, 0:nt] if samp else wsT_b[:, gi, :]
                            mm(PS[0][0:nt, gi * 64:(gi + 1) * 64], lhs, w.vnb[0:nt, gi * 64:(gi + 1) * 64], True, True,
                               [bd_b if samp else wsT_b, w.vnb], [PS[0]])
                        bsrc = bsS if samp else bsB
                        tt("dve", w.g1[0:nt, :], PS[0][0:nt, 0:384], bsrc[0:nt, :], ALU.add, [PS[0], bsrc], [w.g1])
                        tt("pool", w.oab[0:nt, :], w.g1[0:nt, :], w.u32[0:nt, :], ALU.mult, [w.g1, w.u32], [w.oab])
                        for pp in range(3):
                            tr(ps7_b[:, pp * 128:pp * 128 + nt], w.oab[0:nt, pp * 128:(pp + 1) * 128], ident_b[0:nt, 0:nt], [w.oab, ident_b], [PS[7]])
                        cp("act", oaT[:, :, tok0:tok0 + nt], p7v[:, 0:3, 0:nt], [PS[7]], [oaT])

                P.barrier()
                with contextlib.ExitStack() as s2:
                  if KSTOP >= 2:
                    maskA = sbuf(s2, "maskA", [128, 512], BF16); maskH = sbuf(s2, "maskH", [128, 512], BF16)
                    maskS = sbuf(s2, "maskS", [128, 216], BF16); maskN = sbuf(s2, "maskN", [64, 384], BF16)
                    dma("pool", "cp", maskA[:], maskA_d[:, :], writes=[maskA])
                    dma("pool", "cp", maskH[:], maskH_d[:, :], writes=[maskH])
                    dma("pool", "cp", maskS[:], maskS_d[:, :], writes=[maskS])
                    dma("pool", "cp", maskN[:], maskN_d[:, :], writes=[maskN])
                    Pf = [sbuf(s2, f"Pf{i}", [128, 512], BF16) for i in range(2)]
                    Pm = [sbuf(s2, f"Pm{i}", [128, 512], BF16) for i in range(2)]
                    rl = sbuf(s2, "rl", [128, 512])
                    s2a = contextlib.ExitStack()
                    acc = sbuf(s2a, "acc", [128, 2, 3, SEG])
                    vbuf = sbuf(s2a, "vbuf", [128, 32, 3, 192], BF16)
                    P.op("pool", lambda h: h.memset(vbuf[:], 0.0), [], [vbuf.r])

                    def vload(dil):
                        own = vscr[SEG:2 * SEG, :]
                        halo = vscr[0:SEG, :]
                        if dil == 1:
                            srcs = [(own.rearrange("(n j) f -> j n f", j=128), 0, 16),
                                    (halo[SEG - 128:SEG, :].rearrange("(n j) f -> j n f", j=128), 16, 1)]
                        elif dil == 4:
                            srcs = [(own[n * 512:(n + 1) * 512, :].rearrange("(j r) f -> j r f", r=4), 4 * n, 4) for n in range(4)]
                            srcs += [(halo[SEG - 512:SEG, :].rearrange("(j r) f -> j r f", r=4), 16, 4)]
                        else:
                            srcs = [(own.rearrange("(j r) f -> j r f", r=16), 0, 16),
                                    (halo.rearrange("(j r) f -> j r f", r=16), 16, 16)]
                        for src, t0, ntile in srcs:
                            for i in range(2):
                                for c in range(3):
                                    s = src.rearrange("j t (c i d) -> j t c i d", c=3, i=2)[:, :, c, i, :]
                                    d = vbuf[:, t0:t0 + ntile, c, i * 128:i * 128 + 64]
                                    dma("pool", "v", d, s, writes=[vbuf])

                    it2 = 0
                    for bi, dil in enumerate((1, 4, 16)):
                        vload(dil)
                        nblk = 16 // dil
                        bspan = 128 * dil
                        for r in range(dil):
                            for n in range(nblk):
                                qs = slice(n * bspan + r, (n + 1) * bspan, dil)
                                kc = slice(SEG + n * bspan + r, SEG + (n + 1) * bspan, dil)
                                kp = slice(SEG + (n - 1) * bspan + r, SEG + n * bspan, dil)
                                vt_c = n * dil + r
                                vt_p = (n - 1) * dil + r if n > 0 else 16 + r
                                msk = maskH if n == 0 else maskA
                                for p in range(3):
                                    sl = it2 % 2; it2 += 1
                                    psSi = (PS[sl], PS[4 + sl]); psO = PS[2 + sl]
                                    for kt, kcols in enumerate((kp, kc)):
                                        for i in range(2):
                                            mm(psSi[i][:, kt * 128:(kt + 1) * 128], kT[64 * i:64 * i + 64, p, kcols], qT[64 * i:64 * i + 64, p, qs],
                                               True, True, [kT, qT], [psSi[i]])
                                    for i in range(2):
                                        act(Pf[sl][:, i * 256:(i + 1) * 256], psSi[i][:, 0:256], AF.Exp, [psSi[i]], [Pf[sl]], scale=SCALE)
                                    tt("pool", Pm[sl][:, :], Pf[sl][:, :], msk[:, :], ALU.mult, [Pf[sl], msk], [Pm[sl]])
                                    for part in range(2):
                                        k = 0
                                        for kt, vt in enumerate((vt_p, vt_c)):
                                            for i in range(2):
                                                s = i * 2 + kt
                                                lhs = vbuf[:, vt, p, i * 64:i * 64 + 128] if part == 0 else onesAB[:, i * 64:i * 64 + 128]
                                                mm(psO[:, part * 128:(part + 1) * 128], lhs, Pm[sl][:, s * 128:(s + 1) * 128], k == 0, k == 3,
                                                   [vbuf if part == 0 else onesAB, Pm[sl]], [psO])
                                                k += 1
                                    accv = acc[:, :, p, qs]
                                    pov = psO[:, 0:256].rearrange("p (a q) -> p a q", a=2)
                                    if bi == 0:
                                        cp("dve", accv, pov, [psO], [acc])
                                    else:
                                        tt("dve", accv, pov, accv, ALU.add, [psO, acc], [acc])
                    for p in range(3):
                        for q0 in range(0, SEG, 512):
                            recip(rl[:, :], acc[:, 1, p, q0:q0 + 512], [acc], [rl])
                            tt("pool", mixB[:, p, q0:q0 + 512], acc[:, 0, p, q0:q0 + 512], rl[:, :], ALU.mult, [acc, rl], [mixB])
                    P.barrier()
                    s2a.close()

                    for q0 in range(0, SEG, 512):
                        for p in range(2):
                            psO = PS[2]; psL = PS[3]
                            k = 0
                            for mt in range(2):
                                for i in range(2):
                                    sl = it2 % 2; it2 += 1
                                    h = 2 * p + i
                                    mm(PS[sl][:, :], kmT[64 * i:64 * i + 64, p, mt * 128:(mt + 1) * 128], qmT[64 * i:64 * i + 64, p, q0:q0 + 512],
                                       True, True, [kmT, qmT], [PS[sl]])
                                    act(Pf[sl][:, :], PS[sl][:, :], AF.Exp, [PS[sl]], [Pf[sl]], scale=SCALE)
                                    mm(psO[:, :], vmp[:, mt, p, i * 64:i * 64 + 128], Pf[sl][:, :], k == 0, k == 3, [vmp, Pf[sl]], [psO])
                                    mm(psL[:, :], onesAB[:, i * 64:i * 64 + 128], Pf[sl][:, :], k == 0, k == 3, [onesAB, Pf[sl]], [psL])
                                    k += 1
                            recip(rl[:, :], psL[:, :], [psL], [rl])
                            tt("dve", mixM[:, p, q0:q0 + 512], psO[:, :], rl[:, :], ALU.mult, [psO, rl], [mixM])

                    kms = sbuf(s2, "kms", [128, 16, 2, 256], BF16)
                    vms = sbuf(s2, "vms", [128, 16, 2, 2, 192], BF16)
                    dma("pool", "cp", kms[:], cmkT[:, :, :, :], writes=[kms])
                    P.op("pool", lambda h: h.memset(vms[:], 0.0), [], [vms.r])
                    for mt in range(2):
                        for i in range(2):
                            for c in range(2):
                                dma("pool", "cp", vms[:, :, mt, c, i * 128:i * 128 + 64],
                                    cmv[:, :, mt, :].rearrange("m b (c i d) -> m b c i d", c=2, i=2)[:, :, c, i, :], writes=[vms])
                    for b in range(16):
                        for p in range(2):
                            for mt in range(2):
                                for i in range(2):
                                    col = (b * 2 + p) * 2 + mt
                                    mm(PS[i][:, col * 4:col * 4 + 4], kms[64 * i:64 * i + 64, b, p, mt * 128:(mt + 1) * 128],
                                       qmT[64 * i:64 * i + 64, p, SEG + 4 * b:SEG + 4 * b + 4], True, True, [kms, qmT], [PS[i]])
                    for i in range(2):
                        act(Pf[0][:, i * 256:(i + 1) * 256], PS[i][:, 0:256], AF.Exp, [PS[i]], [Pf[0]], scale=SCALE)
                    psO = PS[2]
                    for part in range(2):
                        for b in range(16):
                            for p in range(2):
                                k = 0
                                for mt in range(2):
                                    for i in range(2):
                                        col = i * 64 + (b * 2 + p) * 2 + mt
                                        lhs = vms[:, b, mt, p, i * 64:i * 64 + 128] if part == 0 else onesAB[:, i * 64:i * 64 + 128]
                                        mm(psO[:, part * 128 + p * 64 + 4 * b:part * 128 + p * 64 + 4 * b + 4], lhs, Pf[0][:, col * 4:col * 4 + 4],
                                           k == 0, k == 3, [vms if part == 0 else onesAB, Pf[0]], [psO])
                                        k += 1
                    recip(rl[:, 0:128], psO[:, 128:256], [psO], [rl])
                    tt("dve", mixM[:, :, SEG:SEG + NS], psO[:, 0:128].rearrange("p (c q) -> p c q", c=2),
                       rl[:, 0:128].rearrange("p (c q) -> p c q", c=2), ALU.mult, [psO, rl], [mixM])

                    kws = [sbuf(s2, f"kws{i}", [128, 3, 1152], BF16) for i in range(2)]
                    vws = [sbuf(s2, f"vws{i}", [128, 9, 3, 192], BF16) for i in range(2)]
                    for i in range(2):
                        P.op("pool", lambda h, i=i: h.memset(vws[i][:], 0.0), [], [vws[i].r])
                    onew = sbuf(s2, "onew", [128, 2, 3, 64])
                    psNi = (PS[4], PS[3])
                    for p in range(3):
                        for i in range(2):
                            mm(psNi[i][0:64, p * 64:(p + 1) * 64], kTs[64 * i:64 * i + 64, p, :], qTs[64 * i:64 * i + 64, p, :],
                               True, True, [kTs, qTs], [psNi[i]])
                    for i in range(2):
                        act(Pf[1][0:64, i * 192:(i + 1) * 192], psNi[i][0:64, 0:192], AF.Exp, [psNi[i]], [Pf[1]], scale=SCALE)
                    tt("pool", Pm[1][0:64, 0:384], Pf[1][0:64, 0:384], maskN[:, :], ALU.mult, [Pf[1], maskN], [Pm[1]])
                    psNO = PS[5]
                    for part in range(2):
                        for p in range(3):
                            for i in range(2):
                                lhs = vsp[0:64, p, i * 64:i * 64 + 128] if part == 0 else onesAB[0:64, i * 64:i * 64 + 128]
                                mm(psNO[:, (part * 3 + p) * 64:(part * 3 + p + 1) * 64], lhs, Pm[1][0:64, (i * 3 + p) * 64:(i * 3 + p + 1) * 64],
                                   i == 0, i == 1, [vsp if part == 0 else onesAB, Pm[1]], [psNO])
                    cp("dve", onew[:, :, :, :], psNO[:, 0:384].rearrange("p (a c q) -> p a c q", a=2, c=3), [psNO], [onew])
                    psW = PS[6]
                    for b in range(16):
                        sl = b % 2
                        dma("pool", f"kv{sl}", kws[sl][:], cwkT[b], writes=[kws[sl]])
                        for i in range(2):
                            for c in range(3):
                                dma("pool", f"kv{sl}", vws[sl][:, :, c, i * 128:i * 128 + 64],
                                    cwv[b].rearrange("k t (c i d) -> k t c i d", c=3, i=2)[:, :, c, i, :], writes=[vws[sl]])
                        psSi = (PS[sl], PS[2 + sl])
                        for t9 in range(9):
                            for p in range(3):
                                for i in range(2):
                                    col = t9 * 3 + p
                                    mm(psSi[i][:, col * 4:col * 4 + 4], kws[sl][64 * i:64 * i + 64, p, t9 * 128:(t9 + 1) * 128],
                                       qTs[64 * i:64 * i + 64, p, 4 * b:4 * b + 4], True, True, [kws[sl], qTs], [psSi[i]])
                        for i in range(2):
                            act(Pf[sl][:, i * 108:(i + 1) * 108], psSi[i][:, 0:108], AF.Exp, [psSi[i]], [Pf[sl]], scale=SCALE)
                        tt("pool", Pm[sl][:, 0:216], Pf[sl][:, 0:216], maskS[:, :], ALU.mult, [Pf[sl], maskS], [Pm[sl]])
                        for part in range(2):
                            for p in range(3):
                                k = 0
                                for t9 in range(9):
                                    for i in range(2):
                                        col = i * 27 + t9 * 3 + p
                                        lhs = vws[sl][:, t9, p, i * 64:i * 64 + 128] if part == 0 else onesAB[:, i * 64:i * 64 + 128]
                                        c0 = (part * 3 + p) * 64 + 4 * b
                                        mm(psW[:, c0:c0 + 4], lhs, Pm[sl][:, col * 4:col * 4 + 4], k == 0, k == 17,
                                           [vws[sl] if part == 0 else onesAB, Pm[sl]], [psW])
                                        k += 1
                    tt("dve", onew[:, :, :, :], psW[:, 0:384].rearrange("p (a c q) -> p a c q", a=2, c=3), onew[:, :, :, :], ALU.add,
                       [psW, onew], [onew])
                    recip(rl[:, 0:192], onew[:, 1, :, :].rearrange("p c q -> p (c q)"), [onew], [rl])
                    tt("dve", mixB[:, :, SEG:SEG + NS], onew[:, 0, :, :], rl[:, 0:192].rearrange("p (c q) -> p c q", c=3), ALU.mult,
                       [onew, rl], [mixB])
                P.barrier()
            with contextlib.ExitStack() as s3:
              if KSTOP >= 3:
                w_out_b = sbuf(s3, "w_out_b", [128, 8, D], BF16)
                dma("pool", "cp", w_out_b[:], w_out.rearrange("(c p) n -> p c n", p=128), writes=[w_out_b])
                xt3 = [sbuf(s3, f"xt3_{i}", [128, D]) for i in range(2)]
                x13 = [sbuf(s3, f"x13_{i}", [128, D]) for i in range(2)]
                srcT = [(oaT, 0), (oaT, 1), (oaT, 2), (mixB, 0), (mixB, 1), (mixB, 2), (mixM, 0), (mixM, 1)]
                for j in range(17):
                    nt = 128 if j < 16 else NS
                    par = j % 2
                    src = xp[j * 128:(j + 1) * 128, :] if j < 16 else xs_d[0:NS, :]
                    dma("sp", f"x{par}", xt3[par][0:nt, :], src, writes=[xt3[par]])
                    for hf in range(2):
                        bank = PS[(j * 2 + hf) % 4]
                        for c, (sb_, ci) in enumerate(srcT):
                            mm(bank[0:nt, :], sb_[:, ci, j * 128:j * 128 + nt], w_out_b[:, c, hf * 512:(hf + 1) * 512], c == 0, c == 7,
                               [sb_, w_out_b], [bank])
                        tt("dve", x13[par][0:nt, hf * 512:(hf + 1) * 512], bank[0:nt, :], xt3[par][0:nt, hf * 512:(hf + 1) * 512], ALU.add,
                           [bank, xt3[par]], [x13[par]])
                    dma("sp", f"o{par}", x1scr[j * 128:j * 128 + nt, :], x13[par][0:nt, :], reads=[x13[par]])
                P.barrier()

        NSLOT = 54
        SR = 384
        QT = SR // 128
        NROW = NSLOT * SR
        IO = bass.IndirectOffsetOnAxis
        w_gu2 = w_gu.rearrange("e r n -> (e r) n")
        w_dn2 = w_dn.rearrange("e r n -> (e r) n")

        regs = {}

        def _mkregs(h):
            for name, val in (("tok", NTOK + 127), ("w", NEXP * D - 1), ("row", NROW - 1)):
                r = h.alloc_register("bnd_" + name)
                h.reg_mov(r, val)
                regs[name] = r
        P.ops["pool"].append(_mkregs)

        def gather(sem, out, src, idx, reads, writes, bound):
            P.dmaf("pool", sem, lambda h: h.indirect_dma_start(out=out, out_offset=None, in_=src, in_offset=IO(ap=idx, axis=0),
                                                             bounds_check=regs[bound], oob_is_err=False),
                   [b.r for b in reads], [b.r for b in writes])

        with contextlib.ExitStack() as s4:
          if KSTOP >= 4:
            comb = sbuf(s4, "comb", [128, 17, 32]); msel = sbuf(s4, "msel", [128, 17, 32])
            pos4i = sbuf(s4, "pos4i", [128, 17 * 4], I32); tokid = sbuf(s4, "tokid", [128, 17], I32)
            onehot = sbuf(s4, "onehot", [128, NSLOT, 32])
            idxw = sbuf(s4, "idxw", [128, NSLOT * 8], I32); idxall = sbuf(s4, "idxall", [128, NSLOT * QT], I32)
            bguT = sbuf(s4, "bguT", [128, NEXP, 16])
            mc = sbuf(s4, "mc", [128, 111])
            siota = mc[:, 0:54]; eiota = mc[:, 54:86]; wrow = mc[:, 86:94]
            dma("sp", "c", mc[:], mconst_d[:, :], writes=[mc])
            dma("sp", "c", bguT[:], bguT_d[:, :, :], writes=[bguT])
            ts("dve", bguT[:, :, 8:16], bguT[:, :, 8:16], 1.0, None, ALU.add, None, [bguT], [bguT])
            cp("dve", tokid[:, :], mc[:, 94:111], [mc], [tokid])

            with contextlib.ExitStack() as s4a:
                g2c = sbuf(s4a, "g2c", [128, 8]); g2B = sbuf(s4a, "g2B", [128, D]); wr32 = sbuf(s4a, "wr32", [128, 8, 32])
                brB = sbuf(s4a, "brB", [128, 32]); bdn32 = sbuf(s4a, "bdn32", [32, D])
                triu = sbuf(s4a, "triu", [128, 128]); ones_f = sbuf(s4a, "ones_f", [128, 128])
                zb = sbuf(s4a, "zb", [128, D], BF16); zf = sbuf(s4a, "zf", [128, 32]); fillf = sbuf(s4a, "fillf", [128, NSLOT * QT])
                filli = sbuf(s4a, "filli", [128, NSLOT * QT], I32)
                dma("sp", "c", g2c[:], norm2_g[:, :], writes=[g2c])
                dma("sp", "c", g2B[:], norm2_row.partition_broadcast(128), writes=[g2B])
                dma("sp", "c", wr32[:], w_router.rearrange("(c p) n -> p c n", p=128), writes=[wr32])
                dma("sp", "c", brB[:], b_router.partition_broadcast(128), writes=[brB])
                dma("sp", "c", bdn32[:], b_dn[:, :], writes=[bdn32])
                dma("sp", "c", triu[:], triu_d[:, :], writes=[triu])
                P.op("pool", lambda h: h.memset(ones_f[:], 1.0), [], [ones_f.r])
                P.op("pool", lambda h: h.memset(zb[:], 0.0), [], [zb.r])
                P.op("pool", lambda h: h.memset(zf[:], 0.0), [], [zf.r])
                P.op("pool", lambda h: h.memset(fillf[:], float(NTOK)), [], [fillf.r])
                cp("dve", filli[:, :], fillf[:, :], [fillf], [filli])
                dma("sp", "o0", hn_d[NTOK:NTOK + 128, :], zb[:, :], reads=[zb])
                dma("sp", "o0", comb_d[NTOK:NTOK + 128, :], zf[:, :], reads=[zf])
                Rinv = Res("inv_d")
                P.dma("pool", "g", inv_d.rearrange("(p c) o -> p (c o)", p=128), filli[:, :], [filli.r], [Rinv])

                sm = lambda name, shape, dt=F32: sbuf(s4a, name, shape, dt)

                class TS:
                    pass
                tsets = []
                for gi in range(2):
                    T = TS(); sfx = f"_{gi}"
                    T.junk = sbuf(s4a, "junk4" + sfx, [128, D], BF16); T.hnb = sbuf(s4a, "hnb" + sfx, [128, D], BF16)
                    T.ssq = sbuf(s4a, "ssq4" + sfx, [128, 1]); T.std = sbuf(s4a, "std4" + sfx, [128, 1]); T.rstd = sbuf(s4a, "rstd4" + sfx, [128, 1])
                    T.hn32 = sbuf(s4a, "hn32" + sfx, [128, D]); T.hT32 = sbuf(s4a, "hT32" + sfx, [128, 8, 128])
                    T.lg = sbuf(s4a, "lg" + sfx, [128, 32]); T.m8 = sbuf(s4a, "m8" + sfx, [128, 8]); T.nmx = sbuf(s4a, "nmx" + sfx, [128, 1])
                    T.ex = sbuf(s4a, "ex" + sfx, [128, 32]); T.cw = sbuf(s4a, "cw" + sfx, [128, 32])
                    T.den = sbuf(s4a, "den" + sfx, [128, 1]); T.rden = sbuf(s4a, "rden" + sfx, [128, 1])
                    T.combT = sbuf(s4a, "combT" + sfx, [32, 128])
                    T.ps = [PS[4 * gi + i] for i in range(4)]
                    T.osem = f"o{gi}"
                    tsets.append(T)
                xts = [sbuf(s4a, f"x1t{i}", [128, D]) for i in range(4)]

                def load_x1(j):
                    nt = 128 if j < 16 else NS
                    dma("sp", "m", xts[j % 4][0:nt, :], x1scr[j * 128:j * 128 + nt, :], writes=[xts[j % 4]])

                def setup_tile(j, T):
                    nt = 128 if j < 16 else NS
                    r0 = j * 128
                    xt = xts[j % 4]
                    act(T.junk[0:nt, :], xt[0:nt, :], AF.Square, [xt], [T.junk, T.ssq], accum_out=T.ssq[0:nt, :])
                    yield
                    rstd_from_ssq(T.ssq, T.std, T.rstd, 1, D, nt)
                    yield
                    act(T.hn32[0:nt, :], xt[0:nt, :], AF.Copy, [xt, T.rstd], [T.hn32], scale=T.rstd[0:nt, :])
                    yield
                    tt("pool", T.hnb[0:nt, :], T.hn32[0:nt, :], g2B[0:nt, :], ALU.mult, [T.hn32, g2B], [T.hnb])
                    dma("sp", T.osem, hn_d[r0:r0 + nt, :], T.hnb[0:nt, :], reads=[T.hnb])
                    for c in range(8):
                        bank = T.ps[c // 4]
                        tr(bank[:, (c % 4) * 128:(c % 4) * 128 + nt], T.hn32[0:nt, c * 128:(c + 1) * 128], ident_f[0:nt, 0:nt], [T.hn32, ident_f], [bank])
                    yield
                    for hb in range(2):
                        bank = T.ps[hb]
                        bv = bank[:, :].rearrange("p (c t) -> p c t", c=4)[:, :, 0:nt]
                        gb = g2c[:, hb * 4:hb * 4 + 4].unsqueeze(2).to_broadcast([128, 4, nt])
                        tt("dve", T.hT32[:, hb * 4:hb * 4 + 4, 0:nt], bv, gb, ALU.mult, [bank, g2c], [T.hT32])
                    yield
                    for c in range(8):
                        mm(T.ps[2][0:nt, 0:32], T.hT32[:, c, 0:nt], wr32[:, c, :], c == 0, c == 7, [T.hT32, wr32], [T.ps[2]])
                    yield
                    tt("dve", T.lg[0:nt, :], T.ps[2][0:nt, 0:32], brB[0:nt, :], ALU.add, [T.ps[2], brB], [T.lg])
                    P.op("dve", lambda h: h.max(out=T.m8[0:nt, :], in_=T.lg[0:nt, :]), [T.lg.r], [T.m8.r])
                    ts("dve", T.nmx[0:nt, :], T.m8[0:nt, 0:1], -1.0, None, ALU.mult, None, [T.m8], [T.nmx])
                    yield
                    act(T.ex[0:nt, :], T.lg[0:nt, :], AF.Exp, [T.lg, T.nmx], [T.ex], bias=T.nmx[0:nt, :])
                    yield
                    stt(T.cw[0:nt, :], T.lg[0:nt, :], T.m8[0:nt, 3:4], T.ex[0:nt, :], ALU.is_ge, ALU.mult, [T.lg, T.m8, T.ex], [T.cw, T.den],
                        accum_out=T.den[0:nt, :])
                    recip(T.rden[0:nt, :], T.den[0:nt, :], [T.den], [T.rden])
                    ts("dve", comb[0:nt, j, :], T.cw[0:nt, :], T.rden[0:nt, :], None, ALU.mult, None, [T.cw, T.rden], [comb])
                    ts("dve", msel[0:nt, j, :], T.lg[0:nt, :], T.m8[0:nt, 3:4], None, ALU.is_ge, None, [T.lg, T.m8], [msel])
                    dma("sp", T.osem, comb_d[r0:r0 + nt, :], comb[0:nt, j, :], reads=[comb])
                    tr(T.ps[2][0:32, 128:128 + nt], comb[0:nt, j, :], ident_f[0:nt, 0:nt], [comb, ident_f], [T.ps[2]])
                    yield
                    cp("act", T.combT[:, 0:nt], T.ps[2][0:32, 128:128 + nt], [T.ps[2]], [T.combT])
                    yield
                    for hf in range(2):
                        bank = T.ps[3] if hf == 0 else T.ps[0]
                        mm(bank[0:nt, :], T.combT[:, 0:nt], bdn32[:, hf * 512:(hf + 1) * 512], True, True, [T.combT, bdn32], [bank])
                    yield
                    for hf in range(2):
                        bank = T.ps[3] if hf == 0 else T.ps[0]
                        tt("dve", xt[0:nt, hf * 512:(hf + 1) * 512], bank[0:nt, :], xt[0:nt, hf * 512:(hf + 1) * 512], ALU.add, [bank, xt], [xt])
                    dma("sp", T.osem, x1scr[r0:r0 + nt, :], xt[0:nt, :], reads=[xt])

                load_x1(0); load_x1(1)
                for j0 in range(0, 17, 2):
                    for j in (j0 + 2, j0 + 3):
                        if j < 17:
                            load_x1(j)
                    gens = [setup_tile(j, tsets[j - j0]) for j in (j0, j0 + 1) if j < 17]
                    while gens:
                        for g in list(gens):
                            try:
                                next(g)
                            except StopIteration:
                                gens.remove(g)

                cnt = sm("cnt", [128, 32]); nsl = [sm("nslA", [128, 32]), sm("nslB", [128, 32])]
                cum = [sm("cumA", [128, 32]), sm("cumB", [128, 32])]; st512 = sm("st512", [128, 32])
                tmp3 = sm("tmp3", [128, NSLOT, 32]); eall = sm("eall", [128, NSLOT]); idxwf = sm("idxwf", [128, NSLOT, 8])
                vt = [sm("vtA", [128, 32]), sm("vtB", [128, 32])]; p8 = sm("p8", [128, 8]); p4f = sm("p4f", [128, 4])
                ones_b = sm("ones_b", [128, 128], BF16); triu_b = sm("triu_b", [128, 128], BF16); msel_b = sm("msel_b", [128, 17, 32], BF16)
                cp("act", ones_b[:, :], ones_f[:, :], [ones_f], [ones_b])
                cp("act", triu_b[:, :], triu[:, :], [triu], [triu_b])
                cp("dve", msel_b[:, 0:16, :], msel[:, 0:16, :], [msel], [msel_b])
                cp("dve", msel_b[0:NS, 16, :], msel[0:NS, 16, :], [msel], [msel_b])
                for j in range(17):
                    nt = 128 if j < 16 else NS
                    mm(PS[0][:, 0:32], ones_b[0:nt, :], msel_b[0:nt, j, :], j == 0, j == 16, [ones_b, msel_b], [PS[0]])
                for j in range(17):
                    nt = 128 if j < 16 else NS
                    bank = PS[1] if j < 16 else PS[2]
                    dst = bank[0:nt, (j % 16) * 32:(j % 16) * 32 + 32]
                    for jp in range(j):
                        mm(dst, ones_b[:, 0:nt], msel_b[:, jp, :], jp == 0, False, [ones_b, msel_b], [bank])
                    mm(dst, triu_b[0:nt, 0:nt], msel_b[0:nt, j, :], j == 0, True, [triu_b, msel_b], [bank])
                cp("dve", cnt[:, :], PS[0][:, 0:32], [PS[0]], [cnt])
                ts("dve", nsl[0][:, :], cnt[:, :], 0.0, None, ALU.is_gt, None, [cnt], [nsl[0]])
                a = 0
                for k in range(1, 6):
                    stt(nsl[1 - a][:, :], cnt[:, :], float(SR) * k, nsl[a][:, :], ALU.is_gt, ALU.add, [cnt, nsl[a]], [nsl[1 - a]])
                    a = 1 - a
                nslots = nsl[a]
                cp("dve", cum[0][:, :], nslots[:, :], [nslots], [cum[0]])
                b_ = 0
                for sh in (1, 2, 4, 8, 16):
                    cp("dve", cum[1 - b_][:, 0:sh], cum[b_][:, 0:sh], [cum[b_]], [cum[1 - b_]])
                    tt("dve", cum[1 - b_][:, sh:32], cum[b_][:, sh:32], cum[b_][:, 0:32 - sh], ALU.add, [cum[b_]], [cum[1 - b_]])
                    b_ = 1 - b_
                cumi = cum[b_]
                tt("dve", st512[:, :], cumi[:, :], nslots[:, :], ALU.subtract, [cumi, nslots], [st512])
                ts("dve", st512[:, :], st512[:, :], float(SR), None, ALU.mult, None, [st512], [st512])
                tt("dve", tmp3[:, :, :], cumi[:, :].unsqueeze(1).to_broadcast([128, NSLOT, 32]),
                   siota.unsqueeze(2).to_broadcast([128, NSLOT, 32]), ALU.is_le, [cumi, mc], [tmp3])
                red(eall[:, :], tmp3[:, :, :], [tmp3], [eall])
                tt("dve", onehot[:, :, :], eiota.unsqueeze(1).to_broadcast([128, NSLOT, 32]),
                   eall[:, :].unsqueeze(2).to_broadcast([128, NSLOT, 32]), ALU.is_equal, [mc, eall], [onehot])
                stt(idxwf[:, :, :], eall[:, :].unsqueeze(2).to_broadcast([128, NSLOT, 8]), 1024.0,
                    wrow.unsqueeze(1).to_broadcast([128, NSLOT, 8]), ALU.mult, ALU.add, [eall, mc], [idxwf])
                cp("dve", idxw[:, :], idxwf[:, :, :].rearrange("p s c -> p (s c)"), [idxwf], [idxw])
                for j in range(17):
                    nt = 128 if j < 16 else NS
                    bank = PS[1] if j < 16 else PS[2]
                    rk = bank[0:nt, (j % 16) * 32:(j % 16) * 32 + 32]
                    stt(vt[0][0:nt, :], rk, 1.0, st512[0:nt, :], ALU.add, ALU.add, [bank, st512], [vt[0]])
                    tt("dve", vt[1][0:nt, :], vt[0][0:nt, :], msel[0:nt, j, :], ALU.mult, [vt[0], msel], [vt[1]])
                    P.op("dve", lambda h, nt=nt: h.max(out=p8[0:nt, :], in_=vt[1][0:nt, :]), [vt[1].r], [p8.r])
                    ts("dve", p4f[0:nt, :], p8[0:nt, 0:4], -1.0, None, ALU.add, None, [p8], [p4f])
                    cp("dve", pos4i[0:nt, j * 4:j * 4 + 4], p4f[0:nt, :], [p4f], [pos4i])
                for j in range(17):
                    nt = 128 if j < 16 else NS
                    for k in range(4):
                        P.dmaf("pool", "g", lambda h, nt=nt, j=j, k=k: h.indirect_dma_start(
                            out=inv_d[:, :], out_offset=IO(ap=pos4i[0:nt, j * 4 + k:j * 4 + k + 1], axis=0), in_=tokid[0:nt, j:j + 1], in_offset=None,
                            bounds_check=regs["row"], oob_is_err=False), [pos4i.r, tokid.r, Rinv], [])
                P.barrier()
                for q6 in range(9):
                    dma("sp", "m", idxall[:, q6 * 18:(q6 + 1) * 18],
                        inv_d[q6 * 2304:(q6 + 1) * 2304, :].rearrange("(s p) o -> p (s o)", p=128), writes=[idxall],
                        allow_slow_non_contiguous=True)
            P.barrier()

            with contextlib.ExitStack() as s4b:
                wgu = [sbuf(s4b, f"wgu{i}", [128, 8, 2048], BF16) for i in range(2)]
                wdn = [sbuf(s4b, f"wdn{i}", [128, 8, D], BF16) for i in range(2)]
                xg = [sbuf(s4b, f"xg{i}", [128, QT, D], BF16) for i in range(2)]
                hgT = [sbuf(s4b, f"hgT{i}", [128, 8, SR], BF16) for i in range(2)]
                gcomb = [sbuf(s4b, f"gcomb{i}", [128, QT, 32]) for i in range(2)]
                bias_s = [sbuf(s4b, f"bias_s{i}", [128, 16]) for i in range(2)]
                gate_s = [sbuf(s4b, f"gate_s{i}", [128, QT]) for i in range(2)]
                btmp = sbuf(s4b, "btmp", [128, 16, 32]); gtmp = sbuf(s4b, "gtmp", [128, QT, 32])
                gsb = [sbuf(s4b, f"gsb{i}", [128, SR]) for i in range(2)]
                ssb = [sbuf(s4b, f"ssb{i}", [128, SR]) for i in range(2)]
                usb = [sbuf(s4b, f"usb{i}", [128, SR]) for i in range(2)]
                actT = [sbuf(s4b, f"actT{i}", [128, 8, SR], BF16) for i in range(2)]
                res_sb = [sbuf(s4b, f"res_sb{i}", [128, D]) for i in range(3)]

                order = []
                for i in range(NSLOT // 3):
                    order += [i, 2 * (NSLOT // 3) + i]
                order += list(range(NSLOT // 3, 2 * (NSLOT // 3)))
                assert sorted(order) == list(range(NSLOT))

                def tok_loads(i):
                    s = order[i]
                    par = i % 2
                    for q in range(QT):
                        gather(f"kv{par}", xg[par][:, q, :], hn_d[:, :], idxall[:, s * QT + q:s * QT + q + 1], [idxall], [xg[par]], "tok")
                    for q in range(QT):
                        gather(f"kv{par}", gcomb[par][:, q, :], comb_d[:, :], idxall[:, s * QT + q:s * QT + q + 1], [idxall], [gcomb[par]], "tok")

                def wgu_loads(i):
                    s = order[i]
                    par = i % 2
                    for c in range(8):
                        gather(f"w{par}", wgu[par][:, c, :], w_gu2[:, :], idxw[:, s * 8 + c:s * 8 + c + 1], [idxw], [wgu[par]], "w")

                def wdn_loads(i):
                    s = order[i]
                    par = i % 2
                    for c in range(8):
                        gather(f"wd{par}", wdn[par][:, c, :], w_dn2[:, :], idxw[:, s * 8 + c:s * 8 + c + 1], [idxw], [wdn[par]], "w")

                def slot_transposes(i):
                    par = i % 2
                    for q in range(QT):
                        bank = PS[6 + q % 2]
                        bb = bank[:].bitcast(BF16)
                        for c in range(8):
                            tr(bb[:, c * 128:(c + 1) * 128], xg[par][:, q, c * 128:(c + 1) * 128], ident_b[:, :], [xg[par], ident_b], [bank])
                        cp("act" if q % 2 == 0 else "dve", hgT[par][:, :, q * 128:(q + 1) * 128], bb.rearrange("p (c t) -> p c t", c=8),
                           [bank], [hgT[par]])

                tok_loads(0); wgu_loads(0); wdn_loads(0)
                tok_loads(1) if NSLOT > 1 else None
                wgu_loads(1); wdn_loads(1)
                slot_transposes(0)
                for si in range(NSLOT):
                    s = order[si]
                    par = si % 2
                    if si >= 1 and si + 1 < NSLOT:
                        tok_loads(si + 1)
                    tt("dve", btmp[:, :, :], bguT[:, :, :].rearrange("p e c -> p c e"),
                       onehot[:, s, :].unsqueeze(1).to_broadcast([128, 16, 32]), ALU.mult, [bguT, onehot], [btmp])
                    red(bias_s[par][:, :], btmp[:, :, :], [btmp], [bias_s[par]])
                    tt("dve", gtmp[:, :, :], gcomb[par][:, :, :], onehot[:, s, :].unsqueeze(1).to_broadcast([128, QT, 32]), ALU.mult,
                       [gcomb[par], onehot], [gtmp])
                    red(gate_s[par][:, :], gtmp[:, :, :], [gtmp], [gate_s[par]])
                    aT = actT[par]
                    for c in range(8):
                        db = c % 2
                        bg = PS[db * 2]; bu = PS[db * 2 + 1]
                        for k in range(8):
                            mm(bg[:, 0:SR], wgu[par][:, k, c * 128:(c + 1) * 128], hgT[par][:, k, :], k == 0, k == 7, [wgu[par], hgT[par]], [bg])
                        for k in range(8):
                            mm(bu[:, 0:SR], wgu[par][:, k, 1024 + c * 128:1024 + (c + 1) * 128], hgT[par][:, k, :], k == 0, k == 7,
                               [wgu[par], hgT[par]], [bu])
                        ts("dve", gsb[db][:, :], bg[:, 0:SR], bias_s[par][:, c:c + 1], 7.0, ALU.add, ALU.min, [bg, bias_s[par]], [gsb[db]])
                        act(ssb[db][:, :], gsb[db][:, :], AF.Sigmoid, [gsb[db]], [ssb[db]], scale=1.702)
                        ts("dve", usb[db][:, :], bu[:, 0:SR], bias_s[par][:, 8 + c:9 + c], -6.0, ALU.add, ALU.max, [bu, bias_s[par]], [usb[db]])
                        stt(usb[db][:, :], usb[db][:, :], 8.0, gsb[db][:, :], ALU.min, ALU.mult, [usb[db], gsb[db]], [usb[db]])
                        tt("dve", aT[:, c, :], usb[db][:, :], ssb[db][:, :], ALU.mult, [usb[db], ssb[db]], [aT])
                    if si + 2 < NSLOT:
                        wgu_loads(si + 2)
                    if si + 1 < NSLOT:
                        slot_transposes(si + 1)
                    for q in range(QT):
                        ri = (si * QT + q) % 3
                        rs = res_sb[ri]
                        for hf in range(2):
                            bank = PS[4 + hf]
                            for k in range(8):
                                mm(bank[:, :], aT[:, k, q * 128:(q + 1) * 128], wdn[par][:, k, hf * 512:(hf + 1) * 512], k == 0, k == 7,
                                   [aT, wdn[par]], [bank])
                            if hf == 0:
                                act(rs[:, 0:512], bank[:, :], AF.Copy, [bank, gate_s[par]], [rs], scale=gate_s[par][:, q:q + 1])
                            else:
                                ts("dve", rs[:, 512:1024], bank[:, :], gate_s[par][:, q:q + 1], None, ALU.mult, None, [bank, gate_s[par]], [rs])
                        dma("sp", f"r{ri}", res_d[(s * QT + q) * 128:(s * QT + q + 1) * 128, :], rs[:, :], reads=[rs])
                    if si + 2 < NSLOT:
                        wdn_loads(si + 2)
            P.barrier()

            with contextlib.ExitStack() as s4c:
                NCB = 4
                yb = [sbuf(s4c, f"yb{i}", [128, D]) for i in range(NCB)]
                rg = [[sbuf(s4c, f"rg{i}_{k}", [128, D]) for k in range(4)] for i in range(NCB)]
                csem = ("kv0", "kv1", "w0", "w1")
                xsem = ("x0", "x1", "o0", "o1")

                def comb_loads(j):
                    nt = 128 if j < 16 else NS
                    par = j % NCB
                    dma("sp", xsem[par], yb[par][0:nt, :], x1scr[j * 128:j * 128 + nt, :], writes=[yb[par]])
                    for k in range(4):
                        gather(csem[par], rg[par][k][0:nt, :], res_d[:, :], pos4i[0:nt, j * 4 + k:j * 4 + k + 1], [pos4i], [rg[par][k]], "row")
                for j in range(NCB - 1):
                    comb_loads(j)
                for j in range(17):
                    nt = 128 if j < 16 else NS
                    par = j % NCB
                    if j + NCB - 1 < 17:
                        comb_loads(j + NCB - 1)
                    for k in range(4):
                        tt("dve", yb[par][0:nt, :], yb[par][0:nt, :], rg[par][k][0:nt, :], ALU.add, [yb[par], rg[par][k]], [yb[par]])
                    if j < 16:
                        dma("sp", "y", y_p[j * 128:(j + 1) * 128, :], yb[par][:, :], reads=[yb[par]], is_output=True)
                    else:
                        dma("sp", "y", y_s[:, :], yb[par][0:NS, :], reads=[yb[par]], is_output=True)
        P.finish("sp")
        with block_cm as block:
            P.emit(block)
    return nc


def _host_constants():
    k = np.arange(128)[:, None]
    q = np.arange(128)[None, :]
    cur = (k <= q).astype(np.float32)
    prev = (k >= q).astype(np.float32)
    maskA = np.concatenate([prev, cur, prev, cur], axis=1)
    maskH0 = np.concatenate([0 * prev, cur, 0 * prev, cur], axis=1)
    mS = np.zeros((128, 2, 9, 3, 4), np.float32)
    for t9 in range(8):
        mS[:, :, t9, :, t9 % 4] = 1.0
    for t in range(4):
        mS[t:, :, 8, :, t] = 1.0
    maskS = mS.reshape(128, 216)
    w4 = np.zeros((4, 4), np.float32)
    for tk in range(4):
        for t in range(4):
            w4[tk, t] = (1.0 if tk <= t else 0.0) + (2.0 if tk == t else 0.0)
    mN = np.kron(np.eye(16, dtype=np.float32), w4)
    maskN = np.tile(mN, (1, 6))
    tril4 = np.zeros((4, 4), np.float32)
    for s in range(4):
        for t in range(4):
            tril4[s, t] = 1.0 if s <= t else 0.0
    mbd = np.kron(np.eye(16, dtype=np.float32), tril4)
    mask_bd = np.repeat(mbd[:, None, :], 6, axis=1)
    ident = np.eye(128, dtype=np.float32)
    onesAB = np.concatenate([np.ones((128, 64)), np.zeros((128, 64)), np.ones((128, 64))], axis=1).astype(np.float32)
    return dict(maskA=maskA, maskH0=maskH0, maskS=maskS, maskN=maskN, mask_bd=mask_bd, ident=ident, onesAB=onesAB)


def _rope_tables(pos):
    half = 8
    inv_freq = np.power(np.float32(500000.0), -np.arange(half, dtype=np.float32) / np.float32(half)).astype(np.float32)
    ang = pos.astype(np.float32)[:, None] * inv_freq[None, :]
    c = np.cos(ang).astype(np.float32)
    s = np.sin(ang).astype(np.float32)
    return np.concatenate([c, c], axis=1), np.concatenate([-s, s], axis=1)


_NC_CACHE = {}


def kernel(x_prompt, x_sample, mem_prompt, cache_win_k, cache_win_v, cache_mem_k, cache_mem_v,
           norm1_g, w_in, gv_a, w_s, b_s, gq_b, gk_b, gq_m, gk_m, mem_norm_g, w_mem_kv, w_out,
           norm2_g, w_router, b_router, w_gate_up, b_gate_up, w_down, b_down):
    f = lambda a: np.ascontiguousarray(np.asarray(a, dtype=np.float32))
    x_prompt, x_sample, mem_prompt = f(x_prompt), f(x_sample), f(mem_prompt)
    cwk, cwv = f(cache_win_k)[0], f(cache_win_v)[0]
    cmk, cmv = f(cache_mem_k)[0], f(cache_mem_v)[0]
    C = _host_constants()
    gT = lambda g: f(np.asarray(g)[0].reshape(8, 128).T)
    w_s0 = np.asarray(w_s, np.float32)[0]
    b_s0 = np.asarray(b_s, np.float32)[0]
    shared = {
        "norm1_gT": gT(norm1_g), "norm2_gT": gT(norm2_g), "memn_gT": gT(mem_norm_g),
        "w_in": f(w_in)[0], "w_mem_kv": f(w_mem_kv)[0], "w_out": f(w_out)[0],
        "gv_a": f(gv_a), "gqk": f(np.concatenate([np.tile(np.asarray(gq_b)[0], 6), np.tile(np.asarray(gk_b)[0], 6)])[None]),
        "gqm": f(np.tile(np.asarray(gq_m)[0], 4)[None]), "gkm": f(np.tile(np.asarray(gk_m)[0], 4)[None]),
        "w_sT": f(w_s0.transpose(2, 0, 1)),
        "bsB": f(np.repeat(b_s0.T[:, :, None], 64, axis=2).reshape(128, 384)),
        "maskA": C["maskA"], "maskS": C["maskS"], "maskN": C["maskN"], "mask_bd": C["mask_bd"],
        "ident": C["ident"], "onesAB": C["onesAB"],
        "w_router": f(w_router)[0], "b_router": f(b_router),
        "w_gate_up": f(w_gate_up)[0], "w_down": f(w_down)[0],
        "bguT": f(np.asarray(b_gate_up, np.float32)[0].reshape(NEXP, 16, 128).transpose(2, 0, 1)),
        "b_down": f(b_down)[0],
        "norm2_row": f(norm2_g),
    }
    pp = np.arange(128, dtype=np.float32)[:, None]
    shared["mconst"] = f(np.concatenate([np.tile(np.arange(54, dtype=np.float32)[None], (128, 1)),
                                         np.tile(np.arange(32, dtype=np.float32)[None], (128, 1)),
                                         np.arange(8, dtype=np.float32)[None] * 128 + pp,
                                         np.arange(17, dtype=np.float32)[None] * 128 + pp], axis=1))
    shared["triu"] = f(np.triu(np.ones((128, 128), np.float32), 1))
    bd = np.zeros((64, 6, 64), np.float32)
    for b in range(16):
        bd[4 * b:4 * b + 4, :, 4 * b:4 * b + 4] = w_s0[:, :4, :4].transpose(2, 0, 1)
    shared["bd_ws"] = bd
    shared["bsS"] = f(np.tile(np.repeat(b_s0[:, :4].T[:, :, None], 64, axis=2).reshape(4, 384), (16, 1)))
    rows = np.zeros((9, 128), np.int64)
    for t in range(4):
        rows[t] = t + 16 * np.arange(128)
        rows[4 + t] = 1536 + t + 4 * np.arange(128)
    rows[8] = 1920 + np.arange(128)

    in_maps = []
    for c in range(NCORES):
        b, g = c // 4, c % 4
        s0 = g * SEG
        m = dict(shared)
        m["xp"] = f(x_prompt[b, s0:s0 + SEG])
        m["xh"] = f(x_prompt[b, s0 - SEG:s0]) if g > 0 else np.zeros((SEG, D), np.float32)
        m["xs"] = f(x_sample[16 * c:16 * c + 16].reshape(NS, D))
        m["mem"] = f(mem_prompt[b])
        kk = cwk[16 * c:16 * c + 16][:, rows]
        m["cwkT"] = f(kk.reshape(16, 9, 128, 3, 128).transpose(0, 4, 3, 1, 2).reshape(16, 128, 3, 1152))
        vv = cwv[16 * c:16 * c + 16][:, rows]
        m["cwv"] = f(vv.transpose(0, 2, 1, 3, 4).reshape(16, 128, 9, 384))
        mk = cmk[16 * c:16 * c + 16]
        m["cmkT"] = f(mk.reshape(16, 256, 2, 128).transpose(3, 0, 2, 1))
        mv = cmv[16 * c:16 * c + 16]
        m["cmv"] = f(mv.reshape(16, 2, 128, 256).transpose(2, 0, 1, 3))
        pos = np.concatenate([s0 + np.arange(SEG), s0 - SEG + np.arange(SEG), 8192 + (np.arange(NS) % 4)])
        cs, sn = _rope_tables(pos)
        m["rope_cs"], m["rope_sn"] = cs, sn
        m["maskH"] = C["maskA"] if g > 0 else C["maskH0"]
        in_maps.append(m)

    if "nc" not in _NC_CACHE:
        _NC_CACHE["nc"] = build_program()
    nc = _NC_CACHE["nc"]
    res = run_bass_kernel_spmd(nc, in_maps, core_ids=list(range(NCORES)))
    R = res.results
    y_prompt = np.stack([np.concatenate([R[4 * b + g]["y_p"] for g in range(4)], axis=0) for b in range(2)])
    y_sample = np.concatenate([R[c]["y_s"].reshape(16, 4, D) for c in range(NCORES)], axis=0)
    wk = np.stack([R[4 * b + 3]["wk_p"] for b in range(2)]).reshape(1, 2, SEG, 6, 64)
    wv = np.stack([R[4 * b + 3]["wv_p"] for b in range(2)]).reshape(1, 2, SEG, 6, 64)
    mk = np.stack([R[4 * b]["mk_p"] for b in range(2)]).reshape(1, 2, 256, 4, 64)
    mv = np.stack([R[4 * b]["mv_p"] for b in range(2)]).reshape(1, 2, 256, 4, 64)
    wks = np.concatenate([R[c]["wk_s"].reshape(16, 4, 6, 64) for c in range(NCORES)], axis=0)[None]
    wvs = np.concatenate([R[c]["wv_s"].reshape(16, 4, 6, 64) for c in range(NCORES)], axis=0)[None]
    gvs = np.concatenate([R[c]["gv_s"].reshape(16, 4, 384) for c in range(NCORES)], axis=0)[None]
    return (y_prompt.astype(np.float32), y_sample.astype(np.float32), wk.astype(np.float32), wv.astype(np.float32),
            mk.astype(np.float32), mv.astype(np.float32), wks.astype(np.float32), wvs.astype(np.float32), gvs.astype(np.float32))
```

```python
import contextlib
import numpy as np
import concourse.bass as bass
import concourse.mybir as mybir
from concourse.bass_utils import run_bass_kernel_spmd

F32 = mybir.dt.float32
BF16 = mybir.dt.bfloat16
I32 = mybir.dt.int32
AF = mybir.ActivationFunctionType
ALU = mybir.AluOpType
AX = mybir.AxisListType

NCORES = 8
D = 1024
SEG = 2048
NS = 64
NTOK = SEG + NS
NEXP = 32
EPS = 1e-6
SCALE = 0.125
KSTOP = 9


class Res:
    __slots__ = ("name", "w", "r", "excl")

    def __init__(self, name="", excl=False):
        self.name = name
        self.w = None
        self.r = []
        self.excl = excl


class Prog:
    ENGS = ("pe", "act", "dve", "pool", "sp")

    def __init__(self, nc):
        self.nc = nc
        self.ops = {e: [] for e in self.ENGS}
        self.cnt = {e: 0 for e in self.ENGS}
        self.sems = {}
        self.semval = {}
        self.known = {e: {} for e in self.ENGS}
        self.sealed = {}
        self.out_tokens = []

    def add_sem(self, key, handle):
        self.sems[key] = handle
        self.semval[key] = 0

    def _need(self, eng, tok, same_ok):
        if tok is None:
            return
        key, val = tok
        if key in self.cnt:
            if key == eng and same_ok:
                return
        else:
            if self.known[eng].get(key, 0) >= val:
                return
            val = max(val, self.semval[key])
            self.sealed[key] = True
        if self.known[eng].get(key, 0) >= val:
            return
        self.known[eng][key] = val
        sem = self.sems[key]
        self.ops[eng].append(lambda h, sem=sem, val=val: h.wait_ge(sem, val))

    def _deps(self, eng, reads, writes, is_dma=False, semkey=None):
        same_ok = (not is_dma) and eng != "pool"
        need = {}

        def add(tok, ok):
            if tok is None:
                return
            key, val = tok
            if ok and key == eng:
                return
            if need.get(key, 0) < val:
                need[key] = val
        for r in reads:
            add(r.w, False)
        for w in writes:
            if not (is_dma and w.w is not None and w.w[0] == semkey):
                add(w.w, same_ok)
            for t in w.r:
                add(t, same_ok)
        for key, val in need.items():
            self._need(eng, (key, val), same_ok=False)

    def _commit(self, tok, reads, writes):
        for r in reads:
            r.r.append(tok)
        for w in writes:
            w.w = tok
            w.r = []

    @staticmethod
    def _split(reads, writes):
        writes = list(writes) + [r for r in reads if r.excl and r not in writes]
        reads = [r for r in reads if not r.excl]
        return reads, writes

    def op(self, eng, fn, reads=(), writes=()):
        reads, writes = self._split(reads, writes)
        self._deps(eng, reads, writes)
        self.cnt[eng] += 1
        tok = (eng, self.cnt[eng])
        sem = self.sems[eng]
        self.ops[eng].append(lambda h, fn=fn, sem=sem: fn(h).then_inc(sem, 1))
        self._commit(tok, reads, writes)
        return tok

    def dma(self, eng, semkey, out, in_, reads=(), writes=(), is_output=False, **kw):
        self._deps(eng, reads, writes, is_dma=True, semkey=semkey)
        if self.sealed.get(semkey) and self.semval[semkey] > 0:
            self._need(eng, (semkey, self.semval[semkey]), same_ok=False)
        self.sealed[semkey] = False
        self.semval[semkey] += 16
        tok = (semkey, self.semval[semkey])
        sem = self.sems[semkey]
        self.ops[eng].append(
            lambda h, out=out, in_=in_, sem=sem, kw=kw: h.dma_start(out=out, in_=in_, **kw).then_inc(sem, 16))
        self._commit(tok, reads, writes)
        if is_output:
            self.out_tokens.append(tok)
        return tok

    def dmaf(self, eng, semkey, fn, reads=(), writes=(), is_output=False):
        self._deps(eng, reads, writes, is_dma=True, semkey=semkey)
        if self.sealed.get(semkey) and self.semval[semkey] > 0:
            self._need(eng, (semkey, self.semval[semkey]), same_ok=False)
        self.sealed[semkey] = False
        self.semval[semkey] += 16
        tok = (semkey, self.semval[semkey])
        sem = self.sems[semkey]
        self.ops[eng].append(lambda h, fn=fn, sem=sem: fn(h).then_inc(sem, 16))
        self._commit(tok, reads, writes)
        if is_output:
            self.out_tokens.append(tok)
        return tok

    def barrier(self):
        for e in self.ENGS:
            for f in self.ENGS:
                if (f != e or e == "pool") and self.cnt[f] > 0:
                    self._need(e, (f, self.cnt[f]), same_ok=False)
            for k, v in self.semval.items():
                if v > 0:
                    self._need(e, (k, v), same_ok=False)

    def finish(self, eng="sp"):
        last = {}
        for key, val in self.out_tokens:
            last[key] = max(last.get(key, 0), val)
        for key, val in last.items():
            self._need(eng, (key, val), same_ok=False)

    def emit(self, block):
        hmap = {"pe": "tensor", "act": "scalar", "dve": "vector", "pool": "gpsimd", "sp": "sync"}
        for e in self.ENGS:
            ops = self.ops[e]
            if not ops:
                continue

            def body(h, ops=ops):
                for f in ops:
                    f(h)
            getattr(block, hmap[e])(body)


class B:
    def __init__(self, t, name, excl=False):
        self.t = t
        self.r = Res(name, excl)

    def __getitem__(self, k):
        return self.t[k]


def build_program():
    nc = bass.Bass("TRN2", target_bir_lowering=False)

    def din(name, shape):
        return nc.dram_tensor(name, list(shape), F32, kind="ExternalInput").ap()

    def dout(name, shape):
        return nc.dram_tensor(name, list(shape), F32, kind="ExternalOutput").ap()

    xp = din("xp", [SEG, D]); xh = din("xh", [SEG, D]); xs_d = din("xs", [NS, D]); mem_d = din("mem", [256, D])
    cwkT = din("cwkT", [16, 128, 3, 1152]); cwv = din("cwv", [16, 128, 9, 384])
    cmkT = din("cmkT", [128, 16, 2, 256]); cmv = din("cmv", [128, 16, 2, 256])
    norm1_g = din("norm1_gT", [128, 8]); norm2_g = din("norm2_gT", [128, 8]); memn_g = din("memn_gT", [128, 8])
    w_in = din("w_in", [D, 2176]); w_mem = din("w_mem_kv", [D, 512]); w_out = din("w_out", [D, D])
    gv_a = din("gv_a", [1, 384]); gqk = din("gqk", [1, 768]); gqm = din("gqm", [1, 256]); gkm = din("gkm", [1, 256])
    w_sT = din("w_sT", [128, 6, 128]); bsB_d = din("bsB", [128, 384])
    bd_ws = din("bd_ws", [64, 6, 64]); bsS_d = din("bsS", [64, 384]); mask_bd = din("mask_bd", [64, 6, 64])
    rope_cs = din("rope_cs", [2 * SEG + NS, 16]); rope_sn = din("rope_sn", [2 * SEG + NS, 16])
    maskA_d = din("maskA", [128, 512]); maskH_d = din("maskH", [128, 512])
    maskS_d = din("maskS", [128, 216]); maskN_d = din("maskN", [64, 384])
    ident_d = din("ident", [128, 128]); onesAB_d = din("onesAB", [128, 192])
    w_router = din("w_router", [D, 32]); b_router = din("b_router", [1, 32])
    w_gu = din("w_gate_up", [NEXP, D, 2048]); w_dn = din("w_down", [NEXP, D, D])
    bguT_d = din("bguT", [128, NEXP, 16]); b_dn = din("b_down", [NEXP, D])

    mconst_d = din("mconst", [128, 111]); triu_d = din("triu", [128, 128]); norm2_row = din("norm2_row", [1, D])

    vscr = nc.dram_tensor("vscr", [2 * SEG, 384], F32, kind="Internal").ap()
    x1scr = nc.dram_tensor("x1scr", [NTOK, D], F32, kind="Internal").ap()
    hn_d = nc.dram_tensor("hn_d", [NTOK + 128, D], BF16, kind="Internal").ap()
    comb_d = nc.dram_tensor("comb_d", [NTOK + 128, 32], F32, kind="Internal").ap()
    inv_d = nc.dram_tensor("inv_d", [54 * 384, 1], I32, kind="Internal").ap()
    res_d = nc.dram_tensor("res_d", [54 * 384, D], F32, kind="Internal").ap()

    y_p = dout("y_p", [SEG, D]); y_s = dout("y_s", [NS, D])
    wk_p = dout("wk_p", [SEG, 384]); wv_p = dout("wv_p", [SEG, 384])
    mk_p = dout("mk_p", [256, 256]); mv_p = dout("mv_p", [256, 256])
    wk_s = dout("wk_s", [NS, 384]); wv_s = dout("wv_s", [NS, 384]); gv_s = dout("gv_s", [NS, 384])

    with contextlib.ExitStack() as top:
        P = Prog(nc)
        for e in Prog.ENGS:
            P.add_sem(e, top.enter_context(nc.semaphore("s_" + e)))
        for k in ("c", "cp", "x0", "x1", "o0", "o1", "v", "kv0", "kv1", "w0", "w1", "wd0", "wd1", "g", "r0", "r1", "r2", "m", "y"):
            P.add_sem(k, top.enter_context(nc.semaphore("d_" + k)))
        block_cm = nc.Block()

        def sbuf(es, name, shape, dt=F32):
            return B(es.enter_context(nc.sbuf_tensor("sb_" + name, list(shape), dt)), name)

        PS = [B(top.enter_context(nc.psum_tensor(f"ps{i}", [128, 512], F32)), f"ps{i}", excl=True) for i in range(8)]

        def act(out, in_, func, reads, writes, **kw):
            P.op("act", lambda h: h.activation(out=out, in_=in_, func=func, **kw),
                 [b.r for b in reads], [b.r for b in writes])

        def tt(eng, out, in0, in1, op, reads, writes):
            P.op(eng, lambda h: h.tensor_tensor(out=out, in0=in0, in1=in1, op=op),
                 [b.r for b in reads], [b.r for b in writes])

        def ts(eng, out, in0, s1, s2, op0, op1, reads, writes, **kw):
            if op1 is None:
                P.op(eng, lambda h: h.tensor_scalar(out=out, in0=in0, scalar1=s1, scalar2=None, op0=op0, **kw),
                     [b.r for b in reads], [b.r for b in writes])
            else:
                P.op(eng, lambda h: h.tensor_scalar(out=out, in0=in0, scalar1=s1, scalar2=s2, op0=op0, op1=op1, **kw),
                     [b.r for b in reads], [b.r for b in writes])

        def stt(out, in0, scalar, in1, op0, op1, reads, writes, **kw):
            P.op("dve", lambda h: h.scalar_tensor_tensor(out=out, in0=in0, scalar=scalar, in1=in1, op0=op0, op1=op1, **kw),
                 [b.r for b in reads], [b.r for b in writes])

        def cp(eng, out, in_, reads, writes):
            if eng == "act":
                act(out, in_, AF.Copy, reads, writes)
            else:
                P.op(eng, lambda h: h.tensor_copy(out, in_), [b.r for b in reads], [b.r for b in writes])

        def mm(out, lhsT, rhs, start, stop, reads, writes):
            P.op("pe", lambda h: h.matmul(out, lhsT=lhsT, rhs=rhs, start=start, stop=stop),
                 [b.r for b in reads], [b.r for b in writes])

        def tr(out, in_, ident, reads, writes):
            P.op("pe", lambda h: h.transpose(out=out, in_=in_, identity=ident),
                 [b.r for b in reads], [b.r for b in writes])

        def recip(out, in_, reads, writes):
            P.op("dve", lambda h: h.reciprocal(out, in_), [b.r for b in reads], [b.r for b in writes])

        def red(out, in_, reads, writes):
            P.op("dve", lambda h: h.tensor_reduce(out=out, in_=in_, axis=AX.X, op=ALU.add),
                 [b.r for b in reads], [b.r for b in writes])

        def dma(eng, sem, out, in_, reads=(), writes=(), **kw):
            P.dma(eng, sem, out, in_, [b.r for b in reads], [b.r for b in writes], **kw)

        def rstd_from_ssq(ssq, std, rstd, n, width, rows):
            act(std[0:rows, 0:n], ssq[0:rows, 0:n], AF.Sqrt, [ssq], [std], scale=1.0 / width, bias=EPS)
            recip(rstd[0:rows, 0:n], std[0:rows, 0:n], [std], [rstd])

        ident_f = sbuf(top, "ident_f", [128, 128]); ident_b = sbuf(top, "ident_b", [128, 128], BF16)
        dma("sp", "c", ident_f[:], ident_d[:, :], writes=[ident_f])
        cp("dve", ident_b[:], ident_f[:], [ident_f], [ident_b])

        with contextlib.ExitStack() as sA:
            oaT = sbuf(sA, "oaT", [128, 3, NTOK], BF16)
            mixB = sbuf(sA, "mixB", [128, 3, NTOK], BF16)
            mixM = sbuf(sA, "mixM", [128, 2, NTOK], BF16)
            with contextlib.ExitStack() as sB:
                qT = sbuf(sB, "qT", [128, 3, SEG], BF16)
                kT = sbuf(sB, "kT", [128, 3, 2 * SEG], BF16)
                qmT = sbuf(sB, "qmT", [128, 2, NTOK], BF16)
                kmT = sbuf(sB, "kmT", [128, 2, 256], BF16)
                vmp = sbuf(sB, "vmp", [128, 2, 2, 192], BF16)
                qTs = sbuf(sB, "qTs", [128, 3, NS], BF16)
                kTs = sbuf(sB, "kTs", [128, 3, NS], BF16)
                vsp = sbuf(sB, "vsp", [64, 3, 192], BF16)
                onesAB = sbuf(sB, "onesAB", [128, 192], BF16)
                dma("pool", "cp", onesAB[:], onesAB_d[:, :], writes=[onesAB])
                P.op("pool", lambda h: h.memset(vmp[:], 0.0), [], [vmp.r])
                P.op("pool", lambda h: h.memset(vsp[:], 0.0), [], [vsp.r])

                with contextlib.ExitStack() as s1:
                    w_in_b = sbuf(s1, "w_in_b", [128, 8, 2176], BF16)
                    w_mem_b = sbuf(s1, "w_mem_b", [128, 8, 512], BF16)
                    for hh in range(2):
                        dma("pool", "cp", w_in_b[:, :, hh * 1088:(hh + 1) * 1088],
                            w_in.rearrange("(c p) n -> p c n", p=128)[:, :, hh * 1088:(hh + 1) * 1088], writes=[w_in_b])
                    dma("pool", "cp", w_mem_b[:], w_mem.rearrange("(c p) n -> p c n", p=128), writes=[w_mem_b])
                    wsT_f = sbuf(s1, "wsT_f", [128, 6, 128]); wsT_b = sbuf(s1, "wsT_b", [128, 6, 128], BF16)
                    mcur_f = sbuf(s1, "mcur_f", [128, 128])
                    dma("sp", "c", wsT_f[:], w_sT[:, :, :], writes=[wsT_f])
                    dma("sp", "c", mcur_f[:], maskA_d[:, 384:512], writes=[mcur_f])
                    tt("dve", wsT_b[:], wsT_f[:], mcur_f[:].unsqueeze(1).to_broadcast([128, 6, 128]), ALU.mult,
                       [wsT_f, mcur_f], [wsT_b])
                    bd_f = sbuf(s1, "bd_f", [64, 6, 64]); mbd_f = sbuf(s1, "mbd_f", [64, 6, 64]); bd_b = sbuf(s1, "bd_b", [64, 6, 64], BF16)
                    dma("sp", "c", bd_f[:], bd_ws[:, :, :], writes=[bd_f])
                    dma("sp", "c", mbd_f[:], mask_bd[:, :, :], writes=[mbd_f])
                    tt("dve", bd_b[:], bd_f[:], mbd_f[:], ALU.mult, [bd_f, mbd_f], [bd_b])
                    bsB = sbuf(s1, "bsB", [128, 384]); bsS = sbuf(s1, "bsS", [64, 384])
                    dma("sp", "c", bsB[:], bsB_d[:, :], writes=[bsB])
                    dma("sp", "c", bsS[:], bsS_d[:, :], writes=[bsS])
                    g1c = sbuf(s1, "g1c", [128, 8]); gmc = sbuf(s1, "gmc", [128, 8])
                    dma("sp", "c", g1c[:], norm1_g[:, :], writes=[g1c])
                    dma("sp", "c", gmc[:], memn_g[:, :], writes=[gmc])
                    gvB = sbuf(s1, "gvB", [128, 384]); gqkB = sbuf(s1, "gqkB", [128, 768])
                    gqmB = sbuf(s1, "gqmB", [128, 256]); gkmB = sbuf(s1, "gkmB", [128, 256])
                    dma("sp", "c", gvB[:], gv_a.partition_broadcast(128), writes=[gvB])
                    dma("sp", "c", gqkB[:], gqk.partition_broadcast(128), writes=[gqkB])
                    dma("sp", "c", gqmB[:], gqm.partition_broadcast(128), writes=[gqmB])
                    dma("sp", "c", gkmB[:], gkm.partition_broadcast(128), writes=[gkmB])

                    class WS:
                        pass
                    wsets = []
                    for par in range(2):
                        w = WS()
                        sfx = f"_{par}"
                        w.xt = sbuf(s1, "xt" + sfx, [128, D]); w.junk = sbuf(s1, "junk" + sfx, [128, D], BF16)
                        w.ssq = sbuf(s1, "ssq" + sfx, [128, 1]); w.std = sbuf(s1, "std" + sfx, [128, 1]); w.rstd = sbuf(s1, "rstd" + sfx, [128, 1])
                        w.xs = sbuf(s1, "xs" + sfx, [128, D], BF16); w.hT = sbuf(s1, "hT" + sfx, [128, 8, 128], BF16)
                        w.u32 = sbuf(s1, "u32" + sfx, [128, 384])
                        w.ssqv = sbuf(s1, "ssqv" + sfx, [128, 1]); w.stdv = sbuf(s1, "stdv" + sfx, [128, 1]); w.rstdv = sbuf(s1, "rstdv" + sfx, [128, 1])
                        w.vn32 = sbuf(s1, "vn32" + sfx, [128, 384]); w.vnb = sbuf(s1, "vnb" + sfx, [128, 384], BF16)
                        w.sqt = sbuf(s1, "sqt" + sfx, [128, 768])
                        w.ssqk = sbuf(s1, "ssqk" + sfx, [128, 12]); w.stdk = sbuf(s1, "stdk" + sfx, [128, 12]); w.rstdk = sbuf(s1, "rstdk" + sfx, [128, 12])
                        w.qk32 = sbuf(s1, "qk32" + sfx, [128, 12, 64])
                        w.rt1 = sbuf(s1, "rt1" + sfx, [128, 12, 16]); w.rt2 = sbuf(s1, "rt2" + sfx, [128, 12, 16])
                        w.qkb = sbuf(s1, "qkb" + sfx, [128, 768], BF16)
                        w.v32 = sbuf(s1, "v32" + sfx, [128, 384])
                        w.ssqm = sbuf(s1, "ssqm" + sfx, [128, 4]); w.stdm = sbuf(s1, "stdm" + sfx, [128, 4]); w.rstdm = sbuf(s1, "rstdm" + sfx, [128, 4])
                        w.qm32 = sbuf(s1, "qm32" + sfx, [128, 4, 64]); w.qmb = sbuf(s1, "qmb" + sfx, [128, 256], BF16)
                        w.g1 = sbuf(s1, "g1" + sfx, [128, 384]); w.oab = sbuf(s1, "oab" + sfx, [128, 384], BF16)
                        w.cs = sbuf(s1, "cs" + sfx, [128, 16]); w.sn = sbuf(s1, "sn" + sfx, [128, 16])
                        wsets.append(w)

                    PSxT = PS[0]
                    psxT_b = PS[0][:].bitcast(BF16)
                    ps7_b = PS[7][:].bitcast(BF16)

                    def load_x(idx):
                        kind, it, nt, src_ap, ropebase = tiles1[idx]
                        par = idx % 2
                        w = wsets[par]
                        dma("sp", f"x{par}", w.xt[0:nt, :], src_ap, writes=[w.xt])
                        if ropebase is not None:
                            dma("sp", f"x{par}", w.cs[0:nt, :], rope_cs[ropebase:ropebase + nt, :], writes=[w.cs])
                            dma("sp", f"x{par}", w.sn[0:nt, :], rope_sn[ropebase:ropebase + nt, :], writes=[w.sn])

                    def norm_hT(w, src_ap, nt, gcol, par):
                        act(w.junk[0:nt, :], w.xt[0:nt, :], AF.Square, [w.xt], [w.junk, w.ssq], accum_out=w.ssq[0:nt, :])
                        rstd_from_ssq(w.ssq, w.std, w.rstd, 1, D, nt)
                        act(w.xs[0:nt, :], w.xt[0:nt, :], AF.Copy, [w.xt, w.rstd], [w.xs], scale=w.rstd[0:nt, :])
                        for c in range(8):
                            tr(psxT_b[:, c * 128:c * 128 + nt], w.xs[0:nt, c * 128:(c + 1) * 128], ident_b[0:nt, 0:nt],
                               [w.xs, ident_b], [PSxT])
                        tt("dve", w.hT[:, :, 0:nt], psxT_b.rearrange("p (c t) -> p c t", c=8)[:, :, 0:nt],
                           gcol[:, 0:8].unsqueeze(2).to_broadcast([128, 8, nt]), ALU.mult, [PSxT, gcol], [w.hT])

                    def proj(w, bank, wt, c0, width, nt):
                        for k in range(8):
                            mm(bank[0:nt, 0:width], w.hT[:, k, 0:nt], wt[:, k, c0:c0 + width], k == 0, k == 7,
                               [w.hT, wt], [bank])

                    def head_norm(w, bank, nh, h0, sq, ssq, std, rstd, dst32, nt, sq_off):
                        act(sq[0:nt, sq_off:sq_off + nh * 64], bank[0:nt, 0:nh * 64], AF.Square, [bank], [sq])

                    def rope(w, h0, nh, nt):
                        q = w.qk32
                        csb = w.cs[0:nt, :].unsqueeze(1).to_broadcast([nt, nh, 16])
                        tt("pool", w.rt1[0:nt, h0:h0 + nh, :], q[0:nt, h0:h0 + nh, 0:16], csb, ALU.mult, [w.qk32, w.cs], [w.rt1])
                        tt("pool", w.rt2[0:nt, h0:h0 + nh, 0:8], q[0:nt, h0:h0 + nh, 8:16],
                           w.sn[0:nt, 0:8].unsqueeze(1).to_broadcast([nt, nh, 8]), ALU.mult, [w.qk32, w.sn], [w.rt2])
                        tt("pool", w.rt2[0:nt, h0:h0 + nh, 8:16], q[0:nt, h0:h0 + nh, 0:8],
                           w.sn[0:nt, 8:16].unsqueeze(1).to_broadcast([nt, nh, 8]), ALU.mult, [w.qk32, w.sn], [w.rt2])
                        tt("pool", q[0:nt, h0:h0 + nh, 0:16], w.rt1[0:nt, h0:h0 + nh, :], w.rt2[0:nt, h0:h0 + nh, :], ALU.add,
                           [w.rt1, w.rt2], [w.qk32])

                    def qk_norm_rope(w, nt, with_q, ropebase):
                        h0 = 0 if with_q else 6
                        nh = 12 - h0
                        if with_q:
                            act(w.sqt[0:nt, 0:384], PS[3][0:nt, 0:384], AF.Square, [PS[3]], [w.sqt])
                        act(w.sqt[0:nt, 384:768], PS[4][0:nt, 0:384], AF.Square, [PS[4]], [w.sqt])
                        red(w.ssqk[0:nt, h0:12], w.sqt[0:nt, h0 * 64:768].rearrange("p (h d) -> p h d", d=64), [w.sqt], [w.ssqk])
                        act(w.stdk[0:nt, h0:12], w.ssqk[0:nt, h0:12], AF.Sqrt, [w.ssqk], [w.stdk], scale=1.0 / 64, bias=EPS)
                        recip(w.rstdk[0:nt, h0:12], w.stdk[0:nt, h0:12], [w.stdk], [w.rstdk])
                        if with_q:
                            tt("dve", w.qk32[0:nt, 0:6, :], PS[3][0:nt, 0:384].rearrange("p (h d) -> p h d", d=64),
                               w.rstdk[0:nt, 0:6].unsqueeze(2).to_broadcast([nt, 6, 64]), ALU.mult, [PS[3], w.rstdk], [w.qk32])
                        tt("dve", w.qk32[0:nt, 6:12, :], PS[4][0:nt, 0:384].rearrange("p (h d) -> p h d", d=64),
                           w.rstdk[0:nt, 6:12].unsqueeze(2).to_broadcast([nt, 6, 64]), ALU.mult, [PS[4], w.rstdk], [w.qk32])
                        tt("pool", w.qk32[0:nt, h0:12, :], w.qk32[0:nt, h0:12, :],
                           gqkB[0:nt, h0 * 64:768].rearrange("p (h d) -> p h d", d=64), ALU.mult, [w.qk32, gqkB], [w.qk32])
                        rope(w, h0, nh, nt)
                        cp("act", w.qkb[0:nt, h0 * 64:768], w.qk32[0:nt, h0:12, :].rearrange("p h d -> p (h d)"), [w.qk32], [w.qkb])

                    front_done = set()

                    def do_front(idx):
                        if idx in front_done or idx >= len(tiles1):
                            return
                        kind_, it_, nt_, src_, rb_ = tiles1[idx]
                        norm_hT(wsets[idx % 2], src_, nt_, gmc if kind_ == "mem" else g1c, idx % 2)
                        front_done.add(idx)

                    tiles1 = [("mem", mt, 128, mem_d[mt * 128:(mt + 1) * 128, :], None) for mt in range(2)]
                    tiles1 += [("halo", it, 128, xh[it * 128:(it + 1) * 128, :], SEG + it * 128) for it in range(16)]
                    tiles1 += [("own", it, 128, xp[it * 128:(it + 1) * 128, :], it * 128) for it in range(16)]
                    tiles1 += [("samp", 0, NS, xs_d[0:NS, :], 2 * SEG)]
                    load_x(0)

                    tcount = 0
                    for mt in range(2):
                        par = tcount % 2
                        load_x(tcount + 1)
                        cur = tcount
                        tcount += 1
                        w = wsets[par]
                        do_front(cur)
                        proj(w, PS[1], w_mem_b, 0, 512, 128)
                        act(w.sqt[:, 0:256], PS[1][:, 0:256], AF.Square, [PS[1]], [w.sqt])
                        red(w.ssqm[:, 0:4], w.sqt[:, 0:256].rearrange("p (h d) -> p h d", d=64), [w.sqt], [w.ssqm])
                        act(w.stdm[:, 0:4], w.ssqm[:, 0:4], AF.Sqrt, [w.ssqm], [w.stdm], scale=1.0 / 64, bias=EPS)
                        recip(w.rstdm[:, 0:4], w.stdm[:, 0:4], [w.stdm], [w.rstdm])
                        tt("dve", w.qm32[:, :, :], PS[1][:, 0:256].rearrange("p (h d) -> p h d", d=64),
                           w.rstdm[:, 0:4].unsqueeze(2).to_broadcast([128, 4, 64]), ALU.mult, [PS[1], w.rstdm], [w.qm32])
                        tt("pool", w.qm32[:, :, :], w.qm32[:, :, :], gkmB[:, :].rearrange("p (h d) -> p h d", d=64), ALU.mult,
                           [w.qm32, gkmB], [w.qm32])
                        dma("sp", f"o{par}", mk_p[mt * 128:(mt + 1) * 128, :], w.qm32[:, :, :].rearrange("p h d -> p (h d)"),
                            reads=[w.qm32], is_output=True)
                        cp("act", w.qmb[:, :], w.qm32[:, :, :].rearrange("p h d -> p (h d)"), [w.qm32], [w.qmb])
                        for pp in range(2):
                            tr(ps7_b[:, pp * 128:(pp + 1) * 128], w.qmb[:, pp * 128:(pp + 1) * 128], ident_b[:, :], [w.qmb, ident_b], [PS[7]])
                        cp("dve", kmT[:, :, mt * 128:(mt + 1) * 128], ps7_b[:, 0:256].rearrange("p (c t) -> p c t", c=2), [PS[7]], [kmT])
                        cp("act", w.v32[:, 0:256], PS[1][:, 256:512], [PS[1]], [w.v32])
                        do_front(cur + 1)
                        dma("sp", f"o{par}", mv_p[mt * 128:(mt + 1) * 128, :], w.v32[:, 0:256], reads=[w.v32], is_output=True)
                        for i in range(2):
                            cp("pool", vmp[:, mt, :, i * 128:i * 128 + 64],
                               w.v32[:, 0:256].rearrange("p (c i d) -> p c i d", c=2, i=2)[:, :, i, :], [w.v32], [vmp])

                    for it in range(16):
                        par = tcount % 2
                        load_x(tcount + 1)
                        cur = tcount
                        tcount += 1
                        w = wsets[par]
                        do_front(cur)
                        proj(w, PS[4], w_in_b, 1152, 384, 128)
                        proj(w, PS[5], w_in_b, 1536, 384, 128)
                        qk_norm_rope(w, 128, False, SEG + it * 128)
                        cp("act", w.v32[:, :], PS[5][:, 0:384], [PS[5]], [w.v32])
                        do_front(cur + 1)
                        for pp in range(3):
                            tr(ps7_b[:, (3 + pp) * 128:(4 + pp) * 128], w.qkb[:, 384 + pp * 128:384 + (pp + 1) * 128], ident_b[:, :],
                               [w.qkb, ident_b], [PS[7]])
                        cp("act", kT[:, :, it * 128:(it + 1) * 128], ps7_b[:, 384:768].rearrange("p (c t) -> p c t", c=3), [PS[7]], [kT])
                        dma("sp", f"o{par}", vscr[it * 128:(it + 1) * 128, :], w.v32[:, :], reads=[w.v32])

                    tiles = [("own", it, 128) for it in range(16)] + [("samp", 0, NS)]
                    for kind, it, nt in tiles:
                        par = tcount % 2
                        if tcount + 1 < len(tiles1):
                            load_x(tcount + 1)
                        cur = tcount
                        tcount += 1
                        w = wsets[par]
                        samp = kind == "samp"
                        tok0 = SEG if samp else it * 128
                        src = xs_d[0:NS, :] if samp else xp[it * 128:(it + 1) * 128, :]
                        do_front(cur)
                        for j in range(6):
                            proj(w, PS[1 + j], w_in_b, j * 384, 384 if j < 5 else 256, nt)
                        cp("act", w.u32[0:nt, :], PS[1][0:nt, 0:384], [PS[1]], [w.u32])
                        act(w.junk[0:nt, 0:384], PS[2][0:nt, 0:384], AF.Square, [PS[2]], [w.junk, w.ssqv], accum_out=w.ssqv[0:nt, :])
                        rstd_from_ssq(w.ssqv, w.stdv, w.rstdv, 1, 384, nt)
                        stt(w.vn32[0:nt, :], PS[2][0:nt, 0:384], w.rstdv[0:nt, :], gvB[0:nt, :], ALU.mult, ALU.mult,
                            [PS[2], w.rstdv, gvB], [w.vn32])
                        cp("pool", w.vnb[0:nt, :], w.vn32[0:nt, :], [w.vn32], [w.vnb])
                        if samp:
                            dma("sp", f"o{par}", gv_s[:, :], w.vn32[0:nt, :], reads=[w.vn32], is_output=True)
                        ropebase = 2 * SEG if samp else it * 128
                        qk_norm_rope(w, nt, True, ropebase)
                        if samp:
                            dma("sp", f"o{par}", wk_s[:, :], w.qk32[0:nt, 6:12, :].rearrange("p h d -> p (h d)"), reads=[w.qk32], is_output=True)
                        else:
                            dma("sp", f"o{par}", wk_p[it * 128:(it + 1) * 128, :], w.qk32[0:nt, 6:12, :].rearrange("p h d -> p (h d)"),
                                reads=[w.qk32], is_output=True)
                        cp("act", w.v32[0:nt, :], PS[5][0:nt, 0:384], [PS[5]], [w.v32])
                        if samp:
                            dma("sp", f"o{par}", wv_s[:, :], w.v32[0:nt, :], reads=[w.v32], is_output=True)
                            for i in range(2):
                                cp("pool", vsp[0:nt, :, i * 128:i * 128 + 64],
                                   w.v32[0:nt, :].rearrange("p (c i d) -> p c i d", c=3, i=2)[:, :, i, :], [w.v32], [vsp])
                        else:
                            dma("sp", f"o{par}", wv_p[it * 128:(it + 1) * 128, :], w.v32[0:nt, :], reads=[w.v32], is_output=True)
                            dma("sp", f"o{par}", vscr[SEG + it * 128:SEG + (it + 1) * 128, :], w.v32[0:nt, :], reads=[w.v32])
                        act(w.sqt[0:nt, 0:256], PS[6][0:nt, 0:256], AF.Square, [PS[6]], [w.sqt])
                        red(w.ssqm[0:nt, 0:4], w.sqt[0:nt, 0:256].rearrange("p (h d) -> p h d", d=64), [w.sqt], [w.ssqm])
                        act(w.stdm[0:nt, 0:4], w.ssqm[0:nt, 0:4], AF.Sqrt, [w.ssqm], [w.stdm], scale=1.0 / 64, bias=EPS)
                        recip(w.rstdm[0:nt, 0:4], w.stdm[0:nt, 0:4], [w.stdm], [w.rstdm])
                        tt("dve", w.qm32[0:nt, :, :], PS[6][0:nt, 0:256].rearrange("p (h d) -> p h d", d=64),
                           w.rstdm[0:nt, 0:4].unsqueeze(2).to_broadcast([nt, 4, 64]), ALU.mult, [PS[6], w.rstdm], [w.qm32])
                        tt("pool", w.qmb[0:nt, :].rearrange("p (h d) -> p h d", d=64), w.qm32[0:nt, :, :],
                           gqmB[0:nt, :].rearrange("p (h d) -> p h d", d=64), ALU.mult, [w.qm32, gqmB], [w.qmb])
                        do_front(cur + 1)
                        for pp in range(3):
                            tr(ps7_b[:, pp * 128:pp * 128 + nt], w.qkb[0:nt, pp * 128:(pp + 1) * 128], ident_b[0:nt, 0:nt], [w.qkb, ident_b], [PS[7]])
                        for pp in range(3):
                            tr(ps7_b[:, (3 + pp) * 128:(3 + pp) * 128 + nt], w.qkb[0:nt, 384 + pp * 128:384 + (pp + 1) * 128], ident_b[0:nt, 0:nt],
                               [w.qkb, ident_b], [PS[7]])
                        for pp in range(2):
                            tr(ps7_b[:, (6 + pp) * 128:(6 + pp) * 128 + nt], w.qmb[0:nt, pp * 128:(pp + 1) * 128], ident_b[0:nt, 0:nt],
                               [w.qmb, ident_b], [PS[7]])
                        p7v = ps7_b.rearrange("p (c t) -> p c t", c=8)
                        if samp:
                            cp("dve", qTs[:, :, :], p7v[:, 0:3, 0:nt], [PS[7]], [qTs])
                            cp("act", kTs[:, :, :], p7v[:, 3:6, 0:nt], [PS[7]], [kTs])
                        else:
                            cp("dve", qT[:, :, it * 128:(it + 1) * 128], p7v[:, 0:3, :], [PS[7]], [qT])
                            cp("act", kT[:, :, SEG + it * 128:SEG + (it + 1) * 128], p7v[:, 3:6, :], [PS[7]], [kT])
                        cp("dve", qmT[:, :, tok0:tok0 + nt], p7v[:, 6:8, 0:nt], [PS[7]], [qmT])
                        for gi in range(6):
                            lhs = bd_b[0:nt, gi, 0:nt] if samp else wsT_b[:, gi, :]
                            mm(PS[0][0:nt, gi * 64:(gi + 1) * 64], lhs, w.vnb[0:nt, gi * 64:(gi + 1) * 64], True, True,
                               [bd_b if samp else wsT_b, w.vnb], [PS[0]])
                        bsrc = bsS if samp else bsB
                        tt("dve", w.g1[0:nt, :], PS[0][0:nt, 0:384], bsrc[0:nt, :], ALU.add, [PS[0], bsrc], [w.g1])
                        tt("pool", w.oab[0:nt, :], w.g1[0:nt, :], w.u32[0:nt, :], ALU.mult, [w.g1, w.u32], [w.oab])
                        for pp in range(3):
                            tr(ps7_b[:, pp * 128:pp * 128 + nt], w.oab[0:nt, pp * 128:(pp + 1) * 128], ident_b[0:nt, 0:nt], [w.oab, ident_b], [PS[7]])
                        cp("act", oaT[:, :, tok0:tok0 + nt], p7v[:, 0:3, 0:nt], [PS[7]], [oaT])

                P.barrier()
                with contextlib.ExitStack() as s2:
                  if KSTOP >= 2:
                    maskA = sbuf(s2, "maskA", [128, 512], BF16); maskH = sbuf(s2, "maskH", [128, 512], BF16)
                    maskS = sbuf(s2, "maskS", [128, 216], BF16); maskN = sbuf(s2, "maskN", [64, 384], BF16)
                    dma("pool", "cp", maskA[:], maskA_d[:, :], writes=[maskA])
                    dma("pool", "cp", maskH[:], maskH_d[:, :], writes=[maskH])
                    dma("pool", "cp", maskS[:], maskS_d[:, :], writes=[maskS])
                    dma("pool", "cp", maskN[:], maskN_d[:, :], writes=[maskN])
                    Pf = [sbuf(s2, f"Pf{i}", [128, 512], BF16) for i in range(2)]
                    Pm = [sbuf(s2, f"Pm{i}", [128, 512], BF16) for i in range(2)]
                    rl = sbuf(s2, "rl", [128, 512])
                    s2a = contextlib.ExitStack()
                    acc = sbuf(s2a, "acc", [128, 2, 3, SEG])
                    vbuf = sbuf(s2a, "vbuf", [128, 32, 3, 192], BF16)
                    P.op("pool", lambda h: h.memset(vbuf[:], 0.0), [], [vbuf.r])

                    def vload(dil):
                        own = vscr[SEG:2 * SEG, :]
                        halo = vscr[0:SEG, :]
                        if dil == 1:
                            srcs = [(own.rearrange("(n j) f -> j n f", j=128), 0, 16),
                                    (halo[SEG - 128:SEG, :].rearrange("(n j) f -> j n f", j=128), 16, 1)]
                        elif dil == 4:
                            srcs = [(own[n * 512:(n + 1) * 512, :].rearrange("(j r) f -> j r f", r=4), 4 * n, 4) for n in range(4)]
                            srcs += [(halo[SEG - 512:SEG, :].rearrange("(j r) f -> j r f", r=4), 16, 4)]
                        else:
                            srcs = [(own.rearrange("(j r) f -> j r f", r=16), 0, 16),
                                    (halo.rearrange("(j r) f -> j r f", r=16), 16, 16)]
                        for src, t0, ntile in srcs:
                            for i in range(2):
                                for c in range(3):
                                    s = src.rearrange("j t (c i d) -> j t c i d", c=3, i=2)[:, :, c, i, :]
                                    d = vbuf[:, t0:t0 + ntile, c, i * 128:i * 128 + 64]
                                    dma("pool", "v", d, s, writes=[vbuf])

                    it2 = 0
                    for bi, dil in enumerate((1, 4, 16)):
                        vload(dil)
                        nblk = 16 // dil
                        bspan = 128 * dil
                        for r in range(dil):
                            for n in range(nblk):
                                qs = slice(n * bspan + r, (n + 1) * bspan, dil)
                                kc = slice(SEG + n * bspan + r, SEG + (n + 1) * bspan, dil)
                                kp = slice(SEG + (n - 1) * bspan + r, SEG + n * bspan, dil)
                                vt_c = n * dil + r
                                vt_p = (n - 1) * dil + r if n > 0 else 16 + r
                                msk = maskH if n == 0 else maskA
                                for p in range(3):
                                    sl = it2 % 2; it2 += 1
                                    psSi = (PS[sl], PS[4 + sl]); psO = PS[2 + sl]
                                    for kt, kcols in enumerate((kp, kc)):
                                        for i in range(2):
                                            mm(psSi[i][:, kt * 128:(kt + 1) * 128], kT[64 * i:64 * i + 64, p, kcols], qT[64 * i:64 * i + 64, p, qs],
                                               True, True, [kT, qT], [psSi[i]])
                                    for i in range(2):
                                        act(Pf[sl][:, i * 256:(i + 1) * 256], psSi[i][:, 0:256], AF.Exp, [psSi[i]], [Pf[sl]], scale=SCALE)
                                    tt("pool", Pm[sl][:, :], Pf[sl][:, :], msk[:, :], ALU.mult, [Pf[sl], msk], [Pm[sl]])
                                    for part in range(2):
                                        k = 0
                                        for kt, vt in enumerate((vt_p, vt_c)):
                                            for i in range(2):
                                                s = i * 2 + kt
                                                lhs = vbuf[:, vt, p, i * 64:i * 64 + 128] if part == 0 else onesAB[:, i * 64:i * 64 + 128]
                                                mm(psO[:, part * 128:(part + 1) * 128], lhs, Pm[sl][:, s * 128:(s + 1) * 128], k == 0, k == 3,
                                                   [vbuf if part == 0 else onesAB, Pm[sl]], [psO])
                                                k += 1
                                    accv = acc[:, :, p, qs]
                                    pov = psO[:, 0:256].rearrange("p (a q) -> p a q", a=2)
                                    if bi == 0:
                                        cp("dve", accv, pov, [psO], [acc])
                                    else:
                                        tt("dve", accv, pov, accv, ALU.add, [psO, acc], [acc])
                    for p in range(3):
                        for q0 in range(0, SEG, 512):
                            recip(rl[:, :], acc[:, 1, p, q0:q0 + 512], [acc], [rl])
                            tt("pool", mixB[:, p, q0:q0 + 512], acc[:, 0, p, q0:q0 + 512], rl[:, :], ALU.mult, [acc, rl], [mixB])
                    P.barrier()
                    s2a.close()

                    for q0 in range(0, SEG, 512):
                        for p in range(2):
                            psO = PS[2]; psL = PS[3]
                            k = 0
                            for mt in range(2):
                                for i in range(2):
                                    sl = it2 % 2; it2 += 1
                                    h = 2 * p + i
                                    mm(PS[sl][:, :], kmT[64 * i:64 * i + 64, p, mt * 128:(mt + 1) * 128], qmT[64 * i:64 * i + 64, p, q0:q0 + 512],
                                       True, True, [kmT, qmT], [PS[sl]])
                                    act(Pf[sl][:, :], PS[sl][:, :], AF.Exp, [PS[sl]], [Pf[sl]], scale=SCALE)
                                    mm(psO[:, :], vmp[:, mt, p, i * 64:i * 64 + 128], Pf[sl][:, :], k == 0, k == 3, [vmp, Pf[sl]], [psO])
                                    mm(psL[:, :], onesAB[:, i * 64:i * 64 + 128], Pf[sl][:, :], k == 0, k == 3, [onesAB, Pf[sl]], [psL])
                                    k += 1
                            recip(rl[:, :], psL[:, :], [psL], [rl])
                            tt("dve", mixM[:, p, q0:q0 + 512], psO[:, :], rl[:, :], ALU.mult, [psO, rl], [mixM])

                    kms = sbuf(s2, "kms", [128, 16, 2, 256], BF16)
                    vms = sbuf(s2, "vms", [128, 16, 2, 2, 192], BF16)
                    dma("pool", "cp", kms[:], cmkT[:, :, :, :], writes=[kms])
                    P.op("pool", lambda h: h.memset(vms[:], 0.0), [], [vms.r])
                    for mt in range(2):
                        for i in range(2):
                            for c in range(2):
                                dma("pool", "cp", vms[:, :, mt, c, i * 128:i * 128 + 64],
                                    cmv[:, :, mt, :].rearrange("m b (c i d) -> m b c i d", c=2, i=2)[:, :, c, i, :], writes=[vms])
                    for b in range(16):
                        for p in range(2):
                            for mt in range(2):
                                for i in range(2):
                                    col = (b * 2 + p) * 2 + mt
                                    mm(PS[i][:, col * 4:col * 4 + 4], kms[64 * i:64 * i + 64, b, p, mt * 128:(mt + 1) * 128],
                                       qmT[64 * i:64 * i + 64, p, SEG + 4 * b:SEG + 4 * b + 4], True, True, [kms, qmT], [PS[i]])
                    for i in range(2):
                        act(Pf[0][:, i * 256:(i + 1) * 256], PS[i][:, 0:256], AF.Exp, [PS[i]], [Pf[0]], scale=SCALE)
                    psO = PS[2]
                    for part in range(2):
                        for b in range(16):
                            for p in range(2):
                                k = 0
                                for mt in range(2):
                                    for i in range(2):
                                        col = i * 64 + (b * 2 + p) * 2 + mt
                                        lhs = vms[:, b, mt, p, i * 64:i * 64 + 128] if part == 0 else onesAB[:, i * 64:i * 64 + 128]
                                        mm(psO[:, part * 128 + p * 64 + 4 * b:part * 128 + p * 64 + 4 * b + 4], lhs, Pf[0][:, col * 4:col * 4 + 4],
                                           k == 0, k == 3, [vms if part == 0 else onesAB, Pf[0]], [psO])
                                        k += 1
                    recip(rl[:, 0:128], psO[:, 128:256], [psO], [rl])
                    tt("dve", mixM[:, :, SEG:SEG + NS], psO[:, 0:128].rearrange("p (c q) -> p c q", c=2),
                       rl[:, 0:128].rearrange("p (c q) -> p c q", c=2), ALU.mult, [psO, rl], [mixM])

                    kws = [sbuf(s2, f"kws{i}", [128, 3, 1152], BF16) for i in range(2)]
                    vws = [sbuf(s2, f"vws{i}", [128, 9, 3, 192], BF16) for i in range(2)]
                    for i in range(2):
                        P.op("pool", lambda h, i=i: h.memset(vws[i][:], 0.0), [], [vws[i].r])
                    onew = sbuf(s2, "onew", [128, 2, 3, 64])
                    psNi = (PS[4], PS[3])
                    for p in range(3):
                        for i in range(2):
                            mm(psNi[i][0:64, p * 64:(p + 1) * 64], kTs[64 * i:64 * i + 64, p, :], qTs[64 * i:64 * i + 64, p, :],
                               True, True, [kTs, qTs], [psNi[i]])
                    for i in range(2):
                        act(Pf[1][0:64, i * 192:(i + 1) * 192], psNi[i][0:64, 0:192], AF.Exp, [psNi[i]], [Pf[1]], scale=SCALE)
                    tt("pool", Pm[1][0:64, 0:384], Pf[1][0:64, 0:384], maskN[:, :], ALU.mult, [Pf[1], maskN], [Pm[1]])
                    psNO = PS[5]
                    for part in range(2):
                        for p in range(3):
                            for i in range(2):
                                lhs = vsp[0:64, p, i * 64:i * 64 + 128] if part == 0 else onesAB[0:64, i * 64:i * 64 + 128]
                                mm(psNO[:, (part * 3 + p) * 64:(part * 3 + p + 1) * 64], lhs, Pm[1][0:64, (i * 3 + p) * 64:(i * 3 + p + 1) * 64],
                                   i == 0, i == 1, [vsp if part == 0 else onesAB, Pm[1]], [psNO])
                    cp("dve", onew[:, :, :, :], psNO[:, 0:384].rearrange("p (a c q) -> p a c q", a=2, c=3), [psNO], [onew])
                    psW = PS[6]
                    for b in range(16):
                        sl = b % 2
                        dma("pool", f"kv{sl}", kws[sl][:], cwkT[b], writes=[kws[sl]])
                        for i in range(2):
                            for c in range(3):
                                dma("pool", f"kv{sl}", vws[sl][:, :, c, i * 128:i * 128 + 64],
                                    cwv[b].rearrange("k t (c i d) -> k t c i d", c=3, i=2)[:, :, c, i, :], writes=[vws[sl]])
                        psSi = (PS[sl], PS[2 + sl])
                        for t9 in range(9):
                            for p in range(3):
                                for i in range(2):
                                    col = t9 * 3 + p
                                    mm(psSi[i][:, col * 4:col * 4 + 4], kws[sl][64 * i:64 * i + 64, p, t9 * 128:(t9 + 1) * 128],
                                       qTs[64 * i:64 * i + 64, p, 4 * b:4 * b + 4], True, True, [kws[sl], qTs], [psSi[i]])
                        for i in range(2):
                            act(Pf[sl][:, i * 108:(i + 1) * 108], psSi[i][:, 0:108], AF.Exp, [psSi[i]], [Pf[sl]], scale=SCALE)
                        tt("pool", Pm[sl][:, 0:216], Pf[sl][:, 0:216], maskS[:, :], ALU.mult, [Pf[sl], maskS], [Pm[sl]])
                        for part in range(2):
                            for p in range(3):
                                k = 0
                                for t9 in range(9):
                                    for i in range(2):
                                        col = i * 27 + t9 * 3 + p
                                        lhs = vws[sl][:, t9, p, i * 64:i * 64 + 128] if part == 0 else onesAB[:, i * 64:i * 64 + 128]
                                        c0 = (part * 3 + p) * 64 + 4 * b
                                        mm(psW[:, c0:c0 + 4], lhs, Pm[sl][:, col * 4:col * 4 + 4], k == 0, k == 17,
                                           [vws[sl] if part == 0 else onesAB, Pm[sl]], [psW])
                                        k += 1
                    tt("dve", onew[:, :, :, :], psW[:, 0:384].rearrange("p (a c q) -> p a c q", a=2, c=3), onew[:, :, :, :], ALU.add,
                       [psW, onew], [onew])
                    recip(rl[:, 0:192], onew[:, 1, :, :].rearrange("p c q -> p (c q)"), [onew], [rl])
                    tt("dve", mixB[:, :, SEG:SEG + NS], onew[:, 0, :, :], rl[:, 0:192].rearrange("p (c q) -> p c q", c=3), ALU.mult,
                       [onew, rl], [mixB])
                P.barrier()
            with contextlib.ExitStack() as s3:
              if KSTOP >= 3:
                w_out_b = sbuf(s3, "w_out_b", [128, 8, D], BF16)
                dma("pool", "cp", w_out_b[:], w_out.rearrange("(c p) n -> p c n", p=128), writes=[w_out_b])
                xt3 = [sbuf(s3, f"xt3_{i}", [128, D]) for i in range(2)]
                x13 = [sbuf(s3, f"x13_{i}", [128, D]) for i in range(2)]
                srcT = [(oaT, 0), (oaT, 1), (oaT, 2), (mixB, 0), (mixB, 1), (mixB, 2), (mixM, 0), (mixM, 1)]
                for j in range(17):
                    nt = 128 if j < 16 else NS
                    par = j % 2
                    src = xp[j * 128:(j + 1) * 128, :] if j < 16 else xs_d[0:NS, :]
                    dma("sp", f"x{par}", xt3[par][0:nt, :], src, writes=[xt3[par]])
                    for hf in range(2):
                        bank = PS[(j * 2 + hf) % 4]
                        for c, (sb_, ci) in enumerate(srcT):
                            mm(bank[0:nt, :], sb_[:, ci, j * 128:j * 128 + nt], w_out_b[:, c, hf * 512:(hf + 1) * 512], c == 0, c == 7,
                               [sb_, w_out_b], [bank])
                        tt("dve", x13[par][0:nt, hf * 512:(hf + 1) * 512], bank[0:nt, :], xt3[par][0:nt, hf * 512:(hf + 1) * 512], ALU.add,
                           [bank, xt3[par]], [x13[par]])
                    dma("sp", f"o{par}", x1scr[j * 128:j * 128 + nt, :], x13[par][0:nt, :], reads=[x13[par]])
                P.barrier()

        NSLOT = 54
        SR = 384
        QT = SR // 128
        NROW = NSLOT * SR
        IO = bass.IndirectOffsetOnAxis
        w_gu2 = w_gu.rearrange("e r n -> (e r) n")
        w_dn2 = w_dn.rearrange("e r n -> (e r) n")

        regs = {}

        def _mkregs(h):
            for name, val in (("tok", NTOK + 127), ("w", NEXP * D - 1), ("row", NROW - 1)):
                r = h.alloc_register("bnd_" + name)
                h.reg_mov(r, val)
                regs[name] = r
        P.ops["pool"].append(_mkregs)

        def gather(sem, out, src, idx, reads, writes, bound):
            P.dmaf("pool", sem, lambda h: h.indirect_dma_start(out=out, out_offset=None, in_=src, in_offset=IO(ap=idx, axis=0),
                                                             bounds_check=regs[bound], oob_is_err=False),
                   [b.r for b in reads], [b.r for b in writes])

        with contextlib.ExitStack() as s4:
          if KSTOP >= 4:
            comb = sbuf(s4, "comb", [128, 17, 32]); msel = sbuf(s4, "msel", [128, 17, 32])
            pos4i = sbuf(s4, "pos4i", [128, 17 * 4], I32); tokid = sbuf(s4, "tokid", [128, 17], I32)
            onehot = sbuf(s4, "onehot", [128, NSLOT, 32])
            idxw = sbuf(s4, "idxw", [128, NSLOT * 8], I32); idxall = sbuf(s4, "idxall", [128, NSLOT * QT], I32)
            bguT = sbuf(s4, "bguT", [128, NEXP, 16])
            mc = sbuf(s4, "mc", [128, 111])
            siota = mc[:, 0:54]; eiota = mc[:, 54:86]; wrow = mc[:, 86:94]
            dma("sp", "c", mc[:], mconst_d[:, :], writes=[mc])
            dma("sp", "c", bguT[:], bguT_d[:, :, :], writes=[bguT])
            ts("dve", bguT[:, :, 8:16], bguT[:, :, 8:16], 1.0, None, ALU.add, None, [bguT], [bguT])
            cp("dve", tokid[:, :], mc[:, 94:111], [mc], [tokid])

            with contextlib.ExitStack() as s4a:
                g2c = sbuf(s4a, "g2c", [128, 8]); g2B = sbuf(s4a, "g2B", [128, D]); wr32 = sbuf(s4a, "wr32", [128, 8, 32])
                brB = sbuf(s4a, "brB", [128, 32]); bdn32 = sbuf(s4a, "bdn32", [32, D])
                triu = sbuf(s4a, "triu", [128, 128]); ones_f = sbuf(s4a, "ones_f", [128, 128])
                zb = sbuf(s4a, "zb", [128, D], BF16); zf = sbuf(s4a, "zf", [128, 32]); fillf = sbuf(s4a, "fillf", [128, NSLOT * QT])
                filli = sbuf(s4a, "filli", [128, NSLOT * QT], I32)
                dma("sp", "c", g2c[:], norm2_g[:, :], writes=[g2c])
                dma("sp", "c", g2B[:], norm2_row.partition_broadcast(128), writes=[g2B])
                dma("sp", "c", wr32[:], w_router.rearrange("(c p) n -> p c n", p=128), writes=[wr32])
                dma("sp", "c", brB[:], b_router.partition_broadcast(128), writes=[brB])
                dma("sp", "c", bdn32[:], b_dn[:, :], writes=[bdn32])
                dma("sp", "c", triu[:], triu_d[:, :], writes=[triu])
                P.op("pool", lambda h: h.memset(ones_f[:], 1.0), [], [ones_f.r])
                P.op("pool", lambda h: h.memset(zb[:], 0.0), [], [zb.r])
                P.op("pool", lambda h: h.memset(zf[:], 0.0), [], [zf.r])
                P.op("pool", lambda h: h.memset(fillf[:], float(NTOK)), [], [fillf.r])
                cp("dve", filli[:, :], fillf[:, :], [fillf], [filli])
                dma("sp", "o0", hn_d[NTOK:NTOK + 128, :], zb[:, :], reads=[zb])
                dma("sp", "o0", comb_d[NTOK:NTOK + 128, :], zf[:, :], reads=[zf])
                Rinv = Res("inv_d")
                P.dma("pool", "g", inv_d.rearrange("(p c) o -> p (c o)", p=128), filli[:, :], [filli.r], [Rinv])

                sm = lambda name, shape, dt=F32: sbuf(s4a, name, shape, dt)

                class TS:
                    pass
                tsets = []
                for gi in range(2):
                    T = TS(); sfx = f"_{gi}"
                    T.junk = sbuf(s4a, "junk4" + sfx, [128, D], BF16); T.hnb = sbuf(s4a, "hnb" + sfx, [128, D], BF16)
                    T.ssq = sbuf(s4a, "ssq4" + sfx, [128, 1]); T.std = sbuf(s4a, "std4" + sfx, [128, 1]); T.rstd = sbuf(s4a, "rstd4" + sfx, [128, 1])
                    T.hn32 = sbuf(s4a, "hn32" + sfx, [128, D]); T.hT32 = sbuf(s4a, "hT32" + sfx, [128, 8, 128])
                    T.lg = sbuf(s4a, "lg" + sfx, [128, 32]); T.m8 = sbuf(s4a, "m8" + sfx, [128, 8]); T.nmx = sbuf(s4a, "nmx" + sfx, [128, 1])
                    T.ex = sbuf(s4a, "ex" + sfx, [128, 32]); T.cw = sbuf(s4a, "cw" + sfx, [128, 32])
                    T.den = sbuf(s4a, "den" + sfx, [128, 1]); T.rden = sbuf(s4a, "rden" + sfx, [128, 1])
                    T.combT = sbuf(s4a, "combT" + sfx, [32, 128])
                    T.ps = [PS[4 * gi + i] for i in range(4)]
                    T.osem = f"o{gi}"
                    tsets.append(T)
                xts = [sbuf(s4a, f"x1t{i}", [128, D]) for i in range(4)]

                def load_x1(j):
                    nt = 128 if j < 16 else NS
                    dma("sp", "m", xts[j % 4][0:nt, :], x1scr[j * 128:j * 128 + nt, :], writes=[xts[j % 4]])

                def setup_tile(j, T):
                    nt = 128 if j < 16 else NS
                    r0 = j * 128
                    xt = xts[j % 4]
                    act(T.junk[0:nt, :], xt[0:nt, :], AF.Square, [xt], [T.junk, T.ssq], accum_out=T.ssq[0:nt, :])
                    yield
                    rstd_from_ssq(T.ssq, T.std, T.rstd, 1, D, nt)
                    yield
                    act(T.hn32[0:nt, :], xt[0:nt, :], AF.Copy, [xt, T.rstd], [T.hn32], scale=T.rstd[0:nt, :])
                    yield
                    tt("pool", T.hnb[0:nt, :], T.hn32[0:nt, :], g2B[0:nt, :], ALU.mult, [T.hn32, g2B], [T.hnb])
                    dma("sp", T.osem, hn_d[r0:r0 + nt, :], T.hnb[0:nt, :], reads=[T.hnb])
                    for c in range(8):
                        bank = T.ps[c // 4]
                        tr(bank[:, (c % 4) * 128:(c % 4) * 128 + nt], T.hn32[0:nt, c * 128:(c + 1) * 128], ident_f[0:nt, 0:nt], [T.hn32, ident_f], [bank])
                    yield
                    for hb in range(2):
                        bank = T.ps[hb]
                        bv = bank[:, :].rearrange("p (c t) -> p c t", c=4)[:, :, 0:nt]
                        gb = g2c[:, hb * 4:hb * 4 + 4].unsqueeze(2).to_broadcast([128, 4, nt])
                        tt("dve", T.hT32[:, hb * 4:hb * 4 + 4, 0:nt], bv, gb, ALU.mult, [bank, g2c], [T.hT32])
                    yield
                    for c in range(8):
                        mm(T.ps[2][0:nt, 0:32], T.hT32[:, c, 0:nt], wr32[:, c, :], c == 0, c == 7, [T.hT32, wr32], [T.ps[2]])
                    yield
                    tt("dve", T.lg[0:nt, :], T.ps[2][0:nt, 0:32], brB[0:nt, :], ALU.add, [T.ps[2], brB], [T.lg])
                    P.op("dve", lambda h: h.max(out=T.m8[0:nt, :], in_=T.lg[0:nt, :]), [T.lg.r], [T.m8.r])
                    ts("dve", T.nmx[0:nt, :], T.m8[0:nt, 0:1], -1.0, None, ALU.mult, None, [T.m8], [T.nmx])
                    yield
                    act(T.ex[0:nt, :], T.lg[0:nt, :], AF.Exp, [T.lg, T.nmx], [T.ex], bias=T.nmx[0:nt, :])
                    yield
                    stt(T.cw[0:nt, :], T.lg[0:nt, :], T.m8[0:nt, 3:4], T.ex[0:nt, :], ALU.is_ge, ALU.mult, [T.lg, T.m8, T.ex], [T.cw, T.den],
                        accum_out=T.den[0:nt, :])
                    recip(T.rden[0:nt, :], T.den[0:nt, :], [T.den], [T.rden])
                    ts("dve", comb[0:nt, j, :], T.cw[0:nt, :], T.rden[0:nt, :], None, ALU.mult, None, [T.cw, T.rden], [comb])
                    ts("dve", msel[0:nt, j, :], T.lg[0:nt, :], T.m8[0:nt, 3:4], None, ALU.is_ge, None, [T.lg, T.m8], [msel])
                    dma("sp", T.osem, comb_d[r0:r0 + nt, :], comb[0:nt, j, :], reads=[comb])
                    tr(T.ps[2][0:32, 128:128 + nt], comb[0:nt, j, :], ident_f[0:nt, 0:nt], [comb, ident_f], [T.ps[2]])
                    yield
                    cp("act", T.combT[:, 0:nt], T.ps[2][0:32, 128:128 + nt], [T.ps[2]], [T.combT])
                    yield
                    for hf in range(2):
                        bank = T.ps[3] if hf == 0 else T.ps[0]
                        mm(bank[0:nt, :], T.combT[:, 0:nt], bdn32[:, hf * 512:(hf + 1) * 512], True, True, [T.combT, bdn32], [bank])
                    yield
                    for hf in range(2):
                        bank = T.ps[3] if hf == 0 else T.ps[0]
                        tt("dve", xt[0:nt, hf * 512:(hf + 1) * 512], bank[0:nt, :], xt[0:nt, hf * 512:(hf + 1) * 512], ALU.add, [bank, xt], [xt])
                    dma("sp", T.osem, x1scr[r0:r0 + nt, :], xt[0:nt, :], reads=[xt])

                load_x1(0); load_x1(1)
                for j0 in range(0, 17, 2):
                    for j in (j0 + 2, j0 + 3):
                        if j < 17:
                            load_x1(j)
                    gens = [setup_tile(j, tsets[j - j0]) for j in (j0, j0 + 1) if j < 17]
                    while gens:
                        for g in list(gens):
                            try:
                                next(g)
                            except StopIteration:
                                gens.remove(g)

                cnt = sm("cnt", [128, 32]); nsl = [sm("nslA", [128, 32]), sm("nslB", [128, 32])]
                cum = [sm("cumA", [128, 32]), sm("cumB", [128, 32])]; st512 = sm("st512", [128, 32])
                tmp3 = sm("tmp3", [128, NSLOT, 32]); eall = sm("eall", [128, NSLOT]); idxwf = sm("idxwf", [128, NSLOT, 8])
                vt = [sm("vtA", [128, 32]), sm("vtB", [128, 32])]; p8 = sm("p8", [128, 8]); p4f = sm("p4f", [128, 4])
                ones_b = sm("ones_b", [128, 128], BF16); triu_b = sm("triu_b", [128, 128], BF16); msel_b = sm("msel_b", [128, 17, 32], BF16)
                cp("act", ones_b[:, :], ones_f[:, :], [ones_f], [ones_b])
                cp("act", triu_b[:, :], triu[:, :], [triu], [triu_b])
                cp("dve", msel_b[:, 0:16, :], msel[:, 0:16, :], [msel], [msel_b])
                cp("dve", msel_b[0:NS, 16, :], msel[0:NS, 16, :], [msel], [msel_b])
                for j in range(17):
                    nt = 128 if j < 16 else NS
                    mm(PS[0][:, 0:32], ones_b[0:nt, :], msel_b[0:nt, j, :], j == 0, j == 16, [ones_b, msel_b], [PS[0]])
                for j in range(17):
                    nt = 128 if j < 16 else NS
                    bank = PS[1] if j < 16 else PS[2]
                    dst = bank[0:nt, (j % 16) * 32:(j % 16) * 32 + 32]
                    for jp in range(j):
                        mm(dst, ones_b[:, 0:nt], msel_b[:, jp, :], jp == 0, False, [ones_b, msel_b], [bank])
                    mm(dst, triu_b[0:nt, 0:nt], msel_b[0:nt, j, :], j == 0, True, [triu_b, msel_b], [bank])
                cp("dve", cnt[:, :], PS[0][:, 0:32], [PS[0]], [cnt])
                ts("dve", nsl[0][:, :], cnt[:, :], 0.0, None, ALU.is_gt, None, [cnt], [nsl[0]])
                a = 0
                for k in range(1, 6):
                    stt(nsl[1 - a][:, :], cnt[:, :], float(SR) * k, nsl[a][:, :], ALU.is_gt, ALU.add, [cnt, nsl[a]], [nsl[1 - a]])
                    a = 1 - a
                nslots = nsl[a]
                cp("dve", cum[0][:, :], nslots[:, :], [nslots], [cum[0]])
                b_ = 0
                for sh in (1, 2, 4, 8, 16):
                    cp("dve", cum[1 - b_][:, 0:sh], cum[b_][:, 0:sh], [cum[b_]], [cum[1 - b_]])
                    tt("dve", cum[1 - b_][:, sh:32], cum[b_][:, sh:32], cum[b_][:, 0:32 - sh], ALU.add, [cum[b_]], [cum[1 - b_]])
                    b_ = 1 - b_
                cumi = cum[b_]
                tt("dve", st512[:, :], cumi[:, :], nslots[:, :], ALU.subtract, [cumi, nslots], [st512])
                ts("dve", st512[:, :], st512[:, :], float(SR), None, ALU.mult, None, [st512], [st512])
                tt("dve", tmp3[:, :, :], cumi[:, :].unsqueeze(1).to_broadcast([128, NSLOT, 32]),
                   siota.unsqueeze(2).to_broadcast([128, NSLOT, 32]), ALU.is_le, [cumi, mc], [tmp3])
                red(eall[:, :], tmp3[:, :, :], [tmp3], [eall])
                tt("dve", onehot[:, :, :], eiota.unsqueeze(1).to_broadcast([128, NSLOT, 32]),
                   eall[:, :].unsqueeze(2).to_broadcast([128, NSLOT, 32]), ALU.is_equal, [mc, eall], [onehot])
                stt(idxwf[:, :, :], eall[:, :].unsqueeze(2).to_broadcast([128, NSLOT, 8]), 1024.0,
                    wrow.unsqueeze(1).to_broadcast([128, NSLOT, 8]), ALU.mult, ALU.add, [eall, mc], [idxwf])
                cp("dve", idxw[:, :], idxwf[:, :, :].rearrange("p s c -> p (s c)"), [idxwf], [idxw])
                for j in range(17):
                    nt = 128 if j < 16 else NS
                    bank = PS[1] if j < 16 else PS[2]
                    rk = bank[0:nt, (j % 16) * 32:(j % 16) * 32 + 32]
                    stt(vt[0][0:nt, :], rk, 1.0, st512[0:nt, :], ALU.add, ALU.add, [bank, st512], [vt[0]])
                    tt("dve", vt[1][0:nt, :], vt[0][0:nt, :], msel[0:nt, j, :], ALU.mult, [vt[0], msel], [vt[1]])
                    P.op("dve", lambda h, nt=nt: h.max(out=p8[0:nt, :], in_=vt[1][0:nt, :]), [vt[1].r], [p8.r])
                    ts("dve", p4f[0:nt, :], p8[0:nt, 0:4], -1.0, None, ALU.add, None, [p8], [p4f])
                    cp("dve", pos4i[0:nt, j * 4:j * 4 + 4], p4f[0:nt, :], [p4f], [pos4i])
                for j in range(17):
                    nt = 128 if j < 16 else NS
                    for k in range(4):
                        P.dmaf("pool", "g", lambda h, nt=nt, j=j, k=k: h.indirect_dma_start(
                            out=inv_d[:, :], out_offset=IO(ap=pos4i[0:nt, j * 4 + k:j * 4 + k + 1], axis=0), in_=tokid[0:nt, j:j + 1], in_offset=None,
                            bounds_check=regs["row"], oob_is_err=False), [pos4i.r, tokid.r, Rinv], [])
                P.barrier()
                for q6 in range(9):
                    dma("sp", "m", idxall[:, q6 * 18:(q6 + 1) * 18],
                        inv_d[q6 * 2304:(q6 + 1) * 2304, :].rearrange("(s p) o -> p (s o)", p=128), writes=[idxall],
                        allow_slow_non_contiguous=True)
            P.barrier()

            with contextlib.ExitStack() as s4b:
                wgu = [sbuf(s4b, f"wgu{i}", [128, 8, 2048], BF16) for i in range(2)]
                wdn = [sbuf(s4b, f"wdn{i}", [128, 8, D], BF16) for i in range(2)]
                xg = [sbuf(s4b, f"xg{i}", [128, QT, D], BF16) for i in range(2)]
                hgT = [sbuf(s4b, f"hgT{i}", [128, 8, SR], BF16) for i in range(2)]
                gcomb = [sbuf(s4b, f"gcomb{i}", [128, QT, 32]) for i in range(2)]
                bias_s = [sbuf(s4b, f"bias_s{i}", [128, 16]) for i in range(2)]
                gate_s = [sbuf(s4b, f"gate_s{i}", [128, QT]) for i in range(2)]
                btmp = sbuf(s4b, "btmp", [128, 16, 32]); gtmp = sbuf(s4b, "gtmp", [128, QT, 32])
                gsb = [sbuf(s4b, f"gsb{i}", [128, SR]) for i in range(2)]
                ssb = [sbuf(s4b, f"ssb{i}", [128, SR]) for i in range(2)]
                usb = [sbuf(s4b, f"usb{i}", [128, SR]) for i in range(2)]
                actT = [sbuf(s4b, f"actT{i}", [128, 8, SR], BF16) for i in range(2)]
                res_sb = [sbuf(s4b, f"res_sb{i}", [128, D]) for i in range(3)]

                order = []
                for i in range(NSLOT // 3):
                    order += [i, 2 * (NSLOT // 3) + i]
                order += list(range(NSLOT // 3, 2 * (NSLOT // 3)))
                assert sorted(order) == list(range(NSLOT))

                def tok_loads(i):
                    s = order[i]
                    par = i % 2
                    for q in range(QT):
                        gather(f"kv{par}", xg[par][:, q, :], hn_d[:, :], idxall[:, s * QT + q:s * QT + q + 1], [idxall], [xg[par]], "tok")
                    for q in range(QT):
                        gather(f"kv{par}", gcomb[par][:, q, :], comb_d[:, :], idxall[:, s * QT + q:s * QT + q + 1], [idxall], [gcomb[par]], "tok")

                def wgu_loads(i):
                    s = order[i]
                    par = i % 2
                    for c in range(8):
                        gather(f"w{par}", wgu[par][:, c, :], w_gu2[:, :], idxw[:, s * 8 + c:s * 8 + c + 1], [idxw], [wgu[par]], "w")

                def wdn_loads(i):
                    s = order[i]
                    par = i % 2
                    for c in range(8):
                        gather(f"wd{par}", wdn[par][:, c, :], w_dn2[:, :], idxw[:, s * 8 + c:s * 8 + c + 1], [idxw], [wdn[par]], "w")

                def slot_transposes(i):
                    par = i % 2
                    for q in range(QT):
                        bank = PS[6 + q % 2]
                        bb = bank[:].bitcast(BF16)
                        for c in range(8):
                            tr(bb[:, c * 128:(c + 1) * 128], xg[par][:, q, c * 128:(c + 1) * 128], ident_b[:, :], [xg[par], ident_b], [bank])
                        cp("act" if q % 2 == 0 else "dve", hgT[par][:, :, q * 128:(q + 1) * 128], bb.rearrange("p (c t) -> p c t", c=8),
                           [bank], [hgT[par]])

                tok_loads(0); wgu_loads(0); wdn_loads(0)
                tok_loads(1) if NSLOT > 1 else None
                wgu_loads(1); wdn_loads(1)
                slot_transposes(0)
                for si in range(NSLOT):
                    s = order[si]
                    par = si % 2
                    tt("dve", btmp[:, :, :], bguT[:, :, :].rearrange("p e c -> p c e"),
                       onehot[:, s, :].unsqueeze(1).to_broadcast([128, 16, 32]), ALU.mult, [bguT, onehot], [btmp])
                    red(bias_s[par][:, :], btmp[:, :, :], [btmp], [bias_s[par]])
                    tt("dve", gtmp[:, :, :], gcomb[par][:, :, :], onehot[:, s, :].unsqueeze(1).to_broadcast([128, QT, 32]), ALU.mult,
                       [gcomb[par], onehot], [gtmp])
                    red(gate_s[par][:, :], gtmp[:, :, :], [gtmp], [gate_s[par]])
                    if si + 2 < NSLOT:
                        tok_loads(si + 2)
                    aT = actT[par]
                    for c in range(8):
                        db = c % 2
                        bg = PS[db * 2]; bu = PS[db * 2 + 1]
                        for k in range(8):
                            mm(bg[:, 0:SR], wgu[par][:, k, c * 128:(c + 1) * 128], hgT[par][:, k, :], k == 0, k == 7, [wgu[par], hgT[par]], [bg])
                        for k in range(8):
                            mm(bu[:, 0:SR], wgu[par][:, k, 1024 + c * 128:1024 + (c + 1) * 128], hgT[par][:, k, :], k == 0, k == 7,
                               [wgu[par], hgT[par]], [bu])
                        ts("dve", gsb[db][:, :], bg[:, 0:SR], bias_s[par][:, c:c + 1], 7.0, ALU.add, ALU.min, [bg, bias_s[par]], [gsb[db]])
                        act(ssb[db][:, :], gsb[db][:, :], AF.Sigmoid, [gsb[db]], [ssb[db]], scale=1.702)
                        ts("dve", usb[db][:, :], bu[:, 0:SR], bias_s[par][:, 8 + c:9 + c], -6.0, ALU.add, ALU.max, [bu, bias_s[par]], [usb[db]])
                        stt(usb[db][:, :], usb[db][:, :], 8.0, gsb[db][:, :], ALU.min, ALU.mult, [usb[db], gsb[db]], [usb[db]])
                        tt("dve", aT[:, c, :], usb[db][:, :], ssb[db][:, :], ALU.mult, [usb[db], ssb[db]], [aT])
                    if si + 2 < NSLOT:
                        wgu_loads(si + 2)
                    if si + 1 < NSLOT:
                        slot_transposes(si + 1)
                    for q in range(QT):
                        ri = (si * QT + q) % 3
                        rs = res_sb[ri]
                        for hf in range(2):
                            bank = PS[4 + hf]
                            for k in range(8):
                                mm(bank[:, :], aT[:, k, q * 128:(q + 1) * 128], wdn[par][:, k, hf * 512:(hf + 1) * 512], k == 0, k == 7,
                                   [aT, wdn[par]], [bank])
                            if hf == 0:
                                act(rs[:, 0:512], bank[:, :], AF.Copy, [bank, gate_s[par]], [rs], scale=gate_s[par][:, q:q + 1])
                            else:
                                ts("dve", rs[:, 512:1024], bank[:, :], gate_s[par][:, q:q + 1], None, ALU.mult, None, [bank, gate_s[par]], [rs])
                        dma("sp", f"r{ri}", res_d[(s * QT + q) * 128:(s * QT + q + 1) * 128, :], rs[:, :], reads=[rs])
                    if si + 2 < NSLOT:
                        wdn_loads(si + 2)
            P.barrier()

            with contextlib.ExitStack() as s4c:
                NCB = 4
                yb = [sbuf(s4c, f"yb{i}", [128, D]) for i in range(NCB)]
                rg = [[sbuf(s4c, f"rg{i}_{k}", [128, D]) for k in range(4)] for i in range(NCB)]
                csem = ("kv0", "kv1", "w0", "w1")
                xsem = ("x0", "x1", "o0", "o1")

                def comb_loads(j):
                    nt = 128 if j < 16 else NS
                    par = j % NCB
                    dma("sp", xsem[par], yb[par][0:nt, :], x1scr[j * 128:j * 128 + nt, :], writes=[yb[par]])
                    for k in range(4):
                        gather(csem[par], rg[par][k][0:nt, :], res_d[:, :], pos4i[0:nt, j * 4 + k:j * 4 + k + 1], [pos4i], [rg[par][k]], "row")
                for j in range(NCB - 1):
                    comb_loads(j)
                for j in range(17):
                    nt = 128 if j < 16 else NS
                    par = j % NCB
                    if j + NCB - 1 < 17:
                        comb_loads(j + NCB - 1)
                    for k in range(4):
                        tt("dve", yb[par][0:nt, :], yb[par][0:nt, :], rg[par][k][0:nt, :], ALU.add, [yb[par], rg[par][k]], [yb[par]])
                    if j < 16:
                        dma("sp", "y", y_p[j * 128:(j + 1) * 128, :], yb[par][:, :], reads=[yb[par]], is_output=True)
                    else:
                        dma("sp", "y", y_s[:, :], yb[par][0:NS, :], reads=[yb[par]], is_output=True)
        P.finish("sp")
        with block_cm as block:
            P.emit(block)
    return nc


def _host_constants():
    k = np.arange(128)[:, None]
    q = np.arange(128)[None, :]
    cur = (k <= q).astype(np.float32)
    prev = (k >= q).astype(np.float32)
    maskA = np.concatenate([prev, cur, prev, cur], axis=1)
    maskH0 = np.concatenate([0 * prev, cur, 0 * prev, cur], axis=1)
    mS = np.zeros((128, 2, 9, 3, 4), np.float32)
    for t9 in range(8):
        mS[:, :, t9, :, t9 % 4] = 1.0
    for t in range(4):
        mS[t:, :, 8, :, t] = 1.0
    maskS = mS.reshape(128, 216)
    w4 = np.zeros((4, 4), np.float32)
    for tk in range(4):
        for t in range(4):
            w4[tk, t] = (1.0 if tk <= t else 0.0) + (2.0 if tk == t else 0.0)
    mN = np.kron(np.eye(16, dtype=np.float32), w4)
    maskN = np.tile(mN, (1, 6))
    tril4 = np.zeros((4, 4), np.float32)
    for s in range(4):
        for t in range(4):
            tril4[s, t] = 1.0 if s <= t else 0.0
    mbd = np.kron(np.eye(16, dtype=np.float32), tril4)
    mask_bd = np.repeat(mbd[:, None, :], 6, axis=1)
    ident = np.eye(128, dtype=np.float32)
    onesAB = np.concatenate([np.ones((128, 64)), np.zeros((128, 64)), np.ones((128, 64))], axis=1).astype(np.float32)
    return dict(maskA=maskA, maskH0=maskH0, maskS=maskS, maskN=maskN, mask_bd=mask_bd, ident=ident, onesAB=onesAB)


def _rope_tables(pos):
    half = 8
    inv_freq = np.power(np.float32(500000.0), -np.arange(half, dtype=np.float32) / np.float32(half)).astype(np.float32)
    ang = pos.astype(np.float32)[:, None] * inv_freq[None, :]
    c = np.cos(ang).astype(np.float32)
    s = np.sin(ang).astype(np.float32)
    return np.concatenate([c, c], axis=1), np.concatenate([-s, s], axis=1)


_NC_CACHE = {}


def kernel(x_prompt, x_sample, mem_prompt, cache_win_k, cache_win_v, cache_mem_k, cache_mem_v,
           norm1_g, w_in, gv_a, w_s, b_s, gq_b, gk_b, gq_m, gk_m, mem_norm_g, w_mem_kv, w_out,
           norm2_g, w_router, b_router, w_gate_up, b_gate_up, w_down, b_down):
    f = lambda a: np.ascontiguousarray(np.asarray(a, dtype=np.float32))
    x_prompt, x_sample, mem_prompt = f(x_prompt), f(x_sample), f(mem_prompt)
    cwk, cwv = f(cache_win_k)[0], f(cache_win_v)[0]
    cmk, cmv = f(cache_mem_k)[0], f(cache_mem_v)[0]
    C = _host_constants()
    gT = lambda g: f(np.asarray(g)[0].reshape(8, 128).T)
    w_s0 = np.asarray(w_s, np.float32)[0]
    b_s0 = np.asarray(b_s, np.float32)[0]
    shared = {
        "norm1_gT": gT(norm1_g), "norm2_gT": gT(norm2_g), "memn_gT": gT(mem_norm_g),
        "w_in": f(w_in)[0], "w_mem_kv": f(w_mem_kv)[0], "w_out": f(w_out)[0],
        "gv_a": f(gv_a), "gqk": f(np.concatenate([np.tile(np.asarray(gq_b)[0], 6), np.tile(np.asarray(gk_b)[0], 6)])[None]),
        "gqm": f(np.tile(np.asarray(gq_m)[0], 4)[None]), "gkm": f(np.tile(np.asarray(gk_m)[0], 4)[None]),
        "w_sT": f(w_s0.transpose(2, 0, 1)),
        "bsB": f(np.repeat(b_s0.T[:, :, None], 64, axis=2).reshape(128, 384)),
        "maskA": C["maskA"], "maskS": C["maskS"], "maskN": C["maskN"], "mask_bd": C["mask_bd"],
        "ident": C["ident"], "onesAB": C["onesAB"],
        "w_router": f(w_router)[0], "b_router": f(b_router),
        "w_gate_up": f(w_gate_up)[0], "w_down": f(w_down)[0],
        "bguT": f(np.asarray(b_gate_up, np.float32)[0].reshape(NEXP, 16, 128).transpose(2, 0, 1)),
        "b_down": f(b_down)[0],
        "norm2_row": f(norm2_g),
    }
    pp = np.arange(128, dtype=np.float32)[:, None]
    shared["mconst"] = f(np.concatenate([np.tile(np.arange(54, dtype=np.float32)[None], (128, 1)),
                                         np.tile(np.arange(32, dtype=np.float32)[None], (128, 1)),
                                         np.arange(8, dtype=np.float32)[None] * 128 + pp,
                                         np.arange(17, dtype=np.float32)[None] * 128 + pp], axis=1))
    shared["triu"] = f(np.triu(np.ones((128, 128), np.float32), 1))
    bd = np.zeros((64, 6, 64), np.float32)
    for b in range(16):
        bd[4 * b:4 * b + 4, :, 4 * b:4 * b + 4] = w_s0[:, :4, :4].transpose(2, 0, 1)
    shared["bd_ws"] = bd
    shared["bsS"] = f(np.tile(np.repeat(b_s0[:, :4].T[:, :, None], 64, axis=2).reshape(4, 384), (16, 1)))
    rows = np.zeros((9, 128), np.int64)
    for t in range(4):
        rows[t] = t + 16 * np.arange(128)
        rows[4 + t] = 1536 + t + 4 * np.arange(128)
    rows[8] = 1920 + np.arange(128)

    in_maps = []
    for c in range(NCORES):
        b, g = c // 4, c % 4
        s0 = g * SEG
        m = dict(shared)
        m["xp"] = f(x_prompt[b, s0:s0 + SEG])
        m["xh"] = f(x_prompt[b, s0 - SEG:s0]) if g > 0 else np.zeros((SEG, D), np.float32)
        m["xs"] = f(x_sample[16 * c:16 * c + 16].reshape(NS, D))
        m["mem"] = f(mem_prompt[b])
        kk = cwk[16 * c:16 * c + 16][:, rows]
        m["cwkT"] = f(kk.reshape(16, 9, 128, 3, 128).transpose(0, 4, 3, 1, 2).reshape(16, 128, 3, 1152))
        vv = cwv[16 * c:16 * c + 16][:, rows]
        m["cwv"] = f(vv.transpose(0, 2, 1, 3, 4).reshape(16, 128, 9, 384))
        mk = cmk[16 * c:16 * c + 16]
        m["cmkT"] = f(mk.reshape(16, 256, 2, 128).transpose(3, 0, 2, 1))
        mv = cmv[16 * c:16 * c + 16]
        m["cmv"] = f(mv.reshape(16, 2, 128, 256).transpose(2, 0, 1, 3))
        pos = np.concatenate([s0 + np.arange(SEG), s0 - SEG + np.arange(SEG), 8192 + (np.arange(NS) % 4)])
        cs, sn = _rope_tables(pos)
        m["rope_cs"], m["rope_sn"] = cs, sn
        m["maskH"] = C["maskA"] if g > 0 else C["maskH0"]
        in_maps.append(m)

    if "nc" not in _NC_CACHE:
        _NC_CACHE["nc"] = build_program()
    nc = _NC_CACHE["nc"]
    res = run_bass_kernel_spmd(nc, in_maps, core_ids=list(range(NCORES)))
    R = res.results
    y_prompt = np.stack([np.concatenate([R[4 * b + g]["y_p"] for g in range(4)], axis=0) for b in range(2)])
    y_sample = np.concatenate([R[c]["y_s"].reshape(16, 4, D) for c in range(NCORES)], axis=0)
    wk = np.stack([R[4 * b + 3]["wk_p"] for b in range(2)]).reshape(1, 2, SEG, 6, 64)
    wv = np.stack([R[4 * b + 3]["wv_p"] for b in range(2)]).reshape(1, 2, SEG, 6, 64)
    mk = np.stack([R[4 * b]["mk_p"] for b in range(2)]).reshape(1, 2, 256, 4, 64)
    mv = np.stack([R[4 * b]["mv_p"] for b in range(2)]).reshape(1, 2, 256, 4, 64)
    wks = np.concatenate([R[c]["wk_s"].reshape(16, 4, 6, 64) for c in range(NCORES)], axis=0)[None]
    wvs = np.concatenate([R[c]["wv_s"].reshape(16, 4, 6, 64) for c in range(NCORES)], axis=0)[None]
    gvs = np.concatenate([R[c]["gv_s"].reshape(16, 4, 384) for c in range(NCORES)], axis=0)[None]
    return (y_prompt.astype(np.float32), y_sample.astype(np.float32), wk.astype(np.float32), wv.astype(np.float32),
            mk.astype(np.float32), mv.astype(np.float32), wks.astype(np.float32), wvs.astype(np.float32), gvs.astype(np.float32))
```

```python
import contextlib
import numpy as np
import concourse.bass as bass
import concourse.mybir as mybir
from concourse.bass_utils import run_bass_kernel_spmd

F32 = mybir.dt.float32
BF16 = mybir.dt.bfloat16
I32 = mybir.dt.int32
AF = mybir.ActivationFunctionType
ALU = mybir.AluOpType
AX = mybir.AxisListType

NCORES = 8
D = 1024
SEG = 2048
NS = 64
NTOK = SEG + NS
NEXP = 32
EPS = 1e-6
SCALE = 0.125
KSTOP = 9


class Res:
    __slots__ = ("name", "w", "r", "excl")

    def __init__(self, name="", excl=False):
        self.name = name
        self.w = None
        self.r = []
        self.excl = excl


class Prog:
    ENGS = ("pe", "act", "dve", "pool", "sp")

    def __init__(self, nc):
        self.nc = nc
        self.ops = {e: [] for e in self.ENGS}
        self.cnt = {e: 0 for e in self.ENGS}
        self.sems = {}
        self.semval = {}
        self.known = {e: {} for e in self.ENGS}
        self.sealed = {}
        self.out_tokens = []

    def add_sem(self, key, handle):
        self.sems[key] = handle
        self.semval[key] = 0

    def _need(self, eng, tok, same_ok):
        if tok is None:
            return
        key, val = tok
        if key in self.cnt:
            if key == eng and same_ok:
                return
        else:
            if self.known[eng].get(key, 0) >= val:
                return
            val = max(val, self.semval[key])
            self.sealed[key] = True
        if self.known[eng].get(key, 0) >= val:
            return
        self.known[eng][key] = val
        sem = self.sems[key]
        self.ops[eng].append(lambda h, sem=sem, val=val: h.wait_ge(sem, val))

    def _deps(self, eng, reads, writes, is_dma=False, semkey=None):
        same_ok = (not is_dma) and eng != "pool"
        need = {}

        def add(tok, ok):
            if tok is None:
                return
            key, val = tok
            if ok and key == eng:
                return
            if need.get(key, 0) < val:
                need[key] = val
        for r in reads:
            add(r.w, False)
        for w in writes:
            if not (is_dma and w.w is not None and w.w[0] == semkey):
                add(w.w, same_ok)
            for t in w.r:
                add(t, same_ok)
        for key, val in need.items():
            self._need(eng, (key, val), same_ok=False)

    def _commit(self, tok, reads, writes):
        for r in reads:
            r.r.append(tok)
        for w in writes:
            w.w = tok
            w.r = []

    @staticmethod
    def _split(reads, writes):
        writes = list(writes) + [r for r in reads if r.excl and r not in writes]
        reads = [r for r in reads if not r.excl]
        return reads, writes

    def op(self, eng, fn, reads=(), writes=()):
        reads, writes = self._split(reads, writes)
        self._deps(eng, reads, writes)
        self.cnt[eng] += 1
        tok = (eng, self.cnt[eng])
        sem = self.sems[eng]
        self.ops[eng].append(lambda h, fn=fn, sem=sem: fn(h).then_inc(sem, 1))
        self._commit(tok, reads, writes)
        return tok

    def dma(self, eng, semkey, out, in_, reads=(), writes=(), is_output=False, **kw):
        self._deps(eng, reads, writes, is_dma=True, semkey=semkey)
        if self.sealed.get(semkey) and self.semval[semkey] > 0:
            self._need(eng, (semkey, self.semval[semkey]), same_ok=False)
        self.sealed[semkey] = False
        self.semval[semkey] += 16
        tok = (semkey, self.semval[semkey])
        sem = self.sems[semkey]
        self.ops[eng].append(
            lambda h, out=out, in_=in_, sem=sem, kw=kw: h.dma_start(out=out, in_=in_, **kw).then_inc(sem, 16))
        self._commit(tok, reads, writes)
        if is_output:
            self.out_tokens.append(tok)
        return tok

    def dmaf(self, eng, semkey, fn, reads=(), writes=(), is_output=False):
        self._deps(eng, reads, writes, is_dma=True, semkey=semkey)
        if self.sealed.get(semkey) and self.semval[semkey] > 0:
            self._need(eng, (semkey, self.semval[semkey]), same_ok=False)
        self.sealed[semkey] = False
        self.semval[semkey] += 16
        tok = (semkey, self.semval[semkey])
        sem = self.sems[semkey]
        self.ops[eng].append(lambda h, fn=fn, sem=sem: fn(h).then_inc(sem, 16))
        self._commit(tok, reads, writes)
        if is_output:
            self.out_tokens.append(tok)
        return tok

    def barrier(self):
        for e in self.ENGS:
            for f in self.ENGS:
                if (f != e or e == "pool") and self.cnt[f] > 0:
                    self._need(e, (f, self.cnt[f]), same_ok=False)
            for k, v in self.semval.items():
                if v > 0:
                    self._need(e, (k, v), same_ok=False)

    def finish(self, eng="sp"):
        last = {}
        for key, val in self.out_tokens:
            last[key] = max(last.get(key, 0), val)
        for key, val in last.items():
            self._need(eng, (key, val), same_ok=False)

    def emit(self, block):
        hmap = {"pe": "tensor", "act": "scalar", "dve": "vector", "pool": "gpsimd", "sp": "sync"}
        for e in self.ENGS:
            ops = self.ops[e]
            if not ops:
                continue

            def body(h, ops=ops):
                for f in ops:
                    f(h)
            getattr(block, hmap[e])(body)


class B:
    def __init__(self, t, name, excl=False):
        self.t = t
        self.r = Res(name, excl)

    def __getitem__(self, k):
        return self.t[k]


def build_program():
    nc = bass.Bass("TRN2", target_bir_lowering=False)

    def din(name, shape):
        return nc.dram_tensor(name, list(shape), F32, kind="ExternalInput").ap()

    def dout(name, shape):
        return nc.dram_tensor(name, list(shape), F32, kind="ExternalOutput").ap()

    xp = din("xp", [SEG, D]); xh = din("xh", [SEG, D]); xs_d = din("xs", [NS, D]); mem_d = din("mem", [256, D])
    cwkT = din("cwkT", [16, 128, 3, 1152]); cwv = din("cwv", [16, 128, 9, 384])
    cmkT = din("cmkT", [128, 16, 2, 256]); cmv = din("cmv", [128, 16, 2, 256])
    norm1_g = din("norm1_gT", [128, 8]); norm2_g = din("norm2_gT", [128, 8]); memn_g = din("memn_gT", [128, 8])
    w_in = din("w_in", [D, 2176]); w_mem = din("w_mem_kv", [D, 512]); w_out = din("w_out", [D, D])
    gv_a = din("gv_a", [1, 384]); gqk = din("gqk", [1, 768]); gqm = din("gqm", [1, 256]); gkm = din("gkm", [1, 256])
    w_sT = din("w_sT", [128, 6, 128]); bsB_d = din("bsB", [128, 384])
    bd_ws = din("bd_ws", [64, 6, 64]); bsS_d = din("bsS", [64, 384]); mask_bd = din("mask_bd", [64, 6, 64])
    rope_cs = din("rope_cs", [2 * SEG + NS, 16]); rope_sn = din("rope_sn", [2 * SEG + NS, 16])
    maskA_d = din("maskA", [128, 512]); maskH_d = din("maskH", [128, 512])
    maskS_d = din("maskS", [128, 216]); maskN_d = din("maskN", [64, 384])
    ident_d = din("ident", [128, 128]); onesAB_d = din("onesAB", [128, 192])
    w_router = din("w_router", [D, 32]); b_router = din("b_router", [1, 32])
    w_gu = din("w_gate_up", [NEXP, D, 2048]); w_dn = din("w_down", [NEXP, D, D])
    bguT_d = din("bguT", [128, NEXP, 16]); b_dn = din("b_down", [NEXP, D])

    mconst_d = din("mconst", [128, 111]); triu_d = din("triu", [128, 128]); norm2_row = din("norm2_row", [1, D])

    vscr = nc.dram_tensor("vscr", [2 * SEG, 384], F32, kind="Internal").ap()
    x1scr = nc.dram_tensor("x1scr", [NTOK, D], F32, kind="Internal").ap()
    hn_d = nc.dram_tensor("hn_d", [NTOK + 128, D], BF16, kind="Internal").ap()
    comb_d = nc.dram_tensor("comb_d", [NTOK + 128, 32], F32, kind="Internal").ap()
    inv_d = nc.dram_tensor("inv_d", [54 * 384, 1], I32, kind="Internal").ap()
    res_d = nc.dram_tensor("res_d", [54 * 384, D], F32, kind="Internal").ap()

    y_p = dout("y_p", [SEG, D]); y_s = dout("y_s", [NS, D])
    wk_p = dout("wk_p", [SEG, 384]); wv_p = dout("wv_p", [SEG, 384])
    mk_p = dout("mk_p", [256, 256]); mv_p = dout("mv_p", [256, 256])
    wk_s = dout("wk_s", [NS, 384]); wv_s = dout("wv_s", [NS, 384]); gv_s = dout("gv_s", [NS, 384])

    with contextlib.ExitStack() as top:
        P = Prog(nc)
        for e in Prog.ENGS:
            P.add_sem(e, top.enter_context(nc.semaphore("s_" + e)))
        for k in ("c", "cp", "x0", "x1", "o0", "o1", "v", "kv0", "kv1", "w0", "w1", "wd0", "wd1", "g", "r0", "r1", "r2", "m", "y"):
            P.add_sem(k, top.enter_context(nc.semaphore("d_" + k)))
        block_cm = nc.Block()

        def sbuf(es, name, shape, dt=F32):
            return B(es.enter_context(nc.sbuf_tensor("sb_" + name, list(shape), dt)), name)

        PS = [B(top.enter_context(nc.psum_tensor(f"ps{i}", [128, 512], F32)), f"ps{i}", excl=True) for i in range(8)]

        def act(out, in_, func, reads, writes, **kw):
            P.op("act", lambda h: h.activation(out=out, in_=in_, func=func, **kw),
                 [b.r for b in reads], [b.r for b in writes])

        def tt(eng, out, in0, in1, op, reads, writes):
            P.op(eng, lambda h: h.tensor_tensor(out=out, in0=in0, in1=in1, op=op),
                 [b.r for b in reads], [b.r for b in writes])

        def ts(eng, out, in0, s1, s2, op0, op1, reads, writes, **kw):
            if op1 is None:
                P.op(eng, lambda h: h.tensor_scalar(out=out, in0=in0, scalar1=s1, scalar2=None, op0=op0, **kw),
                     [b.r for b in reads], [b.r for b in writes])
            else:
                P.op(eng, lambda h: h.tensor_scalar(out=out, in0=in0, scalar1=s1, scalar2=s2, op0=op0, op1=op1, **kw),
                     [b.r for b in reads], [b.r for b in writes])

        def stt(out, in0, scalar, in1, op0, op1, reads, writes, **kw):
            P.op("dve", lambda h: h.scalar_tensor_tensor(out=out, in0=in0, scalar=scalar, in1=in1, op0=op0, op1=op1, **kw),
                 [b.r for b in reads], [b.r for b in writes])

        def cp(eng, out, in_, reads, writes):
            if eng == "act":
                act(out, in_, AF.Copy, reads, writes)
            else:
                P.op(eng, lambda h: h.tensor_copy(out, in_), [b.r for b in reads], [b.r for b in writes])

        def mm(out, lhsT, rhs, start, stop, reads, writes):
            P.op("pe", lambda h: h.matmul(out, lhsT=lhsT, rhs=rhs, start=start, stop=stop),
                 [b.r for b in reads], [b.r for b in writes])

        def tr(out, in_, ident, reads, writes):
            P.op("pe", lambda h: h.transpose(out=out, in_=in_, identity=ident),
                 [b.r for b in reads], [b.r for b in writes])

        def recip(out, in_, reads, writes):
            P.op("dve", lambda h: h.reciprocal(out, in_), [b.r for b in reads], [b.r for b in writes])

        def red(out, in_, reads, writes):
            P.op("dve", lambda h: h.tensor_reduce(out=out, in_=in_, axis=AX.X, op=ALU.add),
                 [b.r for b in reads], [b.r for b in writes])

        def dma(eng, sem, out, in_, reads=(), writes=(), **kw):
            P.dma(eng, sem, out, in_, [b.r for b in reads], [b.r for b in writes], **kw)

        def rstd_from_ssq(ssq, std, rstd, n, width, rows):
            act(std[0:rows, 0:n], ssq[0:rows, 0:n], AF.Sqrt, [ssq], [std], scale=1.0 / width, bias=EPS)
            recip(rstd[0:rows, 0:n], std[0:rows, 0:n], [std], [rstd])

        ident_f = sbuf(top, "ident_f", [128, 128]); ident_b = sbuf(top, "ident_b", [128, 128], BF16)
        dma("sp", "c", ident_f[:], ident_d[:, :], writes=[ident_f])
        cp("dve", ident_b[:], ident_f[:], [ident_f], [ident_b])

        with contextlib.ExitStack() as sA:
            oaT = sbuf(sA, "oaT", [128, 3, NTOK], BF16)
            mixB = sbuf(sA, "mixB", [128, 3, NTOK], BF16)
            mixM = sbuf(sA, "mixM", [128, 2, NTOK], BF16)
            with contextlib.ExitStack() as sB:
                qT = sbuf(sB, "qT", [128, 3, SEG], BF16)
                kT = sbuf(sB, "kT", [128, 3, 2 * SEG], BF16)
                qmT = sbuf(sB, "qmT", [128, 2, NTOK], BF16)
                kmT = sbuf(sB, "kmT", [128, 2, 256], BF16)
                vmp = sbuf(sB, "vmp", [128, 2, 2, 192], BF16)
                qTs = sbuf(sB, "qTs", [128, 3, NS], BF16)
                kTs = sbuf(sB, "kTs", [128, 3, NS], BF16)
                vsp = sbuf(sB, "vsp", [64, 3, 192], BF16)
                onesAB = sbuf(sB, "onesAB", [128, 192], BF16)
                dma("pool", "cp", onesAB[:], onesAB_d[:, :], writes=[onesAB])
                P.op("pool", lambda h: h.memset(vmp[:], 0.0), [], [vmp.r])
                P.op("pool", lambda h: h.memset(vsp[:], 0.0), [], [vsp.r])

                with contextlib.ExitStack() as s1:
                    w_in_b = sbuf(s1, "w_in_b", [128, 8, 2176], BF16)
                    w_mem_b = sbuf(s1, "w_mem_b", [128, 8, 512], BF16)
                    for hh in range(2):
                        dma("pool", "cp", w_in_b[:, :, hh * 1088:(hh + 1) * 1088],
                            w_in.rearrange("(c p) n -> p c n", p=128)[:, :, hh * 1088:(hh + 1) * 1088], writes=[w_in_b])
                    dma("pool", "cp", w_mem_b[:], w_mem.rearrange("(c p) n -> p c n", p=128), writes=[w_mem_b])
                    wsT_f = sbuf(s1, "wsT_f", [128, 6, 128]); wsT_b = sbuf(s1, "wsT_b", [128, 6, 128], BF16)
                    mcur_f = sbuf(s1, "mcur_f", [128, 128])
                    dma("sp", "c", wsT_f[:], w_sT[:, :, :], writes=[wsT_f])
                    dma("sp", "c", mcur_f[:], maskA_d[:, 384:512], writes=[mcur_f])
                    tt("dve", wsT_b[:], wsT_f[:], mcur_f[:].unsqueeze(1).to_broadcast([128, 6, 128]), ALU.mult,
                       [wsT_f, mcur_f], [wsT_b])
                    bd_f = sbuf(s1, "bd_f", [64, 6, 64]); mbd_f = sbuf(s1, "mbd_f", [64, 6, 64]); bd_b = sbuf(s1, "bd_b", [64, 6, 64], BF16)
                    dma("sp", "c", bd_f[:], bd_ws[:, :, :], writes=[bd_f])
                    dma("sp", "c", mbd_f[:], mask_bd[:, :, :], writes=[mbd_f])
                    tt("dve", bd_b[:], bd_f[:], mbd_f[:], ALU.mult, [bd_f, mbd_f], [bd_b])
                    bsB = sbuf(s1, "bsB", [128, 384]); bsS = sbuf(s1, "bsS", [64, 384])
                    dma("sp", "c", bsB[:], bsB_d[:, :], writes=[bsB])
                    dma("sp", "c", bsS[:], bsS_d[:, :], writes=[bsS])
                    g1c = sbuf(s1, "g1c", [128, 8]); gmc = sbuf(s1, "gmc", [128, 8])
                    dma("sp", "c", g1c[:], norm1_g[:, :], writes=[g1c])
                    dma("sp", "c", gmc[:], memn_g[:, :], writes=[gmc])
                    gvB = sbuf(s1, "gvB", [128, 384]); gqkB = sbuf(s1, "gqkB", [128, 768])
                    gqmB = sbuf(s1, "gqmB", [128, 256]); gkmB = sbuf(s1, "gkmB", [128, 256])
                    dma("sp", "c", gvB[:], gv_a.partition_broadcast(128), writes=[gvB])
                    dma("sp", "c", gqkB[:], gqk.partition_broadcast(128), writes=[gqkB])
                    dma("sp", "c", gqmB[:], gqm.partition_broadcast(128), writes=[gqmB])
                    dma("sp", "c", gkmB[:], gkm.partition_broadcast(128), writes=[gkmB])

                    class WS:
                        pass
                    wsets = []
                    for par in range(2):
                        w = WS()
                        sfx = f"_{par}"
                        w.xt = sbuf(s1, "xt" + sfx, [128, D]); w.junk = sbuf(s1, "junk" + sfx, [128, D], BF16)
                        w.ssq = sbuf(s1, "ssq" + sfx, [128, 1]); w.std = sbuf(s1, "std" + sfx, [128, 1]); w.rstd = sbuf(s1, "rstd" + sfx, [128, 1])
                        w.xs = sbuf(s1, "xs" + sfx, [128, D], BF16); w.hT = sbuf(s1, "hT" + sfx, [128, 8, 128], BF16)
                        w.u32 = sbuf(s1, "u32" + sfx, [128, 384])
                        w.ssqv = sbuf(s1, "ssqv" + sfx, [128, 1]); w.stdv = sbuf(s1, "stdv" + sfx, [128, 1]); w.rstdv = sbuf(s1, "rstdv" + sfx, [128, 1])
                        w.vn32 = sbuf(s1, "vn32" + sfx, [128, 384]); w.vnb = sbuf(s1, "vnb" + sfx, [128, 384], BF16)
                        w.sqt = sbuf(s1, "sqt" + sfx, [128, 768])
                        w.ssqk = sbuf(s1, "ssqk" + sfx, [128, 12]); w.stdk = sbuf(s1, "stdk" + sfx, [128, 12]); w.rstdk = sbuf(s1, "rstdk" + sfx, [128, 12])
                        w.qk32 = sbuf(s1, "qk32" + sfx, [128, 12, 64])
                        w.rt1 = sbuf(s1, "rt1" + sfx, [128, 12, 16]); w.rt2 = sbuf(s1, "rt2" + sfx, [128, 12, 16])
                        w.qkb = sbuf(s1, "qkb" + sfx, [128, 768], BF16)
                        w.v32 = sbuf(s1, "v32" + sfx, [128, 384])
                        w.ssqm = sbuf(s1, "ssqm" + sfx, [128, 4]); w.stdm = sbuf(s1, "stdm" + sfx, [128, 4]); w.rstdm = sbuf(s1, "rstdm" + sfx, [128, 4])
                        w.qm32 = sbuf(s1, "qm32" + sfx, [128, 4, 64]); w.qmb = sbuf(s1, "qmb" + sfx, [128, 256], BF16)
                        w.g1 = sbuf(s1, "g1" + sfx, [128, 384]); w.oab = sbuf(s1, "oab" + sfx, [128, 384], BF16)
                        w.cs = sbuf(s1, "cs" + sfx, [128, 16]); w.sn = sbuf(s1, "sn" + sfx, [128, 16])
                        wsets.append(w)

                    PSxT = PS[0]
                    psxT_b = PS[0][:].bitcast(BF16)
                    ps7_b = PS[7][:].bitcast(BF16)

                    def load_x(idx):
                        kind, it, nt, src_ap, ropebase = tiles1[idx]
                        par = idx % 2
                        w = wsets[par]
                        dma("sp", f"x{par}", w.xt[0:nt, :], src_ap, writes=[w.xt])
                        if ropebase is not None:
                            dma("sp", f"x{par}", w.cs[0:nt, :], rope_cs[ropebase:ropebase + nt, :], writes=[w.cs])
                            dma("sp", f"x{par}", w.sn[0:nt, :], rope_sn[ropebase:ropebase + nt, :], writes=[w.sn])

                    def norm_hT(w, src_ap, nt, gcol, par):
                        act(w.junk[0:nt, :], w.xt[0:nt, :], AF.Square, [w.xt], [w.junk, w.ssq], accum_out=w.ssq[0:nt, :])
                        rstd_from_ssq(w.ssq, w.std, w.rstd, 1, D, nt)
                        act(w.xs[0:nt, :], w.xt[0:nt, :], AF.Copy, [w.xt, w.rstd], [w.xs], scale=w.rstd[0:nt, :])
                        for c in range(8):
                            tr(psxT_b[:, c * 128:c * 128 + nt], w.xs[0:nt, c * 128:(c + 1) * 128], ident_b[0:nt, 0:nt],
                               [w.xs, ident_b], [PSxT])
                        tt("dve", w.hT[:, :, 0:nt], psxT_b.rearrange("p (c t) -> p c t", c=8)[:, :, 0:nt],
                           gcol[:, 0:8].unsqueeze(2).to_broadcast([128, 8, nt]), ALU.mult, [PSxT, gcol], [w.hT])

                    def proj(w, bank, wt, c0, width, nt):
                        for k in range(8):
                            mm(bank[0:nt, 0:width], w.hT[:, k, 0:nt], wt[:, k, c0:c0 + width], k == 0, k == 7,
                               [w.hT, wt], [bank])

                    def head_norm(w, bank, nh, h0, sq, ssq, std, rstd, dst32, nt, sq_off):
                        act(sq[0:nt, sq_off:sq_off + nh * 64], bank[0:nt, 0:nh * 64], AF.Square, [bank], [sq])

                    def rope(w, h0, nh, nt):
                        q = w.qk32
                        csb = w.cs[0:nt, :].unsqueeze(1).to_broadcast([nt, nh, 16])
                        tt("pool", w.rt1[0:nt, h0:h0 + nh, :], q[0:nt, h0:h0 + nh, 0:16], csb, ALU.mult, [w.qk32, w.cs], [w.rt1])
                        tt("pool", w.rt2[0:nt, h0:h0 + nh, 0:8], q[0:nt, h0:h0 + nh, 8:16],
                           w.sn[0:nt, 0:8].unsqueeze(1).to_broadcast([nt, nh, 8]), ALU.mult, [w.qk32, w.sn], [w.rt2])
                        tt("pool", w.rt2[0:nt, h0:h0 + nh, 8:16], q[0:nt, h0:h0 + nh, 0:8],
                           w.sn[0:nt, 8:16].unsqueeze(1).to_broadcast([nt, nh, 8]), ALU.mult, [w.qk32, w.sn], [w.rt2])
                        tt("pool", q[0:nt, h0:h0 + nh, 0:16], w.rt1[0:nt, h0:h0 + nh, :], w.rt2[0:nt, h0:h0 + nh, :], ALU.add,
                           [w.rt1, w.rt2], [w.qk32])

                    def qk_norm_rope(w, nt, with_q, ropebase):
                        h0 = 0 if with_q else 6
                        nh = 12 - h0
                        if with_q:
                            act(w.sqt[0:nt, 0:384], PS[3][0:nt, 0:384], AF.Square, [PS[3]], [w.sqt])
                        act(w.sqt[0:nt, 384:768], PS[4][0:nt, 0:384], AF.Square, [PS[4]], [w.sqt])
                        red(w.ssqk[0:nt, h0:12], w.sqt[0:nt, h0 * 64:768].rearrange("p (h d) -> p h d", d=64), [w.sqt], [w.ssqk])
                        act(w.stdk[0:nt, h0:12], w.ssqk[0:nt, h0:12], AF.Sqrt, [w.ssqk], [w.stdk], scale=1.0 / 64, bias=EPS)
                        recip(w.rstdk[0:nt, h0:12], w.stdk[0:nt, h0:12], [w.stdk], [w.rstdk])
                        if with_q:
                            tt("dve", w.qk32[0:nt, 0:6, :], PS[3][0:nt, 0:384].rearrange("p (h d) -> p h d", d=64),
                               w.rstdk[0:nt, 0:6].unsqueeze(2).to_broadcast([nt, 6, 64]), ALU.mult, [PS[3], w.rstdk], [w.qk32])
                        tt("dve", w.qk32[0:nt, 6:12, :], PS[4][0:nt, 0:384].rearrange("p (h d) -> p h d", d=64),
                           w.rstdk[0:nt, 6:12].unsqueeze(2).to_broadcast([nt, 6, 64]), ALU.mult, [PS[4], w.rstdk], [w.qk32])
                        tt("pool", w.qk32[0:nt, h0:12, :], w.qk32[0:nt, h0:12, :],
                           gqkB[0:nt, h0 * 64:768].rearrange("p (h d) -> p h d", d=64), ALU.mult, [w.qk32, gqkB], [w.qk32])
                        rope(w, h0, nh, nt)
                        cp("act", w.qkb[0:nt, h0 * 64:768], w.qk32[0:nt, h0:12, :].rearrange("p h d -> p (h d)"), [w.qk32], [w.qkb])

                    front_done = set()

                    def do_front(idx):
                        if idx in front_done or idx >= len(tiles1):
                            return
                        kind_, it_, nt_, src_, rb_ = tiles1[idx]
                        norm_hT(wsets[idx % 2], src_, nt_, gmc if kind_ == "mem" else g1c, idx % 2)
                        front_done.add(idx)

                    tiles1 = [("mem", mt, 128, mem_d[mt * 128:(mt + 1) * 128, :], None) for mt in range(2)]
                    tiles1 += [("halo", it, 128, xh[it * 128:(it + 1) * 128, :], SEG + it * 128) for it in range(16)]
                    tiles1 += [("own", it, 128, xp[it * 128:(it + 1) * 128, :], it * 128) for it in range(16)]
                    tiles1 += [("samp", 0, NS, xs_d[0:NS, :], 2 * SEG)]
                    load_x(0)

                    tcount = 0
                    for mt in range(2):
                        par = tcount % 2
                        load_x(tcount + 1)
                        cur = tcount
                        tcount += 1
                        w = wsets[par]
                        do_front(cur)
                        proj(w, PS[1], w_mem_b, 0, 512, 128)
                        act(w.sqt[:, 0:256], PS[1][:, 0:256], AF.Square, [PS[1]], [w.sqt])
                        red(w.ssqm[:, 0:4], w.sqt[:, 0:256].rearrange("p (h d) -> p h d", d=64), [w.sqt], [w.ssqm])
                        act(w.stdm[:, 0:4], w.ssqm[:, 0:4], AF.Sqrt, [w.ssqm], [w.stdm], scale=1.0 / 64, bias=EPS)
                        recip(w.rstdm[:, 0:4], w.stdm[:, 0:4], [w.stdm], [w.rstdm])
                        tt("dve", w.qm32[:, :, :], PS[1][:, 0:256].rearrange("p (h d) -> p h d", d=64),
                           w.rstdm[:, 0:4].unsqueeze(2).to_broadcast([128, 4, 64]), ALU.mult, [PS[1], w.rstdm], [w.qm32])
                        tt("pool", w.qm32[:, :, :], w.qm32[:, :, :], gkmB[:, :].rearrange("p (h d) -> p h d", d=64), ALU.mult,
                           [w.qm32, gkmB], [w.qm32])
                        dma("sp", f"o{par}", mk_p[mt * 128:(mt + 1) * 128, :], w.qm32[:, :, :].rearrange("p h d -> p (h d)"),
                            reads=[w.qm32], is_output=True)
                        cp("act", w.qmb[:, :], w.qm32[:, :, :].rearrange("p h d -> p (h d)"), [w.qm32], [w.qmb])
                        for pp in range(2):
                            tr(ps7_b[:, pp * 128:(pp + 1) * 128], w.qmb[:, pp * 128:(pp + 1) * 128], ident_b[:, :], [w.qmb, ident_b], [PS[7]])
                        cp("dve", kmT[:, :, mt * 128:(mt + 1) * 128], ps7_b[:, 0:256].rearrange("p (c t) -> p c t", c=2), [PS[7]], [kmT])
                        cp("act", w.v32[:, 0:256], PS[1][:, 256:512], [PS[1]], [w.v32])
                        do_front(cur + 1)
                        dma("sp", f"o{par}", mv_p[mt * 128:(mt + 1) * 128, :], w.v32[:, 0:256], reads=[w.v32], is_output=True)
                        for i in range(2):
                            cp("pool", vmp[:, mt, :, i * 128:i * 128 + 64],
                               w.v32[:, 0:256].rearrange("p (c i d) -> p c i d", c=2, i=2)[:, :, i, :], [w.v32], [vmp])

                    for it in range(16):
                        par = tcount % 2
                        load_x(tcount + 1)
                        cur = tcount
                        tcount += 1
                        w = wsets[par]
                        do_front(cur)
                        proj(w, PS[4], w_in_b, 1152, 384, 128)
                        proj(w, PS[5], w_in_b, 1536, 384, 128)
                        qk_norm_rope(w, 128, False, SEG + it * 128)
                        cp("act", w.v32[:, :], PS[5][:, 0:384], [PS[5]], [w.v32])
                        do_front(cur + 1)
                        for pp in range(3):
                            tr(ps7_b[:, (3 + pp) * 128:(4 + pp) * 128], w.qkb[:, 384 + pp * 128:384 + (pp + 1) * 128], ident_b[:, :],
                               [w.qkb, ident_b], [PS[7]])
                        cp("act", kT[:, :, it * 128:(it + 1) * 128], ps7_b[:, 384:768].rearrange("p (c t) -> p c t", c=3), [PS[7]], [kT])
                        dma("sp", f"o{par}", vscr[it * 128:(it + 1) * 128, :], w.v32[:, :], reads=[w.v32])

                    tiles = [("own", it, 128) for it in range(16)] + [("samp", 0, NS)]
                    for kind, it, nt in tiles:
                        par = tcount % 2
                        if tcount + 1 < len(tiles1):
                            load_x(tcount + 1)
                        cur = tcount
                        tcount += 1
                        w = wsets[par]
                        samp = kind == "samp"
                        tok0 = SEG if samp else it * 128
                        src = xs_d[0:NS, :] if samp else xp[it * 128:(it + 1) * 128, :]
                        do_front(cur)
                        for j in range(6):
                            proj(w, PS[1 + j], w_in_b, j * 384, 384 if j < 5 else 256, nt)
                        cp("act", w.u32[0:nt, :], PS[1][0:nt, 0:384], [PS[1]], [w.u32])
                        act(w.junk[0:nt, 0:384], PS[2][0:nt, 0:384], AF.Square, [PS[2]], [w.junk, w.ssqv], accum_out=w.ssqv[0:nt, :])
                        rstd_from_ssq(w.ssqv, w.stdv, w.rstdv, 1, 384, nt)
                        stt(w.vn32[0:nt, :], PS[2][0:nt, 0:384], w.rstdv[0:nt, :], gvB[0:nt, :], ALU.mult, ALU.mult,
                            [PS[2], w.rstdv, gvB], [w.vn32])
                        cp("pool", w.vnb[0:nt, :], w.vn32[0:nt, :], [w.vn32], [w.vnb])
                        if samp:
                            dma("sp", f"o{par}", gv_s[:, :], w.vn32[0:nt, :], reads=[w.vn32], is_output=True)
                        ropebase = 2 * SEG if samp else it * 128
                        qk_norm_rope(w, nt, True, ropebase)
                        if samp:
                            dma("sp", f"o{par}", wk_s[:, :], w.qk32[0:nt, 6:12, :].rearrange("p h d -> p (h d)"), reads=[w.qk32], is_output=True)
                        else:
                            dma("sp", f"o{par}", wk_p[it * 128:(it + 1) * 128, :], w.qk32[0:nt, 6:12, :].rearrange("p h d -> p (h d)"),
                                reads=[w.qk32], is_output=True)
                        cp("act", w.v32[0:nt, :], PS[5][0:nt, 0:384], [PS[5]], [w.v32])
                        if samp:
                            dma("sp", f"o{par}", wv_s[:, :], w.v32[0:nt, :], reads=[w.v32], is_output=True)
                            for i in range(2):
                                cp("pool", vsp[0:nt, :, i * 128:i * 128 + 64],
                                   w.v32[0:nt, :].rearrange("p (c i d) -> p c i d", c=3, i=2)[:, :, i, :], [w.v32], [vsp])
                        else:
                            dma("sp", f"o{par}", wv_p[it * 128:(it + 1) * 128, :], w.v32[0:nt, :], reads=[w.v32], is_output=True)
                            dma("sp", f"o{par}", vscr[SEG + it * 128:SEG + (it + 1) * 128, :], w.v32[0:nt, :], reads=[w.v32])
                        act(w.sqt[0:nt, 0:256], PS[6][0:nt, 0:256], AF.Square, [PS[6]], [w.sqt])
                        red(w.ssqm[0:nt, 0:4], w.sqt[0:nt, 0:256].rearrange("p (h d) -> p h d", d=64), [w.sqt], [w.ssqm])
                        act(w.stdm[0:nt, 0:4], w.ssqm[0:nt, 0:4], AF.Sqrt, [w.ssqm], [w.stdm], scale=1.0 / 64, bias=EPS)
                        recip(w.rstdm[0:nt, 0:4], w.stdm[0:nt, 0:4], [w.stdm], [w.rstdm])
                        tt("dve", w.qm32[0:nt, :, :], PS[6][0:nt, 0:256].rearrange("p (h d) -> p h d", d=64),
                           w.rstdm[0:nt, 0:4].unsqueeze(2).to_broadcast([nt, 4, 64]), ALU.mult, [PS[6], w.rstdm], [w.qm32])
                        tt("pool", w.qmb[0:nt, :].rearrange("p (h d) -> p h d", d=64), w.qm32[0:nt, :, :],
                           gqmB[0:nt, :].rearrange("p (h d) -> p h d", d=64), ALU.mult, [w.qm32, gqmB], [w.qmb])
                        do_front(cur + 1)
                        for pp in range(3):
                            tr(ps7_b[:, pp * 128:pp * 128 + nt], w.qkb[0:nt, pp * 128:(pp + 1) * 128], ident_b[0:nt, 0:nt], [w.qkb, ident_b], [PS[7]])
                        for pp in range(3):
                            tr(ps7_b[:, (3 + pp) * 128:(3 + pp) * 128 + nt], w.qkb[0:nt, 384 + pp * 128:384 + (pp + 1) * 128], ident_b[0:nt, 0:nt],
                               [w.qkb, ident_b], [PS[7]])
                        for pp in range(2):
                            tr(ps7_b[:, (6 + pp) * 128:(6 + pp) * 128 + nt], w.qmb[0:nt, pp * 128:(pp + 1) * 128], ident_b[0:nt, 0:nt],
                               [w.qmb, ident_b], [PS[7]])
                        p7v = ps7_b.rearrange("p (c t) -> p c t", c=8)
                        if samp:
                            cp("dve", qTs[:, :, :], p7v[:, 0:3, 0:nt], [PS[7]], [qTs])
                            cp("act", kTs[:, :, :], p7v[:, 3:6, 0:nt], [PS[7]], [kTs])
                        else:
                            cp("dve", qT[:, :, it * 128:(it + 1) * 128], p7v[:, 0:3, :], [PS[7]], [qT])
                            cp("act", kT[:, :, SEG + it * 128:SEG + (it + 1) * 128], p7v[:, 3:6, :], [PS[7]], [kT])
                        cp("dve", qmT[:, :, tok0:tok0 + nt], p7v[:, 6:8, 0:nt], [PS[7]], [qmT])
                        for gi in range(6):
                            lhs = bd_b[0:nt, gi, 0:nt] if samp else wsT_b[:, gi, :]
                            mm(PS[0][0:nt, gi * 64:(gi + 1) * 64], lhs, w.vnb[0:nt, gi * 64:(gi + 1) * 64], True, True,
                               [bd_b if samp else wsT_b, w.vnb], [PS[0]])
                        bsrc = bsS if samp else bsB
                        tt("dve", w.g1[0:nt, :], PS[0][0:nt, 0:384], bsrc[0:nt, :], ALU.add, [PS[0], bsrc], [w.g1])
                        tt("pool", w.oab[0:nt, :], w.g1[0:nt, :], w.u32[0:nt, :], ALU.mult, [w.g1, w.u32], [w.oab])
                        for pp in range(3):
                            tr(ps7_b[:, pp * 128:pp * 128 + nt], w.oab[0:nt, pp * 128:(pp + 1) * 128], ident_b[0:nt, 0:nt], [w.oab, ident_b], [PS[7]])
                        cp("act", oaT[:, :, tok0:tok0 + nt], p7v[:, 0:3, 0:nt], [PS[7]], [oaT])

                P.barrier()
                with contextlib.ExitStack() as s2:
                  if KSTOP >= 2:
                    maskA = sbuf(s2, "maskA", [128, 512], BF16); maskH = sbuf(s2, "maskH", [128, 512], BF16)
                    maskS = sbuf(s2, "maskS", [128, 216], BF16); maskN = sbuf(s2, "maskN", [64, 384], BF16)
                    dma("pool", "cp", maskA[:], maskA_d[:, :], writes=[maskA])
                    dma("pool", "cp", maskH[:], maskH_d[:, :], writes=[maskH])
                    dma("pool", "cp", maskS[:], maskS_d[:, :], writes=[maskS])
                    dma("pool", "cp", maskN[:], maskN_d[:, :], writes=[maskN])
                    Pf = [sbuf(s2, f"Pf{i}", [128, 512], BF16) for i in range(2)]
                    Pm = [sbuf(s2, f"Pm{i}", [128, 512], BF16) for i in range(2)]
                    rl = sbuf(s2, "rl", [128, 512]); rl2 = sbuf(s2, "rl2", [128, 512])
                    s2a = contextlib.ExitStack()
                    acc = sbuf(s2a, "acc", [128, 2, 3, SEG])
                    vbuf = sbuf(s2a, "vbuf", [128, 32, 3, 192], BF16)
                    P.op("pool", lambda h: h.memset(vbuf[:], 0.0), [], [vbuf.r])

                    def vload(dil):
                        own = vscr[SEG:2 * SEG, :]
                        halo = vscr[0:SEG, :]
                        if dil == 1:
                            srcs = [(own.rearrange("(n j) f -> j n f", j=128), 0, 16),
                                    (halo[SEG - 128:SEG, :].rearrange("(n j) f -> j n f", j=128), 16, 1)]
                        elif dil == 4:
                            srcs = [(own[n * 512:(n + 1) * 512, :].rearrange("(j r) f -> j r f", r=4), 4 * n, 4) for n in range(4)]
                            srcs += [(halo[SEG - 512:SEG, :].rearrange("(j r) f -> j r f", r=4), 16, 4)]
                        else:
                            srcs = [(own.rearrange("(j r) f -> j r f", r=16), 0, 16),
                                    (halo.rearrange("(j r) f -> j r f", r=16), 16, 16)]
                        for src, t0, ntile in srcs:
                            for i in range(2):
                                for c in range(3):
                                    s = src.rearrange("j t (c i d) -> j t c i d", c=3, i=2)[:, :, c, i, :]
                                    d = vbuf[:, t0:t0 + ntile, c, i * 128:i * 128 + 64]
                                    dma("pool", "v", d, s, writes=[vbuf])

                    def mem_attn(q0s):
                        nonlocal it2
                        for q0 in q0s:
                            for p in range(2):
                                psO = PS[2]; psL = PS[3]
                                k = 0
                                for mt in range(2):
                                    for i in range(2):
                                        sl = it2 % 2; it2 += 1
                                        mm(PS[sl][:, :], kmT[64 * i:64 * i + 64, p, mt * 128:(mt + 1) * 128], qmT[64 * i:64 * i + 64, p, q0:q0 + 512],
                                           True, True, [kmT, qmT], [PS[sl]])
                                        act(Pf[sl][:, :], PS[sl][:, :], AF.Exp, [PS[sl]], [Pf[sl]], scale=SCALE)
                                        mm(psO[:, :], vmp[:, mt, p, i * 64:i * 64 + 128], Pf[sl][:, :], k == 0, k == 3, [vmp, Pf[sl]], [psO])
                                        mm(psL[:, :], onesAB[:, i * 64:i * 64 + 128], Pf[sl][:, :], k == 0, k == 3, [onesAB, Pf[sl]], [psL])
                                        k += 1
                                recip(rl[:, :], psL[:, :], [psL], [rl])
                                tt("dve", mixM[:, p, q0:q0 + 512], psO[:, :], rl[:, :], ALU.mult, [psO, rl], [mixM])

                    it2 = 0
                    dils = (1, 4, 16)
                    vload(dils[0])
                    for bi, dil in enumerate(dils):
                        nblk = 16 // dil
                        bspan = 128 * dil
                        for r in range(dil):
                            for n in range(nblk):
                                qs = slice(n * bspan + r, (n + 1) * bspan, dil)
                                kc = slice(SEG + n * bspan + r, SEG + (n + 1) * bspan, dil)
                                kp = slice(SEG + (n - 1) * bspan + r, SEG + n * bspan, dil)
                                vt_c = n * dil + r
                                vt_p = (n - 1) * dil + r if n > 0 else 16 + r
                                msk = maskH if n == 0 else maskA
                                for p in range(3):
                                    sl = it2 % 2; it2 += 1
                                    psSi = (PS[sl], PS[4 + sl]); psO = PS[2 + sl]
                                    for kt, kcols in enumerate((kp, kc)):
                                        for i in range(2):
                                            mm(psSi[i][:, kt * 128:(kt + 1) * 128], kT[64 * i:64 * i + 64, p, kcols], qT[64 * i:64 * i + 64, p, qs],
                                               True, True, [kT, qT], [psSi[i]])
                                    for i in range(2):
                                        act(Pf[sl][:, i * 256:(i + 1) * 256], psSi[i][:, 0:256], AF.Exp, [psSi[i]], [Pf[sl]], scale=SCALE)
                                    tt("pool", Pm[sl][:, :], Pf[sl][:, :], msk[:, :], ALU.mult, [Pf[sl], msk], [Pm[sl]])
                                    for part in range(2):
                                        k = 0
                                        for kt, vt in enumerate((vt_p, vt_c)):
                                            for i in range(2):
                                                s = i * 2 + kt
                                                lhs = vbuf[:, vt, p, i * 64:i * 64 + 128] if part == 0 else onesAB[:, i * 64:i * 64 + 128]
                                                mm(psO[:, part * 128:(part + 1) * 128], lhs, Pm[sl][:, s * 128:(s + 1) * 128], k == 0, k == 3,
                                                   [vbuf if part == 0 else onesAB, Pm[sl]], [psO])
                                                k += 1
                                    accv = acc[:, :, p, qs]
                                    pov = psO[:, 0:256].rearrange("p (a q) -> p a q", a=2)
                                    if bi == 0:
                                        cp("dve", accv, pov, [psO], [acc])
                                    else:
                                        tt("dve", accv, pov, accv, ALU.add, [psO, acc], [acc])
                        if bi < 2:
                            vload(dils[bi + 1])
                            mem_attn((0, 512) if bi == 0 else (1024, 1536))
                    for p in range(3):
                        for qi, q0 in enumerate(range(0, SEG, 512)):
                            rlx = rl if qi % 2 == 0 else rl2
                            recip(rlx[:, :], acc[:, 1, p, q0:q0 + 512], [acc], [rlx])
                            tt("pool", mixB[:, p, q0:q0 + 512], acc[:, 0, p, q0:q0 + 512], rlx[:, :], ALU.mult, [acc, rlx], [mixB])
                    P.barrier()
                    s2a.close()

                    kms = sbuf(s2, "kms", [128, 16, 2, 256], BF16)
                    vms = sbuf(s2, "vms", [128, 16, 2, 2, 192], BF16)
                    dma("pool", "cp", kms[:], cmkT[:, :, :, :], writes=[kms])
                    P.op("pool", lambda h: h.memset(vms[:], 0.0), [], [vms.r])
                    for mt in range(2):
                        for i in range(2):
                            for c in range(2):
                                dma("pool", "cp", vms[:, :, mt, c, i * 128:i * 128 + 64],
                                    cmv[:, :, mt, :].rearrange("m b (c i d) -> m b c i d", c=2, i=2)[:, :, c, i, :], writes=[vms])
                    for b in range(16):
                        for p in range(2):
                            for mt in range(2):
                                for i in range(2):
                                    col = (b * 2 + p) * 2 + mt
                                    mm(PS[i][:, col * 4:col * 4 + 4], kms[64 * i:64 * i + 64, b, p, mt * 128:(mt + 1) * 128],
                                       qmT[64 * i:64 * i + 64, p, SEG + 4 * b:SEG + 4 * b + 4], True, True, [kms, qmT], [PS[i]])
                    for i in range(2):
                        act(Pf[0][:, i * 256:(i + 1) * 256], PS[i][:, 0:256], AF.Exp, [PS[i]], [Pf[0]], scale=SCALE)
                    psO = PS[2]
                    for part in range(2):
                        for b in range(16):
                            for p in range(2):
                                k = 0
                                for mt in range(2):
                                    for i in range(2):
                                        col = i * 64 + (b * 2 + p) * 2 + mt
                                        lhs = vms[:, b, mt, p, i * 64:i * 64 + 128] if part == 0 else onesAB[:, i * 64:i * 64 + 128]
                                        mm(psO[:, part * 128 + p * 64 + 4 * b:part * 128 + p * 64 + 4 * b + 4], lhs, Pf[0][:, col * 4:col * 4 + 4],
                                           k == 0, k == 3, [vms if part == 0 else onesAB, Pf[0]], [psO])
                                        k += 1
                    recip(rl[:, 0:128], psO[:, 128:256], [psO], [rl])
                    tt("dve", mixM[:, :, SEG:SEG + NS], psO[:, 0:128].rearrange("p (c q) -> p c q", c=2),
                       rl[:, 0:128].rearrange("p (c q) -> p c q", c=2), ALU.mult, [psO, rl], [mixM])

                    kws = [sbuf(s2, f"kws{i}", [128, 3, 1152], BF16) for i in range(2)]
                    vws = [sbuf(s2, f"vws{i}", [128, 9, 3, 192], BF16) for i in range(2)]
                    for i in range(2):
                        P.op("pool", lambda h, i=i: h.memset(vws[i][:], 0.0), [], [vws[i].r])
                    onew = sbuf(s2, "onew", [128, 2, 3, 64])
                    psNi = (PS[4], PS[3])
                    for p in range(3):
                        for i in range(2):
                            mm(psNi[i][0:64, p * 64:(p + 1) * 64], kTs[64 * i:64 * i + 64, p, :], qTs[64 * i:64 * i + 64, p, :],
                               True, True, [kTs, qTs], [psNi[i]])
                    for i in range(2):
                        act(Pf[1][0:64, i * 192:(i + 1) * 192], psNi[i][0:64, 0:192], AF.Exp, [psNi[i]], [Pf[1]], scale=SCALE)
                    tt("pool", Pm[1][0:64, 0:384], Pf[1][0:64, 0:384], maskN[:, :], ALU.mult, [Pf[1], maskN], [Pm[1]])
                    psNO = PS[5]
                    for part in range(2):
                        for p in range(3):
                            for i in range(2):
                                lhs = vsp[0:64, p, i * 64:i * 64 + 128] if part == 0 else onesAB[0:64, i * 64:i * 64 + 128]
                                mm(psNO[:, (part * 3 + p) * 64:(part * 3 + p + 1) * 64], lhs, Pm[1][0:64, (i * 3 + p) * 64:(i * 3 + p + 1) * 64],
                                   i == 0, i == 1, [vsp if part == 0 else onesAB, Pm[1]], [psNO])
                    cp("dve", onew[:, :, :, :], psNO[:, 0:384].rearrange("p (a c q) -> p a c q", a=2, c=3), [psNO], [onew])
                    psW = PS[6]
                    for b in range(16):
                        sl = b % 2
                        dma("pool", f"kv{sl}", kws[sl][:], cwkT[b], writes=[kws[sl]])
                        for i in range(2):
                            for c in range(3):
                                dma("pool", f"kv{sl}", vws[sl][:, :, c, i * 128:i * 128 + 64],
                                    cwv[b].rearrange("k t (c i d) -> k t c i d", c=3, i=2)[:, :, c, i, :], writes=[vws[sl]])
                        psSi = (PS[sl], PS[2 + sl])
                        for t9 in range(9):
                            for p in range(3):
                                for i in range(2):
                                    col = t9 * 3 + p
                                    mm(psSi[i][:, col * 4:col * 4 + 4], kws[sl][64 * i:64 * i + 64, p, t9 * 128:(t9 + 1) * 128],
                                       qTs[64 * i:64 * i + 64, p, 4 * b:4 * b + 4], True, True, [kws[sl], qTs], [psSi[i]])
                        for i in range(2):
                            act(Pf[sl][:, i * 108:(i + 1) * 108], psSi[i][:, 0:108], AF.Exp, [psSi[i]], [Pf[sl]], scale=SCALE)
                        tt("pool", Pm[sl][:, 0:216], Pf[sl][:, 0:216], maskS[:, :], ALU.mult, [Pf[sl], maskS], [Pm[sl]])
                        for part in range(2):
                            for p in range(3):
                                k = 0
                                for t9 in range(9):
                                    for i in range(2):
                                        col = i * 27 + t9 * 3 + p
                                        lhs = vws[sl][:, t9, p, i * 64:i * 64 + 128] if part == 0 else onesAB[:, i * 64:i * 64 + 128]
                                        c0 = (part * 3 + p) * 64 + 4 * b
                                        mm(psW[:, c0:c0 + 4], lhs, Pm[sl][:, col * 4:col * 4 + 4], k == 0, k == 17,
                                           [vws[sl] if part == 0 else onesAB, Pm[sl]], [psW])
                                        k += 1
                    tt("dve", onew[:, :, :, :], psW[:, 0:384].rearrange("p (a c q) -> p a c q", a=2, c=3), onew[:, :, :, :], ALU.add,
                       [psW, onew], [onew])
                    recip(rl[:, 0:192], onew[:, 1, :, :].rearrange("p c q -> p (c q)"), [onew], [rl])
                    tt("dve", mixB[:, :, SEG:SEG + NS], onew[:, 0, :, :], rl[:, 0:192].rearrange("p (c q) -> p c q", c=3), ALU.mult,
                       [onew, rl], [mixB])
                P.barrier()
            with contextlib.ExitStack() as s3:
              if KSTOP >= 3:
                w_out_b = sbuf(s3, "w_out_b", [128, 8, D], BF16)
                dma("pool", "cp", w_out_b[:], w_out.rearrange("(c p) n -> p c n", p=128), writes=[w_out_b])
                xt3 = [sbuf(s3, f"xt3_{i}", [128, D]) for i in range(2)]
                x13 = [sbuf(s3, f"x13_{i}", [128, D]) for i in range(2)]
                srcT = [(oaT, 0), (oaT, 1), (oaT, 2), (mixB, 0), (mixB, 1), (mixB, 2), (mixM, 0), (mixM, 1)]
                for j in range(17):
                    nt = 128 if j < 16 else NS
                    par = j % 2
                    src = xp[j * 128:(j + 1) * 128, :] if j < 16 else xs_d[0:NS, :]
                    dma("sp", f"x{par}", xt3[par][0:nt, :], src, writes=[xt3[par]])
                    for hf in range(2):
                        bank = PS[(j * 2 + hf) % 4]
                        for c, (sb_, ci) in enumerate(srcT):
                            mm(bank[0:nt, :], sb_[:, ci, j * 128:j * 128 + nt], w_out_b[:, c, hf * 512:(hf + 1) * 512], c == 0, c == 7,
                               [sb_, w_out_b], [bank])
                        tt("dve", x13[par][0:nt, hf * 512:(hf + 1) * 512], bank[0:nt, :], xt3[par][0:nt, hf * 512:(hf + 1) * 512], ALU.add,
                           [bank, xt3[par]], [x13[par]])
                    dma("sp", f"o{par}", x1scr[j * 128:j * 128 + nt, :], x13[par][0:nt, :], reads=[x13[par]])
                P.barrier()

        NSLOT = 54
        SR = 384
        QT = SR // 128
        NROW = NSLOT * SR
        IO = bass.IndirectOffsetOnAxis
        w_gu2 = w_gu.rearrange("e r n -> (e r) n")
        w_dn2 = w_dn.rearrange("e r n -> (e r) n")

        regs = {}

        def _mkregs(h):
            for name, val in (("tok", NTOK + 127), ("w", NEXP * D - 1), ("row", NROW - 1)):
                r = h.alloc_register("bnd_" + name)
                h.reg_mov(r, val)
                regs[name] = r
        P.ops["pool"].append(_mkregs)

        def gather(sem, out, src, idx, reads, writes, bound):
            P.dmaf("pool", sem, lambda h: h.indirect_dma_start(out=out, out_offset=None, in_=src, in_offset=IO(ap=idx, axis=0),
                                                             bounds_check=regs[bound], oob_is_err=False),
                   [b.r for b in reads], [b.r for b in writes])

        with contextlib.ExitStack() as s4:
          if KSTOP >= 4:
            comb = sbuf(s4, "comb", [128, 17, 32]); msel = sbuf(s4, "msel", [128, 17, 32])
            pos4i = sbuf(s4, "pos4i", [128, 17 * 4], I32); tokid = sbuf(s4, "tokid", [128, 17], I32)
            onehot = sbuf(s4, "onehot", [128, NSLOT, 32])
            idxw = sbuf(s4, "idxw", [128, NSLOT * 8], I32); idxall = sbuf(s4, "idxall", [128, NSLOT * QT], I32)
            bguT = sbuf(s4, "bguT", [128, NEXP, 16])
            mc = sbuf(s4, "mc", [128, 111])
            siota = mc[:, 0:54]; eiota = mc[:, 54:86]; wrow = mc[:, 86:94]
            dma("sp", "c", mc[:], mconst_d[:, :], writes=[mc])
            dma("sp", "c", bguT[:], bguT_d[:, :, :], writes=[bguT])
            ts("dve", bguT[:, :, 8:16], bguT[:, :, 8:16], 1.0, None, ALU.add, None, [bguT], [bguT])
            cp("dve", tokid[:, :], mc[:, 94:111], [mc], [tokid])

            with contextlib.ExitStack() as s4a:
                g2c = sbuf(s4a, "g2c", [128, 8]); g2B = sbuf(s4a, "g2B", [128, D]); wr32 = sbuf(s4a, "wr32", [128, 8, 32])
                brB = sbuf(s4a, "brB", [128, 32]); bdn32 = sbuf(s4a, "bdn32", [32, D])
                triu = sbuf(s4a, "triu", [128, 128]); ones_f = sbuf(s4a, "ones_f", [128, 128])
                zb = sbuf(s4a, "zb", [128, D], BF16); zf = sbuf(s4a, "zf", [128, 32]); fillf = sbuf(s4a, "fillf", [128, NSLOT * QT])
                filli = sbuf(s4a, "filli", [128, NSLOT * QT], I32)
                dma("sp", "c", g2c[:], norm2_g[:, :], writes=[g2c])
                dma("sp", "c", g2B[:], norm2_row.partition_broadcast(128), writes=[g2B])
                dma("sp", "c", wr32[:], w_router.rearrange("(c p) n -> p c n", p=128), writes=[wr32])
                dma("sp", "c", brB[:], b_router.partition_broadcast(128), writes=[brB])
                dma("sp", "c", bdn32[:], b_dn[:, :], writes=[bdn32])
                dma("sp", "c", triu[:], triu_d[:, :], writes=[triu])
                P.op("pool", lambda h: h.memset(ones_f[:], 1.0), [], [ones_f.r])
                P.op("pool", lambda h: h.memset(zb[:], 0.0), [], [zb.r])
                P.op("pool", lambda h: h.memset(zf[:], 0.0), [], [zf.r])
                P.op("pool", lambda h: h.memset(fillf[:], float(NTOK)), [], [fillf.r])
                cp("dve", filli[:, :], fillf[:, :], [fillf], [filli])
                dma("sp", "o0", hn_d[NTOK:NTOK + 128, :], zb[:, :], reads=[zb])
                dma("sp", "o0", comb_d[NTOK:NTOK + 128, :], zf[:, :], reads=[zf])
                Rinv = Res("inv_d")
                P.dma("pool", "g", inv_d.rearrange("(p c) o -> p (c o)", p=128), filli[:, :], [filli.r], [Rinv])

                sm = lambda name, shape, dt=F32: sbuf(s4a, name, shape, dt)

                class TS:
                    pass
                tsets = []
                for gi in range(2):
                    T = TS(); sfx = f"_{gi}"
                    T.junk = sbuf(s4a, "junk4" + sfx, [128, D], BF16); T.hnb = sbuf(s4a, "hnb" + sfx, [128, D], BF16)
                    T.ssq = sbuf(s4a, "ssq4" + sfx, [128, 1]); T.std = sbuf(s4a, "std4" + sfx, [128, 1]); T.rstd = sbuf(s4a, "rstd4" + sfx, [128, 1])
                    T.hn32 = sbuf(s4a, "hn32" + sfx, [128, D]); T.hT32 = sbuf(s4a, "hT32" + sfx, [128, 8, 128])
                    T.lg = sbuf(s4a, "lg" + sfx, [128, 32]); T.m8 = sbuf(s4a, "m8" + sfx, [128, 8]); T.nmx = sbuf(s4a, "nmx" + sfx, [128, 1])
                    T.ex = sbuf(s4a, "ex" + sfx, [128, 32]); T.cw = sbuf(s4a, "cw" + sfx, [128, 32])
                    T.den = sbuf(s4a, "den" + sfx, [128, 1]); T.rden = sbuf(s4a, "rden" + sfx, [128, 1])
                    T.combT = sbuf(s4a, "combT" + sfx, [32, 128])
                    T.ps = [PS[4 * gi + i] for i in range(4)]
                    T.osem = f"o{gi}"
                    tsets.append(T)
                xts = [sbuf(s4a, f"x1t{i}", [128, D]) for i in range(4)]

                def load_x1(j):
                    nt = 128 if j < 16 else NS
                    dma("sp", "m", xts[j % 4][0:nt, :], x1scr[j * 128:j * 128 + nt, :], writes=[xts[j % 4]])

                def setup_tile(j, T):
                    nt = 128 if j < 16 else NS
                    r0 = j * 128
                    xt = xts[j % 4]
                    act(T.junk[0:nt, :], xt[0:nt, :], AF.Square, [xt], [T.junk, T.ssq], accum_out=T.ssq[0:nt, :])
                    yield
                    rstd_from_ssq(T.ssq, T.std, T.rstd, 1, D, nt)
                    yield
                    act(T.hn32[0:nt, :], xt[0:nt, :], AF.Copy, [xt, T.rstd], [T.hn32], scale=T.rstd[0:nt, :])
                    yield
                    tt("pool", T.hnb[0:nt, :], T.hn32[0:nt, :], g2B[0:nt, :], ALU.mult, [T.hn32, g2B], [T.hnb])
                    dma("sp", T.osem, hn_d[r0:r0 + nt, :], T.hnb[0:nt, :], reads=[T.hnb])
                    for c in range(8):
                        bank = T.ps[c // 4]
                        tr(bank[:, (c % 4) * 128:(c % 4) * 128 + nt], T.hn32[0:nt, c * 128:(c + 1) * 128], ident_f[0:nt, 0:nt], [T.hn32, ident_f], [bank])
                    yield
                    for hb in range(2):
                        bank = T.ps[hb]
                        bv = bank[:, :].rearrange("p (c t) -> p c t", c=4)[:, :, 0:nt]
                        gb = g2c[:, hb * 4:hb * 4 + 4].unsqueeze(2).to_broadcast([128, 4, nt])
                        tt("dve", T.hT32[:, hb * 4:hb * 4 + 4, 0:nt], bv, gb, ALU.mult, [bank, g2c], [T.hT32])
                    yield
                    for c in range(8):
                        mm(T.ps[2][0:nt, 0:32], T.hT32[:, c, 0:nt], wr32[:, c, :], c == 0, c == 7, [T.hT32, wr32], [T.ps[2]])
                    yield
                    tt("dve", T.lg[0:nt, :], T.ps[2][0:nt, 0:32], brB[0:nt, :], ALU.add, [T.ps[2], brB], [T.lg])
                    P.op("dve", lambda h: h.max(out=T.m8[0:nt, :], in_=T.lg[0:nt, :]), [T.lg.r], [T.m8.r])
                    ts("dve", T.nmx[0:nt, :], T.m8[0:nt, 0:1], -1.0, None, ALU.mult, None, [T.m8], [T.nmx])
                    yield
                    act(T.ex[0:nt, :], T.lg[0:nt, :], AF.Exp, [T.lg, T.nmx], [T.ex], bias=T.nmx[0:nt, :])
                    yield
                    stt(T.cw[0:nt, :], T.lg[0:nt, :], T.m8[0:nt, 3:4], T.ex[0:nt, :], ALU.is_ge, ALU.mult, [T.lg, T.m8, T.ex], [T.cw, T.den],
                        accum_out=T.den[0:nt, :])
                    recip(T.rden[0:nt, :], T.den[0:nt, :], [T.den], [T.rden])
                    ts("dve", comb[0:nt, j, :], T.cw[0:nt, :], T.rden[0:nt, :], None, ALU.mult, None, [T.cw, T.rden], [comb])
                    ts("dve", msel[0:nt, j, :], T.lg[0:nt, :], T.m8[0:nt, 3:4], None, ALU.is_ge, None, [T.lg, T.m8], [msel])
                    dma("sp", T.osem, comb_d[r0:r0 + nt, :], comb[0:nt, j, :], reads=[comb])
                    tr(T.ps[2][0:32, 128:128 + nt], comb[0:nt, j, :], ident_f[0:nt, 0:nt], [comb, ident_f], [T.ps[2]])
                    yield
                    cp("act", T.combT[:, 0:nt], T.ps[2][0:32, 128:128 + nt], [T.ps[2]], [T.combT])
                    yield
                    for hf in range(2):
                        bank = T.ps[3] if hf == 0 else T.ps[0]
                        mm(bank[0:nt, :], T.combT[:, 0:nt], bdn32[:, hf * 512:(hf + 1) * 512], True, True, [T.combT, bdn32], [bank])
                    yield
                    for hf in range(2):
                        bank = T.ps[3] if hf == 0 else T.ps[0]
                        tt("dve", xt[0:nt, hf * 512:(hf + 1) * 512], bank[0:nt, :], xt[0:nt, hf * 512:(hf + 1) * 512], ALU.add, [bank, xt], [xt])
                    dma("sp", T.osem, x1scr[r0:r0 + nt, :], xt[0:nt, :], reads=[xt])

                load_x1(0); load_x1(1)
                for j0 in range(0, 17, 2):
                    for j in (j0 + 2, j0 + 3):
                        if j < 17:
                            load_x1(j)
                    gens = [setup_tile(j, tsets[j - j0]) for j in (j0, j0 + 1) if j < 17]
                    while gens:
                        for g in list(gens):
                            try:
                                next(g)
                            except StopIteration:
                                gens.remove(g)

                cnt = sm("cnt", [128, 32]); nsl = [sm("nslA", [128, 32]), sm("nslB", [128, 32])]
                cum = [sm("cumA", [128, 32]), sm("cumB", [128, 32])]; st512 = sm("st512", [128, 32])
                tmp3 = sm("tmp3", [128, NSLOT, 32]); eall = sm("eall", [128, NSLOT]); idxwf = sm("idxwf", [128, NSLOT, 8])
                vt = [sm("vtA", [128, 32]), sm("vtB", [128, 32])]; p8 = sm("p8", [128, 8]); p4f = sm("p4f", [128, 4])
                ones_b = sm("ones_b", [128, 128], BF16); triu_b = sm("triu_b", [128, 128], BF16); msel_b = sm("msel_b", [128, 17, 32], BF16)
                cp("act", ones_b[:, :], ones_f[:, :], [ones_f], [ones_b])
                cp("act", triu_b[:, :], triu[:, :], [triu], [triu_b])
                cp("dve", msel_b[:, 0:16, :], msel[:, 0:16, :], [msel], [msel_b])
                cp("dve", msel_b[0:NS, 16, :], msel[0:NS, 16, :], [msel], [msel_b])
                for j in range(17):
                    nt = 128 if j < 16 else NS
                    mm(PS[0][:, 0:32], ones_b[0:nt, :], msel_b[0:nt, j, :], j == 0, j == 16, [ones_b, msel_b], [PS[0]])
                for j in range(17):
                    nt = 128 if j < 16 else NS
                    bank = PS[1] if j < 16 else PS[2]
                    dst = bank[0:nt, (j % 16) * 32:(j % 16) * 32 + 32]
                    for jp in range(j):
                        mm(dst, ones_b[:, 0:nt], msel_b[:, jp, :], jp == 0, False, [ones_b, msel_b], [bank])
                    mm(dst, triu_b[0:nt, 0:nt], msel_b[0:nt, j, :], j == 0, True, [triu_b, msel_b], [bank])
                cp("dve", cnt[:, :], PS[0][:, 0:32], [PS[0]], [cnt])
                ts("dve", nsl[0][:, :], cnt[:, :], 0.0, None, ALU.is_gt, None, [cnt], [nsl[0]])
                a = 0
                for k in range(1, 6):
                    stt(nsl[1 - a][:, :], cnt[:, :], float(SR) * k, nsl[a][:, :], ALU.is_gt, ALU.add, [cnt, nsl[a]], [nsl[1 - a]])
                    a = 1 - a
                nslots = nsl[a]
                cp("dve", cum[0][:, :], nslots[:, :], [nslots], [cum[0]])
                b_ = 0
                for sh in (1, 2, 4, 8, 16):
                    cp("dve", cum[1 - b_][:, 0:sh], cum[b_][:, 0:sh], [cum[b_]], [cum[1 - b_]])
                    tt("dve", cum[1 - b_][:, sh:32], cum[b_][:, sh:32], cum[b_][:, 0:32 - sh], ALU.add, [cum[b_]], [cum[1 - b_]])
                    b_ = 1 - b_
                cumi = cum[b_]
                tt("dve", st512[:, :], cumi[:, :], nslots[:, :], ALU.subtract, [cumi, nslots], [st512])
                ts("dve", st512[:, :], st512[:, :], float(SR), None, ALU.mult, None, [st512], [st512])
                tt("dve", tmp3[:, :, :], cumi[:, :].unsqueeze(1).to_broadcast([128, NSLOT, 32]),
                   siota.unsqueeze(2).to_broadcast([128, NSLOT, 32]), ALU.is_le, [cumi, mc], [tmp3])
                red(eall[:, :], tmp3[:, :, :], [tmp3], [eall])
                tt("dve", onehot[:, :, :], eiota.unsqueeze(1).to_broadcast([128, NSLOT, 32]),
                   eall[:, :].unsqueeze(2).to_broadcast([128, NSLOT, 32]), ALU.is_equal, [mc, eall], [onehot])
                stt(idxwf[:, :, :], eall[:, :].unsqueeze(2).to_broadcast([128, NSLOT, 8]), 1024.0,
                    wrow.unsqueeze(1).to_broadcast([128, NSLOT, 8]), ALU.mult, ALU.add, [eall, mc], [idxwf])
                cp("dve", idxw[:, :], idxwf[:, :, :].rearrange("p s c -> p (s c)"), [idxwf], [idxw])
                for j in range(17):
                    nt = 128 if j < 16 else NS
                    bank = PS[1] if j < 16 else PS[2]
                    rk = bank[0:nt, (j % 16) * 32:(j % 16) * 32 + 32]
                    stt(vt[0][0:nt, :], rk, 1.0, st512[0:nt, :], ALU.add, ALU.add, [bank, st512], [vt[0]])
                    tt("dve", vt[1][0:nt, :], vt[0][0:nt, :], msel[0:nt, j, :], ALU.mult, [vt[0], msel], [vt[1]])
                    P.op("dve", lambda h, nt=nt: h.max(out=p8[0:nt, :], in_=vt[1][0:nt, :]), [vt[1].r], [p8.r])
                    ts("dve", p4f[0:nt, :], p8[0:nt, 0:4], -1.0, None, ALU.add, None, [p8], [p4f])
                    cp("dve", pos4i[0:nt, j * 4:j * 4 + 4], p4f[0:nt, :], [p4f], [pos4i])
                for j in range(17):
                    nt = 128 if j < 16 else NS
                    for k in range(4):
                        P.dmaf("pool", "g", lambda h, nt=nt, j=j, k=k: h.indirect_dma_start(
                            out=inv_d[:, :], out_offset=IO(ap=pos4i[0:nt, j * 4 + k:j * 4 + k + 1], axis=0), in_=tokid[0:nt, j:j + 1], in_offset=None,
                            bounds_check=regs["row"], oob_is_err=False), [pos4i.r, tokid.r, Rinv], [])
                for q6 in range(9):
                    P.dma("sp", "m", idxall[:, q6 * 18:(q6 + 1) * 18],
                          inv_d[q6 * 2304:(q6 + 1) * 2304, :].rearrange("(s p) o -> p (s o)", p=128), [], [idxall.r, Rinv],
                          allow_slow_non_contiguous=True)
            P.barrier()

            with contextlib.ExitStack() as s4b:
                wgu = [sbuf(s4b, f"wgu{i}", [128, 8, 2048], BF16) for i in range(2)]
                wdn = [sbuf(s4b, f"wdn{i}", [128, 8, D], BF16) for i in range(2)]
                xg = [sbuf(s4b, f"xg{i}", [128, QT, D], BF16) for i in range(2)]
                hgT = [sbuf(s4b, f"hgT{i}", [128, 8, SR], BF16) for i in range(2)]
                gcomb = [sbuf(s4b, f"gcomb{i}", [128, QT, 32]) for i in range(2)]
                bias_s = [sbuf(s4b, f"bias_s{i}", [128, 16]) for i in range(2)]
                gate_s = [sbuf(s4b, f"gate_s{i}", [128, QT]) for i in range(2)]
                btmp = sbuf(s4b, "btmp", [128, 16, 32]); gtmp = sbuf(s4b, "gtmp", [128, QT, 32])
                gsb = [sbuf(s4b, f"gsb{i}", [128, SR]) for i in range(2)]
                ssb = [sbuf(s4b, f"ssb{i}", [128, SR]) for i in range(2)]
                usb = [sbuf(s4b, f"usb{i}", [128, SR]) for i in range(2)]
                actT = [sbuf(s4b, f"actT{i}", [128, 8, SR], BF16) for i in range(2)]
                res_sb = [sbuf(s4b, f"res_sb{i}", [128, D]) for i in range(3)]

                order = []
                for i in range(NSLOT // 3):
                    order += [i, 2 * (NSLOT // 3) + i]
                order += list(range(NSLOT // 3, 2 * (NSLOT // 3)))
                assert sorted(order) == list(range(NSLOT))

                def tok_loads(i):
                    s = order[i]
                    par = i % 2
                    for q in range(QT):
                        gather(f"kv{par}", xg[par][:, q, :], hn_d[:, :], idxall[:, s * QT + q:s * QT + q + 1], [idxall], [xg[par]], "tok")
                    for q in range(QT):
                        gather(f"kv{par}", gcomb[par][:, q, :], comb_d[:, :], idxall[:, s * QT + q:s * QT + q + 1], [idxall], [gcomb[par]], "tok")

                def wgu_loads(i):
                    s = order[i]
                    par = i % 2
                    for c in range(8):
                        gather(f"w{par}", wgu[par][:, c, :], w_gu2[:, :], idxw[:, s * 8 + c:s * 8 + c + 1], [idxw], [wgu[par]], "w")

                def wdn_loads(i):
                    s = order[i]
                    par = i % 2
                    for c in range(8):
                        gather(f"wd{par}", wdn[par][:, c, :], w_dn2[:, :], idxw[:, s * 8 + c:s * 8 + c + 1], [idxw], [wdn[par]], "w")

                def slot_transposes(i):
                    par = i % 2
                    for q in range(QT):
                        bank = PS[6 + q % 2]
                        bb = bank[:].bitcast(BF16)
                        for c in range(8):
                            tr(bb[:, c * 128:(c + 1) * 128], xg[par][:, q, c * 128:(c + 1) * 128], ident_b[:, :], [xg[par], ident_b], [bank])
                        cp("act" if q % 2 == 0 else "dve", hgT[par][:, :, q * 128:(q + 1) * 128], bb.rearrange("p (c t) -> p c t", c=8),
                           [bank], [hgT[par]])

                tok_loads(0); wgu_loads(0); wdn_loads(0)
                tok_loads(1) if NSLOT > 1 else None
                wgu_loads(1); wdn_loads(1)
                slot_transposes(0)
                for si in range(NSLOT):
                    s = order[si]
                    par = si % 2
                    tt("dve", btmp[:, :, :], bguT[:, :, :].rearrange("p e c -> p c e"),
                       onehot[:, s, :].unsqueeze(1).to_broadcast([128, 16, 32]), ALU.mult, [bguT, onehot], [btmp])
                    red(bias_s[par][:, :], btmp[:, :, :], [btmp], [bias_s[par]])
                    tt("dve", gtmp[:, :, :], gcomb[par][:, :, :], onehot[:, s, :].unsqueeze(1).to_broadcast([128, QT, 32]), ALU.mult,
                       [gcomb[par], onehot], [gtmp])
                    red(gate_s[par][:, :], gtmp[:, :, :], [gtmp], [gate_s[par]])
                    if si + 2 < NSLOT:
                        tok_loads(si + 2)
                    aT = actT[par]
                    for c in range(8):
                        db = c % 2
                        bg = PS[db * 2]; bu = PS[db * 2 + 1]
                        for k in range(8):
                            mm(bg[:, 0:SR], wgu[par][:, k, c * 128:(c + 1) * 128], hgT[par][:, k, :], k == 0, k == 7, [wgu[par], hgT[par]], [bg])
                        for k in range(8):
                            mm(bu[:, 0:SR], wgu[par][:, k, 1024 + c * 128:1024 + (c + 1) * 128], hgT[par][:, k, :], k == 0, k == 7,
                               [wgu[par], hgT[par]], [bu])
                        ts("dve", gsb[db][:, :], bg[:, 0:SR], bias_s[par][:, c:c + 1], 7.0, ALU.add, ALU.min, [bg, bias_s[par]], [gsb[db]])
                        act(ssb[db][:, :], gsb[db][:, :], AF.Sigmoid, [gsb[db]], [ssb[db]], scale=1.702)
                        ts("dve", usb[db][:, :], bu[:, 0:SR], bias_s[par][:, 8 + c:9 + c], -6.0, ALU.add, ALU.max, [bu, bias_s[par]], [usb[db]])
                        stt(usb[db][:, :], usb[db][:, :], 8.0, gsb[db][:, :], ALU.min, ALU.mult, [usb[db], gsb[db]], [usb[db]])
                        tt("dve", aT[:, c, :], usb[db][:, :], ssb[db][:, :], ALU.mult, [usb[db], ssb[db]], [aT])
                    if si + 2 < NSLOT:
                        wgu_loads(si + 2)
                    if si + 1 < NSLOT:
                        slot_transposes(si + 1)
                    for q in range(QT):
                        ri = (si * QT + q) % 3
                        rs = res_sb[ri]
                        for hf in range(2):
                            bank = PS[4 + hf]
                            for k in range(8):
                                mm(bank[:, :], aT[:, k, q * 128:(q + 1) * 128], wdn[par][:, k, hf * 512:(hf + 1) * 512], k == 0, k == 7,
                                   [aT, wdn[par]], [bank])
                            if hf == 0:
                                act(rs[:, 0:512], bank[:, :], AF.Copy, [bank, gate_s[par]], [rs], scale=gate_s[par][:, q:q + 1])
                            else:
                                ts("dve", rs[:, 512:1024], bank[:, :], gate_s[par][:, q:q + 1], None, ALU.mult, None, [bank, gate_s[par]], [rs])
                        dma("sp", f"r{ri}", res_d[(s * QT + q) * 128:(s * QT + q + 1) * 128, :], rs[:, :], reads=[rs])
                    if si + 2 < NSLOT:
                        wdn_loads(si + 2)
            P.barrier()

            with contextlib.ExitStack() as s4c:
                NCB = 4
                yb = [sbuf(s4c, f"yb{i}", [128, D]) for i in range(NCB)]
                rg = [[sbuf(s4c, f"rg{i}_{k}", [128, D]) for k in range(4)] for i in range(NCB)]
                csem = ("kv0", "kv1", "w0", "w1")
                xsem = ("x0", "x1", "o0", "o1")

                def comb_loads(j):
                    nt = 128 if j < 16 else NS
                    par = j % NCB
                    dma("sp", xsem[par], yb[par][0:nt, :], x1scr[j * 128:j * 128 + nt, :], writes=[yb[par]])
                    for k in range(4):
                        gather(csem[par], rg[par][k][0:nt, :], res_d[:, :], pos4i[0:nt, j * 4 + k:j * 4 + k + 1], [pos4i], [rg[par][k]], "row")
                for j in range(NCB - 1):
                    comb_loads(j)
                for j in range(17):
                    nt = 128 if j < 16 else NS
                    par = j % NCB
                    if j + NCB - 1 < 17:
                        comb_loads(j + NCB - 1)
                    for k in range(4):
                        tt("dve", yb[par][0:nt, :], yb[par][0:nt, :], rg[par][k][0:nt, :], ALU.add, [yb[par], rg[par][k]], [yb[par]])
                    if j < 16:
                        dma("sp", "y", y_p[j * 128:(j + 1) * 128, :], yb[par][:, :], reads=[yb[par]], is_output=True)
                    else:
                        dma("sp", "y", y_s[:, :], yb[par][0:NS, :], reads=[yb[par]], is_output=True)
        P.finish("sp")
        with block_cm as block:
            P.emit(block)
    return nc


def _host_constants():
    k = np.arange(128)[:, None]
    q = np.arange(128)[None, :]
    cur = (k <= q).astype(np.float32)
    prev = (k >= q).astype(np.float32)
    maskA = np.concatenate([prev, cur, prev, cur], axis=1)
    maskH0 = np.concatenate([0 * prev, cur, 0 * prev, cur], axis=1)
    mS = np.zeros((128, 2, 9, 3, 4), np.float32)
    for t9 in range(8):
        mS[:, :, t9, :, t9 % 4] = 1.0
    for t in range(4):
        mS[t:, :, 8, :, t] = 1.0
    maskS = mS.reshape(128, 216)
    w4 = np.zeros((4, 4), np.float32)
    for tk in range(4):
        for t in range(4):
            w4[tk, t] = (1.0 if tk <= t else 0.0) + (2.0 if tk == t else 0.0)
    mN = np.kron(np.eye(16, dtype=np.float32), w4)
    maskN = np.tile(mN, (1, 6))
    tril4 = np.zeros((4, 4), np.float32)
    for s in range(4):
        for t in range(4):
            tril4[s, t] = 1.0 if s <= t else 0.0
    mbd = np.kron(np.eye(16, dtype=np.float32), tril4)
    mask_bd = np.repeat(mbd[:, None, :], 6, axis=1)
    ident = np.eye(128, dtype=np.float32)
    onesAB = np.concatenate([np.ones((128, 64)), np.zeros((128, 64)), np.ones((128, 64))], axis=1).astype(np.float32)
    return dict(maskA=maskA, maskH0=maskH0, maskS=maskS, maskN=maskN, mask_bd=mask_bd, ident=ident, onesAB=onesAB)


def _rope_tables(pos):
    half = 8
    inv_freq = np.power(np.float32(500000.0), -np.arange(half, dtype=np.float32) / np.float32(half)).astype(np.float32)
    ang = pos.astype(np.float32)[:, None] * inv_freq[None, :]
    c = np.cos(ang).astype(np.float32)
    s = np.sin(ang).astype(np.float32)
    return np.concatenate([c, c], axis=1), np.concatenate([-s, s], axis=1)


_NC_CACHE = {}


def kernel(x_prompt, x_sample, mem_prompt, cache_win_k, cache_win_v, cache_mem_k, cache_mem_v,
           norm1_g, w_in, gv_a, w_s, b_s, gq_b, gk_b, gq_m, gk_m, mem_norm_g, w_mem_kv, w_out,
           norm2_g, w_router, b_router, w_gate_up, b_gate_up, w_down, b_down):
    f = lambda a: np.ascontiguousarray(np.asarray(a, dtype=np.float32))
    x_prompt, x_sample, mem_prompt = f(x_prompt), f(x_sample), f(mem_prompt)
    cwk, cwv = f(cache_win_k)[0], f(cache_win_v)[0]
    cmk, cmv = f(cache_mem_k)[0], f(cache_mem_v)[0]
    C = _host_constants()
    gT = lambda g: f(np.asarray(g)[0].reshape(8, 128).T)
    w_s0 = np.asarray(w_s, np.float32)[0]
    b_s0 = np.asarray(b_s, np.float32)[0]
    shared = {
        "norm1_gT": gT(norm1_g), "norm2_gT": gT(norm2_g), "memn_gT": gT(mem_norm_g),
        "w_in": f(w_in)[0], "w_mem_kv": f(w_mem_kv)[0], "w_out": f(w_out)[0],
        "gv_a": f(gv_a), "gqk": f(np.concatenate([np.tile(np.asarray(gq_b)[0], 6), np.tile(np.asarray(gk_b)[0], 6)])[None]),
        "gqm": f(np.tile(np.asarray(gq_m)[0], 4)[None]), "gkm": f(np.tile(np.asarray(gk_m)[0], 4)[None]),
        "w_sT": f(w_s0.transpose(2, 0, 1)),
        "bsB": f(np.repeat(b_s0.T[:, :, None], 64, axis=2).reshape(128, 384)),
        "maskA": C["maskA"], "maskS": C["maskS"], "maskN": C["maskN"], "mask_bd": C["mask_bd"],
        "ident": C["ident"], "onesAB": C["onesAB"],
        "w_router": f(w_router)[0], "b_router": f(b_router),
        "w_gate_up": f(w_gate_up)[0], "w_down": f(w_down)[0],
        "bguT": f(np.asarray(b_gate_up, np.float32)[0].reshape(NEXP, 16, 128).transpose(2, 0, 1)),
        "b_down": f(b_down)[0],
        "norm2_row": f(norm2_g),
    }
    pp = np.arange(128, dtype=np.float32)[:, None]
    shared["mconst"] = f(np.concatenate([np.tile(np.arange(54, dtype=np.float32)[None], (128, 1)),
                                         np.tile(np.arange(32, dtype=np.float32)[None], (128, 1)),
                                         np.arange(8, dtype=np.float32)[None] * 128 + pp,
                                         np.arange(17, dtype=np.float32)[None] * 128 + pp], axis=1))
    shared["triu"] = f(np.triu(np.ones((128, 128), np.float32), 1))
    bd = np.zeros((64, 6, 64), np.float32)
    for b in range(16):
        bd[4 * b:4 * b + 4, :, 4 * b:4 * b + 4] = w_s0[:, :4, :4].transpose(2, 0, 1)
    shared["bd_ws"] = bd
    shared["bsS"] = f(np.tile(np.repeat(b_s0[:, :4].T[:, :, None], 64, axis=2).reshape(4, 384), (16, 1)))
    rows = np.zeros((9, 128), np.int64)
    for t in range(4):
        rows[t] = t + 16 * np.arange(128)
        rows[4 + t] = 1536 + t + 4 * np.arange(128)
    rows[8] = 1920 + np.arange(128)

    in_maps = []
    for c in range(NCORES):
        b, g = c // 4, c % 4
        s0 = g * SEG
        m = dict(shared)
        m["xp"] = f(x_prompt[b, s0:s0 + SEG])
        m["xh"] = f(x_prompt[b, s0 - SEG:s0]) if g > 0 else np.zeros((SEG, D), np.float32)
        m["xs"] = f(x_sample[16 * c:16 * c + 16].reshape(NS, D))
        m["mem"] = f(mem_prompt[b])
        kk = cwk[16 * c:16 * c + 16][:, rows]
        m["cwkT"] = f(kk.reshape(16, 9, 128, 3, 128).transpose(0, 4, 3, 1, 2).reshape(16, 128, 3, 1152))
        vv = cwv[16 * c:16 * c + 16][:, rows]
        m["cwv"] = f(vv.transpose(0, 2, 1, 3, 4).reshape(16, 128, 9, 384))
        mk = cmk[16 * c:16 * c + 16]
        m["cmkT"] = f(mk.reshape(16, 256, 2, 128).transpose(3, 0, 2, 1))
        mv = cmv[16 * c:16 * c + 16]
        m["cmv"] = f(mv.reshape(16, 2, 128, 256).transpose(2, 0, 1, 3))
        pos = np.concatenate([s0 + np.arange(SEG), s0 - SEG + np.arange(SEG), 8192 + (np.arange(NS) % 4)])
        cs, sn = _rope_tables(pos)
        m["rope_cs"], m["rope_sn"] = cs, sn
        m["maskH"] = C["maskA"] if g > 0 else C["maskH0"]
        in_maps.append(m)

    if "nc" not in _NC_CACHE:
        _NC_CACHE["nc"] = build_program()
    nc = _NC_CACHE["nc"]
    res = run_bass_kernel_spmd(nc, in_maps, core_ids=list(range(NCORES)))
    R = res.results
    y_prompt = np.stack([np.concatenate([R[4 * b + g]["y_p"] for g in range(4)], axis=0) for b in range(2)])
    y_sample = np.concatenate([R[c]["y_s"].reshape(16, 4, D) for c in range(NCORES)], axis=0)
    wk = np.stack([R[4 * b + 3]["wk_p"] for b in range(2)]).reshape(1, 2, SEG, 6, 64)
    wv = np.stack([R[4 * b + 3]["wv_p"] for b in range(2)]).reshape(1, 2, SEG, 6, 64)
    mk = np.stack([R[4 * b]["mk_p"] for b in range(2)]).reshape(1, 2, 256, 4, 64)
    mv = np.stack([R[4 * b]["mv_p"] for b in range(2)]).reshape(1, 2, 256, 4, 64)
    wks = np.concatenate([R[c]["wk_s"].reshape(16, 4, 6, 64) for c in range(NCORES)], axis=0)[None]
    wvs = np.concatenate([R[c]["wv_s"].reshape(16, 4, 6, 64) for c in range(NCORES)], axis=0)[None]
    gvs = np.concatenate([R[c]["gv_s"].reshape(16, 4, 384) for c in range(NCORES)], axis=0)[None]
    return (y_prompt.astype(np.float32), y_sample.astype(np.float32), wk.astype(np.float32), wv.astype(np.float32),
            mk.astype(np.float32), mv.astype(np.float32), wks.astype(np.float32), wvs.astype(np.float32), gvs.astype(np.float32))
```

```python
import contextlib
import numpy as np
import concourse.bass as bass
import concourse.mybir as mybir
from concourse.bass_utils import run_bass_kernel_spmd

F32 = mybir.dt.float32
BF16 = mybir.dt.bfloat16
I32 = mybir.dt.int32
AF = mybir.ActivationFunctionType
ALU = mybir.AluOpType
AX = mybir.AxisListType

NCORES = 8
D = 1024
SEG = 2048
NS = 64
NTOK = SEG + NS
NEXP = 32
EPS = 1e-6
SCALE = 0.125
KSTOP = 9


class Res:
    __slots__ = ("name", "w", "r", "excl")

    def __init__(self, name="", excl=False):
        self.name = name
        self.w = None
        self.r = []
        self.excl = excl


class Prog:
    ENGS = ("pe", "act", "dve", "pool", "sp")

    def __init__(self, nc):
        self.nc = nc
        self.ops = {e: [] for e in self.ENGS}
        self.cnt = {e: 0 for e in self.ENGS}
        self.sems = {}
        self.semval = {}
        self.known = {e: {} for e in self.ENGS}
        self.sealed = {}
        self.out_tokens = []

    def add_sem(self, key, handle):
        self.sems[key] = handle
        self.semval[key] = 0

    def _need(self, eng, tok, same_ok):
        if tok is None:
            return
        key, val = tok
        if key in self.cnt:
            if key == eng and same_ok:
                return
        else:
            if self.known[eng].get(key, 0) >= val:
                return
            val = max(val, self.semval[key])
            self.sealed[key] = True
        if self.known[eng].get(key, 0) >= val:
            return
        self.known[eng][key] = val
        sem = self.sems[key]
        self.ops[eng].append(lambda h, sem=sem, val=val: h.wait_ge(sem, val))

    def _deps(self, eng, reads, writes, is_dma=False, semkey=None):
        same_ok = (not is_dma) and eng != "pool"
        need = {}

        def add(tok, ok):
            if tok is None:
                return
            key, val = tok
            if ok and key == eng:
                return
            if need.get(key, 0) < val:
                need[key] = val
        for r in reads:
            add(r.w, False)
        for w in writes:
            if not (is_dma and w.w is not None and w.w[0] == semkey):
                add(w.w, same_ok)
            for t in w.r:
                add(t, same_ok)
        for key, val in need.items():
            self._need(eng, (key, val), same_ok=False)

    def _commit(self, tok, reads, writes):
        for r in reads:
            r.r.append(tok)
        for w in writes:
            w.w = tok
            w.r = []

    @staticmethod
    def _split(reads, writes):
        writes = list(writes) + [r for r in reads if r.excl and r not in writes]
        reads = [r for r in reads if not r.excl]
        return reads, writes

    def op(self, eng, fn, reads=(), writes=()):
        reads, writes = self._split(reads, writes)
        self._deps(eng, reads, writes)
        self.cnt[eng] += 1
        tok = (eng, self.cnt[eng])
        sem = self.sems[eng]
        self.ops[eng].append(lambda h, fn=fn, sem=sem: fn(h).then_inc(sem, 1))
        self._commit(tok, reads, writes)
        return tok

    def dma(self, eng, semkey, out, in_, reads=(), writes=(), is_output=False, **kw):
        self._deps(eng, reads, writes, is_dma=True, semkey=semkey)
        if self.sealed.get(semkey) and self.semval[semkey] > 0:
            self._need(eng, (semkey, self.semval[semkey]), same_ok=False)
        self.sealed[semkey] = False
        self.semval[semkey] += 16
        tok = (semkey, self.semval[semkey])
        sem = self.sems[semkey]
        self.ops[eng].append(
            lambda h, out=out, in_=in_, sem=sem, kw=kw: h.dma_start(out=out, in_=in_, **kw).then_inc(sem, 16))
        self._commit(tok, reads, writes)
        if is_output:
            self.out_tokens.append(tok)
        return tok

    def dmaf(self, eng, semkey, fn, reads=(), writes=(), is_output=False):
        self._deps(eng, reads, writes, is_dma=True, semkey=semkey)
        if self.sealed.get(semkey) and self.semval[semkey] > 0:
            self._need(eng, (semkey, self.semval[semkey]), same_ok=False)
        self.sealed[semkey] = False
        self.semval[semkey] += 16
        tok = (semkey, self.semval[semkey])
        sem = self.sems[semkey]
        self.ops[eng].append(lambda h, fn=fn, sem=sem: fn(h).then_inc(sem, 16))
        self._commit(tok, reads, writes)
        if is_output:
            self.out_tokens.append(tok)
        return tok

    def barrier(self):
        for e in self.ENGS:
            for f in self.ENGS:
                if (f != e or e == "pool") and self.cnt[f] > 0:
                    self._need(e, (f, self.cnt[f]), same_ok=False)
            for k, v in self.semval.items():
                if v > 0:
                    self._need(e, (k, v), same_ok=False)

    def finish(self, eng="sp"):
        last = {}
        for key, val in self.out_tokens:
            last[key] = max(last.get(key, 0), val)
        for key, val in last.items():
            self._need(eng, (key, val), same_ok=False)

    def emit(self, block):
        hmap = {"pe": "tensor", "act": "scalar", "dve": "vector", "pool": "gpsimd", "sp": "sync"}
        for e in self.ENGS:
            ops = self.ops[e]
            if not ops:
                continue

            def body(h, ops=ops):
                for f in ops:
                    f(h)
            getattr(block, hmap[e])(body)


class B:
    def __init__(self, t, name, excl=False):
        self.t = t
        self.r = Res(name, excl)

    def __getitem__(self, k):
        return self.t[k]


def build_program():
    nc = bass.Bass("TRN2", target_bir_lowering=False)

    def din(name, shape):
        return nc.dram_tensor(name, list(shape), F32, kind="ExternalInput").ap()

    def dout(name, shape):
        return nc.dram_tensor(name, list(shape), F32, kind="ExternalOutput").ap()

    xp = din("xp", [SEG, D]); xh = din("xh", [SEG, D]); xs_d = din("xs", [NS, D]); mem_d = din("mem", [256, D])
    cwkT = din("cwkT", [16, 128, 3, 1152]); cwv = din("cwv", [16, 128, 9, 384])
    cmkT = din("cmkT", [128, 16, 2, 256]); cmv = din("cmv", [128, 16, 2, 256])
    norm1_g = din("norm1_gT", [128, 8]); norm2_g = din("norm2_gT", [128, 8]); memn_g = din("memn_gT", [128, 8])
    w_in = din("w_in", [D, 2176]); w_mem = din("w_mem_kv", [D, 512]); w_out = din("w_out", [D, D])
    gv_a = din("gv_a", [1, 384]); gqk = din("gqk", [1, 768]); gqm = din("gqm", [1, 256]); gkm = din("gkm", [1, 256])
    w_sT = din("w_sT", [128, 6, 128]); bsB_d = din("bsB", [128, 384])
    bd_ws = din("bd_ws", [64, 6, 64]); bsS_d = din("bsS", [64, 384]); mask_bd = din("mask_bd", [64, 6, 64])
    rope_cs = din("rope_cs", [2 * SEG + NS, 16]); rope_sn = din("rope_sn", [2 * SEG + NS, 16])
    maskA_d = din("maskA", [128, 512]); maskH_d = din("maskH", [128, 512])
    maskS_d = din("maskS", [128, 216]); maskN_d = din("maskN", [64, 384])
    ident_d = din("ident", [128, 128]); onesAB_d = din("onesAB", [128, 192])
    w_router = din("w_router", [D, 32]); b_router = din("b_router", [1, 32])
    w_gu = din("w_gate_up", [NEXP, D, 2048]); w_dn = din("w_down", [NEXP, D, D])
    bguT_d = din("bguT", [128, NEXP, 16]); b_dn = din("b_down", [NEXP, D])

    mconst_d = din("mconst", [128, 111]); triu_d = din("triu", [128, 128]); norm2_row = din("norm2_row", [1, D])

    vscr = nc.dram_tensor("vscr", [2 * SEG, 384], F32, kind="Internal").ap()
    x1scr = nc.dram_tensor("x1scr", [NTOK, D], F32, kind="Internal").ap()
    hn_d = nc.dram_tensor("hn_d", [NTOK + 128, D], BF16, kind="Internal").ap()
    comb_d = nc.dram_tensor("comb_d", [NTOK + 128, 32], F32, kind="Internal").ap()
    inv_d = nc.dram_tensor("inv_d", [54 * 384, 1], I32, kind="Internal").ap()
    res_d = nc.dram_tensor("res_d", [54 * 384, D], F32, kind="Internal").ap()

    y_p = dout("y_p", [SEG, D]); y_s = dout("y_s", [NS, D])
    wk_p = dout("wk_p", [SEG, 384]); wv_p = dout("wv_p", [SEG, 384])
    mk_p = dout("mk_p", [256, 256]); mv_p = dout("mv_p", [256, 256])
    wk_s = dout("wk_s", [NS, 384]); wv_s = dout("wv_s", [NS, 384]); gv_s = dout("gv_s", [NS, 384])

    with contextlib.ExitStack() as top:
        P = Prog(nc)
        for e in Prog.ENGS:
            P.add_sem(e, top.enter_context(nc.semaphore("s_" + e)))
        for k in ("c", "cp", "x0", "x1", "o0", "o1", "v", "kv0", "kv1", "w0", "w1", "wd0", "wd1", "g", "r0", "r1", "r2", "m", "y"):
            P.add_sem(k, top.enter_context(nc.semaphore("d_" + k)))
        block_cm = nc.Block()

        def sbuf(es, name, shape, dt=F32):
            return B(es.enter_context(nc.sbuf_tensor("sb_" + name, list(shape), dt)), name)

        PS = [B(top.enter_context(nc.psum_tensor(f"ps{i}", [128, 512], F32)), f"ps{i}", excl=True) for i in range(8)]

        def act(out, in_, func, reads, writes, **kw):
            P.op("act", lambda h: h.activation(out=out, in_=in_, func=func, **kw),
                 [b.r for b in reads], [b.r for b in writes])

        def tt(eng, out, in0, in1, op, reads, writes):
            P.op(eng, lambda h: h.tensor_tensor(out=out, in0=in0, in1=in1, op=op),
                 [b.r for b in reads], [b.r for b in writes])

        def ts(eng, out, in0, s1, s2, op0, op1, reads, writes, **kw):
            if op1 is None:
                P.op(eng, lambda h: h.tensor_scalar(out=out, in0=in0, scalar1=s1, scalar2=None, op0=op0, **kw),
                     [b.r for b in reads], [b.r for b in writes])
            else:
                P.op(eng, lambda h: h.tensor_scalar(out=out, in0=in0, scalar1=s1, scalar2=s2, op0=op0, op1=op1, **kw),
                     [b.r for b in reads], [b.r for b in writes])

        def stt(out, in0, scalar, in1, op0, op1, reads, writes, **kw):
            P.op("dve", lambda h: h.scalar_tensor_tensor(out=out, in0=in0, scalar=scalar, in1=in1, op0=op0, op1=op1, **kw),
                 [b.r for b in reads], [b.r for b in writes])

        def cp(eng, out, in_, reads, writes):
            if eng == "act":
                act(out, in_, AF.Copy, reads, writes)
            else:
                P.op(eng, lambda h: h.tensor_copy(out, in_), [b.r for b in reads], [b.r for b in writes])

        def mm(out, lhsT, rhs, start, stop, reads, writes):
            P.op("pe", lambda h: h.matmul(out, lhsT=lhsT, rhs=rhs, start=start, stop=stop),
                 [b.r for b in reads], [b.r for b in writes])

        def tr(out, in_, ident, reads, writes):
            P.op("pe", lambda h: h.transpose(out=out, in_=in_, identity=ident),
                 [b.r for b in reads], [b.r for b in writes])

        def recip(out, in_, reads, writes):
            P.op("dve", lambda h: h.reciprocal(out, in_), [b.r for b in reads], [b.r for b in writes])

        def red(out, in_, reads, writes):
            P.op("dve", lambda h: h.tensor_reduce(out=out, in_=in_, axis=AX.X, op=ALU.add),
                 [b.r for b in reads], [b.r for b in writes])

        def dma(eng, sem, out, in_, reads=(), writes=(), **kw):
            P.dma(eng, sem, out, in_, [b.r for b in reads], [b.r for b in writes], **kw)

        def rstd_from_ssq(ssq, std, rstd, n, width, rows):
            act(std[0:rows, 0:n], ssq[0:rows, 0:n], AF.Sqrt, [ssq], [std], scale=1.0 / width, bias=EPS)
            recip(rstd[0:rows, 0:n], std[0:rows, 0:n], [std], [rstd])

        ident_f = sbuf(top, "ident_f", [128, 128]); ident_b = sbuf(top, "ident_b", [128, 128], BF16)
        dma("sp", "c", ident_f[:], ident_d[:, :], writes=[ident_f])
        cp("dve", ident_b[:], ident_f[:], [ident_f], [ident_b])

        with contextlib.ExitStack() as sA:
            oaT = sbuf(sA, "oaT", [128, 3, NTOK], BF16)
            mixB = sbuf(sA, "mixB", [128, 3, NTOK], BF16)
            mixM = sbuf(sA, "mixM", [128, 2, NTOK], BF16)
            with contextlib.ExitStack() as sB:
                qT = sbuf(sB, "qT", [128, 3, SEG], BF16)
                kT = sbuf(sB, "kT", [128, 3, 2 * SEG], BF16)
                qmT = sbuf(sB, "qmT", [128, 2, NTOK], BF16)
                kmT = sbuf(sB, "kmT", [128, 2, 256], BF16)
                vmp = sbuf(sB, "vmp", [128, 2, 2, 192], BF16)
                qTs = sbuf(sB, "qTs", [128, 3, NS], BF16)
                kTs = sbuf(sB, "kTs", [128, 3, NS], BF16)
                vsp = sbuf(sB, "vsp", [64, 3, 192], BF16)
                onesAB = sbuf(sB, "onesAB", [128, 192], BF16)
                dma("pool", "cp", onesAB[:], onesAB_d[:, :], writes=[onesAB])
                P.op("pool", lambda h: h.memset(vmp[:], 0.0), [], [vmp.r])
                P.op("pool", lambda h: h.memset(vsp[:], 0.0), [], [vsp.r])

                with contextlib.ExitStack() as s1:
                    w_in_b = sbuf(s1, "w_in_b", [128, 8, 2176], BF16)
                    w_mem_b = sbuf(s1, "w_mem_b", [128, 8, 512], BF16)
                    for hh in range(2):
                        dma("pool", "cp", w_in_b[:, :, hh * 1088:(hh + 1) * 1088],
                            w_in.rearrange("(c p) n -> p c n", p=128)[:, :, hh * 1088:(hh + 1) * 1088], writes=[w_in_b])
                    dma("pool", "cp", w_mem_b[:], w_mem.rearrange("(c p) n -> p c n", p=128), writes=[w_mem_b])
                    wsT_f = sbuf(s1, "wsT_f", [128, 6, 128]); wsT_b = sbuf(s1, "wsT_b", [128, 6, 128], BF16)
                    mcur_f = sbuf(s1, "mcur_f", [128, 128])
                    dma("sp", "c", wsT_f[:], w_sT[:, :, :], writes=[wsT_f])
                    dma("sp", "c", mcur_f[:], maskA_d[:, 384:512], writes=[mcur_f])
                    tt("dve", wsT_b[:], wsT_f[:], mcur_f[:].unsqueeze(1).to_broadcast([128, 6, 128]), ALU.mult,
                       [wsT_f, mcur_f], [wsT_b])
                    bd_f = sbuf(s1, "bd_f", [64, 6, 64]); mbd_f = sbuf(s1, "mbd_f", [64, 6, 64]); bd_b = sbuf(s1, "bd_b", [64, 6, 64], BF16)
                    dma("sp", "c", bd_f[:], bd_ws[:, :, :], writes=[bd_f])
                    dma("sp", "c", mbd_f[:], mask_bd[:, :, :], writes=[mbd_f])
                    tt("dve", bd_b[:], bd_f[:], mbd_f[:], ALU.mult, [bd_f, mbd_f], [bd_b])
                    bsB = sbuf(s1, "bsB", [128, 384]); bsS = sbuf(s1, "bsS", [64, 384])
                    dma("sp", "c", bsB[:], bsB_d[:, :], writes=[bsB])
                    dma("sp", "c", bsS[:], bsS_d[:, :], writes=[bsS])
                    g1c = sbuf(s1, "g1c", [128, 8]); gmc = sbuf(s1, "gmc", [128, 8])
                    dma("sp", "c", g1c[:], norm1_g[:, :], writes=[g1c])
                    dma("sp", "c", gmc[:], memn_g[:, :], writes=[gmc])
                    gvB = sbuf(s1, "gvB", [128, 384]); gqkB = sbuf(s1, "gqkB", [128, 768])
                    gqmB = sbuf(s1, "gqmB", [128, 256]); gkmB = sbuf(s1, "gkmB", [128, 256])
                    dma("sp", "c", gvB[:], gv_a.partition_broadcast(128), writes=[gvB])
                    dma("sp", "c", gqkB[:], gqk.partition_broadcast(128), writes=[gqkB])
                    dma("sp", "c", gqmB[:], gqm.partition_broadcast(128), writes=[gqmB])
                    dma("sp", "c", gkmB[:], gkm.partition_broadcast(128), writes=[gkmB])

                    class WS:
                        pass
                    wsets = []
                    for par in range(2):
                        w = WS()
                        sfx = f"_{par}"
                        w.xt = sbuf(s1, "xt" + sfx, [128, D]); w.junk = sbuf(s1, "junk" + sfx, [128, D], BF16)
                        w.ssq = sbuf(s1, "ssq" + sfx, [128, 1]); w.std = sbuf(s1, "std" + sfx, [128, 1]); w.rstd = sbuf(s1, "rstd" + sfx, [128, 1])
                        w.xs = sbuf(s1, "xs" + sfx, [128, D], BF16); w.hT = sbuf(s1, "hT" + sfx, [128, 8, 128], BF16)
                        w.u32 = sbuf(s1, "u32" + sfx, [128, 384])
                        w.ssqv = sbuf(s1, "ssqv" + sfx, [128, 1]); w.stdv = sbuf(s1, "stdv" + sfx, [128, 1]); w.rstdv = sbuf(s1, "rstdv" + sfx, [128, 1])
                        w.vn32 = sbuf(s1, "vn32" + sfx, [128, 384]); w.vnb = sbuf(s1, "vnb" + sfx, [128, 384], BF16)
                        w.sqt = sbuf(s1, "sqt" + sfx, [128, 768])
                        w.ssqk = sbuf(s1, "ssqk" + sfx, [128, 12]); w.stdk = sbuf(s1, "stdk" + sfx, [128, 12]); w.rstdk = sbuf(s1, "rstdk" + sfx, [128, 12])
                        w.qk32 = sbuf(s1, "qk32" + sfx, [128, 12, 64])
                        w.rt1 = sbuf(s1, "rt1" + sfx, [128, 12, 16]); w.rt2 = sbuf(s1, "rt2" + sfx, [128, 12, 16])
                        w.qkb = sbuf(s1, "qkb" + sfx, [128, 768], BF16)
                        w.v32 = sbuf(s1, "v32" + sfx, [128, 384])
                        w.ssqm = sbuf(s1, "ssqm" + sfx, [128, 4]); w.stdm = sbuf(s1, "stdm" + sfx, [128, 4]); w.rstdm = sbuf(s1, "rstdm" + sfx, [128, 4])
                        w.qm32 = sbuf(s1, "qm32" + sfx, [128, 4, 64]); w.qmb = sbuf(s1, "qmb" + sfx, [128, 256], BF16)
                        w.g1 = sbuf(s1, "g1" + sfx, [128, 384]); w.oab = sbuf(s1, "oab" + sfx, [128, 384], BF16)
                        w.cs = sbuf(s1, "cs" + sfx, [128, 16]); w.sn = sbuf(s1, "sn" + sfx, [128, 16])
                        wsets.append(w)

                    PSxT = PS[0]
                    psxT_b = PS[0][:].bitcast(BF16)
                    ps7_b = PS[7][:].bitcast(BF16)

                    def load_x(idx):
                        kind, it, nt, src_ap, ropebase = tiles1[idx]
                        par = idx % 2
                        w = wsets[par]
                        dma("sp", f"x{par}", w.xt[0:nt, :], src_ap, writes=[w.xt])
                        if ropebase is not None:
                            dma("sp", f"x{par}", w.cs[0:nt, :], rope_cs[ropebase:ropebase + nt, :], writes=[w.cs])
                            dma("sp", f"x{par}", w.sn[0:nt, :], rope_sn[ropebase:ropebase + nt, :], writes=[w.sn])

                    def norm_hT(w, src_ap, nt, gcol, par):
                        act(w.junk[0:nt, :], w.xt[0:nt, :], AF.Square, [w.xt], [w.junk, w.ssq], accum_out=w.ssq[0:nt, :])
                        rstd_from_ssq(w.ssq, w.std, w.rstd, 1, D, nt)
                        act(w.xs[0:nt, :], w.xt[0:nt, :], AF.Copy, [w.xt, w.rstd], [w.xs], scale=w.rstd[0:nt, :])
                        for c in range(8):
                            tr(psxT_b[:, c * 128:c * 128 + nt], w.xs[0:nt, c * 128:(c + 1) * 128], ident_b[0:nt, 0:nt],
                               [w.xs, ident_b], [PSxT])
                        tt("dve", w.hT[:, :, 0:nt], psxT_b.rearrange("p (c t) -> p c t", c=8)[:, :, 0:nt],
                           gcol[:, 0:8].unsqueeze(2).to_broadcast([128, 8, nt]), ALU.mult, [PSxT, gcol], [w.hT])

                    def proj(w, bank, wt, c0, width, nt):
                        for k in range(8):
                            mm(bank[0:nt, 0:width], w.hT[:, k, 0:nt], wt[:, k, c0:c0 + width], k == 0, k == 7,
                               [w.hT, wt], [bank])

                    def head_norm(w, bank, nh, h0, sq, ssq, std, rstd, dst32, nt, sq_off):
                        act(sq[0:nt, sq_off:sq_off + nh * 64], bank[0:nt, 0:nh * 64], AF.Square, [bank], [sq])

                    def rope(w, h0, nh, nt):
                        q = w.qk32
                        csb = w.cs[0:nt, :].unsqueeze(1).to_broadcast([nt, nh, 16])
                        tt("pool", w.rt1[0:nt, h0:h0 + nh, :], q[0:nt, h0:h0 + nh, 0:16], csb, ALU.mult, [w.qk32, w.cs], [w.rt1])
                        tt("pool", w.rt2[0:nt, h0:h0 + nh, 0:8], q[0:nt, h0:h0 + nh, 8:16],
                           w.sn[0:nt, 0:8].unsqueeze(1).to_broadcast([nt, nh, 8]), ALU.mult, [w.qk32, w.sn], [w.rt2])
                        tt("pool", w.rt2[0:nt, h0:h0 + nh, 8:16], q[0:nt, h0:h0 + nh, 0:8],
                           w.sn[0:nt, 8:16].unsqueeze(1).to_broadcast([nt, nh, 8]), ALU.mult, [w.qk32, w.sn], [w.rt2])
                        tt("pool", q[0:nt, h0:h0 + nh, 0:16], w.rt1[0:nt, h0:h0 + nh, :], w.rt2[0:nt, h0:h0 + nh, :], ALU.add,
                           [w.rt1, w.rt2], [w.qk32])

                    def qk_norm_rope(w, nt, with_q, ropebase):
                        h0 = 0 if with_q else 6
                        nh = 12 - h0
                        if with_q:
                            act(w.sqt[0:nt, 0:384], PS[3][0:nt, 0:384], AF.Square, [PS[3]], [w.sqt])
                        act(w.sqt[0:nt, 384:768], PS[4][0:nt, 0:384], AF.Square, [PS[4]], [w.sqt])
                        red(w.ssqk[0:nt, h0:12], w.sqt[0:nt, h0 * 64:768].rearrange("p (h d) -> p h d", d=64), [w.sqt], [w.ssqk])
                        act(w.stdk[0:nt, h0:12], w.ssqk[0:nt, h0:12], AF.Sqrt, [w.ssqk], [w.stdk], scale=1.0 / 64, bias=EPS)
                        recip(w.rstdk[0:nt, h0:12], w.stdk[0:nt, h0:12], [w.stdk], [w.rstdk])
                        if with_q:
                            tt("dve", w.qk32[0:nt, 0:6, :], PS[3][0:nt, 0:384].rearrange("p (h d) -> p h d", d=64),
                               w.rstdk[0:nt, 0:6].unsqueeze(2).to_broadcast([nt, 6, 64]), ALU.mult, [PS[3], w.rstdk], [w.qk32])
                        tt("dve", w.qk32[0:nt, 6:12, :], PS[4][0:nt, 0:384].rearrange("p (h d) -> p h d", d=64),
                           w.rstdk[0:nt, 6:12].unsqueeze(2).to_broadcast([nt, 6, 64]), ALU.mult, [PS[4], w.rstdk], [w.qk32])
                        tt("pool", w.qk32[0:nt, h0:12, :], w.qk32[0:nt, h0:12, :],
                           gqkB[0:nt, h0 * 64:768].rearrange("p (h d) -> p h d", d=64), ALU.mult, [w.qk32, gqkB], [w.qk32])
                        rope(w, h0, nh, nt)
                        cp("act", w.qkb[0:nt, h0 * 64:768], w.qk32[0:nt, h0:12, :].rearrange("p h d -> p (h d)"), [w.qk32], [w.qkb])

                    front_done = set()

                    def do_front(idx):
                        if idx in front_done or idx >= len(tiles1):
                            return
                        kind_, it_, nt_, src_, rb_ = tiles1[idx]
                        norm_hT(wsets[idx % 2], src_, nt_, gmc if kind_ == "mem" else g1c, idx % 2)
                        front_done.add(idx)

                    tiles1 = [("mem", mt, 128, mem_d[mt * 128:(mt + 1) * 128, :], None) for mt in range(2)]
                    tiles1 += [("halo", it, 128, xh[it * 128:(it + 1) * 128, :], SEG + it * 128) for it in range(16)]
                    tiles1 += [("own", it, 128, xp[it * 128:(it + 1) * 128, :], it * 128) for it in range(16)]
                    tiles1 += [("samp", 0, NS, xs_d[0:NS, :], 2 * SEG)]
                    load_x(0)

                    tcount = 0
                    for mt in range(2):
                        par = tcount % 2
                        load_x(tcount + 1)
                        cur = tcount
                        tcount += 1
                        w = wsets[par]
                        do_front(cur)
                        proj(w, PS[1], w_mem_b, 0, 512, 128)
                        act(w.sqt[:, 0:256], PS[1][:, 0:256], AF.Square, [PS[1]], [w.sqt])
                        red(w.ssqm[:, 0:4], w.sqt[:, 0:256].rearrange("p (h d) -> p h d", d=64), [w.sqt], [w.ssqm])
                        act(w.stdm[:, 0:4], w.ssqm[:, 0:4], AF.Sqrt, [w.ssqm], [w.stdm], scale=1.0 / 64, bias=EPS)
                        recip(w.rstdm[:, 0:4], w.stdm[:, 0:4], [w.stdm], [w.rstdm])
                        tt("dve", w.qm32[:, :, :], PS[1][:, 0:256].rearrange("p (h d) -> p h d", d=64),
                           w.rstdm[:, 0:4].unsqueeze(2).to_broadcast([128, 4, 64]), ALU.mult, [PS[1], w.rstdm], [w.qm32])
                        tt("pool", w.qm32[:, :, :], w.qm32[:, :, :], gkmB[:, :].rearrange("p (h d) -> p h d", d=64), ALU.mult,
                           [w.qm32, gkmB], [w.qm32])
                        dma("sp", f"o{par}", mk_p[mt * 128:(mt + 1) * 128, :], w.qm32[:, :, :].rearrange("p h d -> p (h d)"),
                            reads=[w.qm32], is_output=True)
                        cp("act", w.qmb[:, :], w.qm32[:, :, :].rearrange("p h d -> p (h d)"), [w.qm32], [w.qmb])
                        for pp in range(2):
                            tr(ps7_b[:, pp * 128:(pp + 1) * 128], w.qmb[:, pp * 128:(pp + 1) * 128], ident_b[:, :], [w.qmb, ident_b], [PS[7]])
                        cp("dve", kmT[:, :, mt * 128:(mt + 1) * 128], ps7_b[:, 0:256].rearrange("p (c t) -> p c t", c=2), [PS[7]], [kmT])
                        cp("act", w.v32[:, 0:256], PS[1][:, 256:512], [PS[1]], [w.v32])
                        do_front(cur + 1)
                        dma("sp", f"o{par}", mv_p[mt * 128:(mt + 1) * 128, :], w.v32[:, 0:256], reads=[w.v32], is_output=True)
                        for i in range(2):
                            cp("pool", vmp[:, mt, :, i * 128:i * 128 + 64],
                               w.v32[:, 0:256].rearrange("p (c i d) -> p c i d", c=2, i=2)[:, :, i, :], [w.v32], [vmp])

                    for it in range(16):
                        par = tcount % 2
                        load_x(tcount + 1)
                        cur = tcount
                        tcount += 1
                        w = wsets[par]
                        do_front(cur)
                        proj(w, PS[4], w_in_b, 1152, 384, 128)
                        proj(w, PS[5], w_in_b, 1536, 384, 128)
                        qk_norm_rope(w, 128, False, SEG + it * 128)
                        cp("act", w.v32[:, :], PS[5][:, 0:384], [PS[5]], [w.v32])
                        do_front(cur + 1)
                        for pp in range(3):
                            tr(ps7_b[:, (3 + pp) * 128:(4 + pp) * 128], w.qkb[:, 384 + pp * 128:384 + (pp + 1) * 128], ident_b[:, :],
                               [w.qkb, ident_b], [PS[7]])
                        cp("act", kT[:, :, it * 128:(it + 1) * 128], ps7_b[:, 384:768].rearrange("p (c t) -> p c t", c=3), [PS[7]], [kT])
                        dma("sp", f"o{par}", vscr[it * 128:(it + 1) * 128, :], w.v32[:, :], reads=[w.v32])

                    tiles = [("own", it, 128) for it in range(16)] + [("samp", 0, NS)]
                    for kind, it, nt in tiles:
                        par = tcount % 2
                        if tcount + 1 < len(tiles1):
                            load_x(tcount + 1)
                        cur = tcount
                        tcount += 1
                        w = wsets[par]
                        samp = kind == "samp"
                        tok0 = SEG if samp else it * 128
                        src = xs_d[0:NS, :] if samp else xp[it * 128:(it + 1) * 128, :]
                        do_front(cur)
                        for j in range(6):
                            proj(w, PS[1 + j], w_in_b, j * 384, 384 if j < 5 else 256, nt)
                        cp("act", w.u32[0:nt, :], PS[1][0:nt, 0:384], [PS[1]], [w.u32])
                        act(w.junk[0:nt, 0:384], PS[2][0:nt, 0:384], AF.Square, [PS[2]], [w.junk, w.ssqv], accum_out=w.ssqv[0:nt, :])
                        rstd_from_ssq(w.ssqv, w.stdv, w.rstdv, 1, 384, nt)
                        stt(w.vn32[0:nt, :], PS[2][0:nt, 0:384], w.rstdv[0:nt, :], gvB[0:nt, :], ALU.mult, ALU.mult,
                            [PS[2], w.rstdv, gvB], [w.vn32])
                        cp("pool", w.vnb[0:nt, :], w.vn32[0:nt, :], [w.vn32], [w.vnb])
                        if samp:
                            dma("sp", f"o{par}", gv_s[:, :], w.vn32[0:nt, :], reads=[w.vn32], is_output=True)
                        ropebase = 2 * SEG if samp else it * 128
                        qk_norm_rope(w, nt, True, ropebase)
                        if samp:
                            dma("sp", f"o{par}", wk_s[:, :], w.qk32[0:nt, 6:12, :].rearrange("p h d -> p (h d)"), reads=[w.qk32], is_output=True)
                        else:
                            dma("sp", f"o{par}", wk_p[it * 128:(it + 1) * 128, :], w.qk32[0:nt, 6:12, :].rearrange("p h d -> p (h d)"),
                                reads=[w.qk32], is_output=True)
                        cp("act", w.v32[0:nt, :], PS[5][0:nt, 0:384], [PS[5]], [w.v32])
                        if samp:
                            dma("sp", f"o{par}", wv_s[:, :], w.v32[0:nt, :], reads=[w.v32], is_output=True)
                            for i in range(2):
                                cp("pool", vsp[0:nt, :, i * 128:i * 128 + 64],
                                   w.v32[0:nt, :].rearrange("p (c i d) -> p c i d", c=3, i=2)[:, :, i, :], [w.v32], [vsp])
                        else:
                            dma("sp", f"o{par}", wv_p[it * 128:(it + 1) * 128, :], w.v32[0:nt, :], reads=[w.v32], is_output=True)
                            dma("sp", f"o{par}", vscr[SEG + it * 128:SEG + (it + 1) * 128, :], w.v32[0:nt, :], reads=[w.v32])
                        act(w.sqt[0:nt, 0:256], PS[6][0:nt, 0:256], AF.Square, [PS[6]], [w.sqt])
                        red(w.ssqm[0:nt, 0:4], w.sqt[0:nt, 0:256].rearrange("p (h d) -> p h d", d=64), [w.sqt], [w.ssqm])
                        act(w.stdm[0:nt, 0:4], w.ssqm[0:nt, 0:4], AF.Sqrt, [w.ssqm], [w.stdm], scale=1.0 / 64, bias=EPS)
                        recip(w.rstdm[0:nt, 0:4], w.stdm[0:nt, 0:4], [w.stdm], [w.rstdm])
                        tt("dve", w.qm32[0:nt, :, :], PS[6][0:nt, 0:256].rearrange("p (h d) -> p h d", d=64),
                           w.rstdm[0:nt, 0:4].unsqueeze(2).to_broadcast([nt, 4, 64]), ALU.mult, [PS[6], w.rstdm], [w.qm32])
                        tt("pool", w.qmb[0:nt, :].rearrange("p (h d) -> p h d", d=64), w.qm32[0:nt, :, :],
                           gqmB[0:nt, :].rearrange("p (h d) -> p h d", d=64), ALU.mult, [w.qm32, gqmB], [w.qmb])
                        do_front(cur + 1)
                        for pp in range(3):
                            tr(ps7_b[:, pp * 128:pp * 128 + nt], w.qkb[0:nt, pp * 128:(pp + 1) * 128], ident_b[0:nt, 0:nt], [w.qkb, ident_b], [PS[7]])
                        for pp in range(3):
                            tr(ps7_b[:, (3 + pp) * 128:(3 + pp) * 128 + nt], w.qkb[0:nt, 384 + pp * 128:384 + (pp + 1) * 128], ident_b[0:nt, 0:nt],
                               [w.qkb, ident_b], [PS[7]])
                        for pp in range(2):
                            tr(ps7_b[:, (6 + pp) * 128:(6 + pp) * 128 + nt], w.qmb[0:nt, pp * 128:(pp + 1) * 128], ident_b[0:nt, 0:nt],
                               [w.qmb, ident_b], [PS[7]])
                        p7v = ps7_b.rearrange("p (c t) -> p c t", c=8)
                        if samp:
                            cp("dve", qTs[:, :, :], p7v[:, 0:3, 0:nt], [PS[7]], [qTs])
                            cp("act", kTs[:, :, :], p7v[:, 3:6, 0:nt], [PS[7]], [kTs])
                        else:
                            cp("dve", qT[:, :, it * 128:(it + 1) * 128], p7v[:, 0:3, :], [PS[7]], [qT])
                            cp("act", kT[:, :, SEG + it * 128:SEG + (it + 1) * 128], p7v[:, 3:6, :], [PS[7]], [kT])
                        cp("dve", qmT[:, :, tok0:tok0 + nt], p7v[:, 6:8, 0:nt], [PS[7]], [qmT])
                        for gi in range(6):
                            lhs = bd_b[0:nt, gi, 0:nt] if samp else wsT_b[:, gi, :]
                            mm(PS[0][0:nt, gi * 64:(gi + 1) * 64], lhs, w.vnb[0:nt, gi * 64:(gi + 1) * 64], True, True,
                               [bd_b if samp else wsT_b, w.vnb], [PS[0]])
                        bsrc = bsS if samp else bsB
                        tt("dve", w.g1[0:nt, :], PS[0][0:nt, 0:384], bsrc[0:nt, :], ALU.add, [PS[0], bsrc], [w.g1])
                        tt("pool", w.oab[0:nt, :], w.g1[0:nt, :], w.u32[0:nt, :], ALU.mult, [w.g1, w.u32], [w.oab])
                        for pp in range(3):
                            tr(ps7_b[:, pp * 128:pp * 128 + nt], w.oab[0:nt, pp * 128:(pp + 1) * 128], ident_b[0:nt, 0:nt], [w.oab, ident_b], [PS[7]])
                        cp("act", oaT[:, :, tok0:tok0 + nt], p7v[:, 0:3, 0:nt], [PS[7]], [oaT])

                P.barrier()
                with contextlib.ExitStack() as s2:
                  if KSTOP >= 2:
                    maskA = sbuf(s2, "maskA", [128, 512], BF16); maskH = sbuf(s2, "maskH", [128, 512], BF16)
                    maskS = sbuf(s2, "maskS", [128, 216], BF16); maskN = sbuf(s2, "maskN", [64, 384], BF16)
                    dma("pool", "cp", maskA[:], maskA_d[:, :], writes=[maskA])
                    dma("pool", "cp", maskH[:], maskH_d[:, :], writes=[maskH])
                    dma("pool", "cp", maskS[:], maskS_d[:, :], writes=[maskS])
                    dma("pool", "cp", maskN[:], maskN_d[:, :], writes=[maskN])
                    Pf = [sbuf(s2, f"Pf{i}", [128, 512], BF16) for i in range(2)]
                    Pm = [sbuf(s2, f"Pm{i}", [128, 512], BF16) for i in range(2)]
                    rl = sbuf(s2, "rl", [128, 512]); rl2 = sbuf(s2, "rl2", [128, 512])
                    s2a = contextlib.ExitStack()
                    acc = sbuf(s2a, "acc", [128, 2, 3, SEG])
                    vbuf = sbuf(s2a, "vbuf", [128, 32, 3, 192], BF16)
                    P.op("pool", lambda h: h.memset(vbuf[:], 0.0), [], [vbuf.r])

                    def vload(dil):
                        own = vscr[SEG:2 * SEG, :]
                        halo = vscr[0:SEG, :]
                        if dil == 1:
                            srcs = [(own.rearrange("(n j) f -> j n f", j=128), 0, 16),
                                    (halo[SEG - 128:SEG, :].rearrange("(n j) f -> j n f", j=128), 16, 1)]
                        elif dil == 4:
                            srcs = [(own[n * 512:(n + 1) * 512, :].rearrange("(j r) f -> j r f", r=4), 4 * n, 4) for n in range(4)]
                            srcs += [(halo[SEG - 512:SEG, :].rearrange("(j r) f -> j r f", r=4), 16, 4)]
                        else:
                            srcs = [(own.rearrange("(j r) f -> j r f", r=16), 0, 16),
                                    (halo.rearrange("(j r) f -> j r f", r=16), 16, 16)]
                        for src, t0, ntile in srcs:
                            for i in range(2):
                                for c in range(3):
                                    s = src.rearrange("j t (c i d) -> j t c i d", c=3, i=2)[:, :, c, i, :]
                                    d = vbuf[:, t0:t0 + ntile, c, i * 128:i * 128 + 64]
                                    dma("pool", "v", d, s, writes=[vbuf])

                    def mem_attn(q0s):
                        nonlocal it2
                        for q0 in q0s:
                            for p in range(2):
                                psO = PS[2]; psL = PS[3]
                                k = 0
                                for mt in range(2):
                                    for i in range(2):
                                        sl = it2 % 2; it2 += 1
                                        mm(PS[sl][:, :], kmT[64 * i:64 * i + 64, p, mt * 128:(mt + 1) * 128], qmT[64 * i:64 * i + 64, p, q0:q0 + 512],
                                           True, True, [kmT, qmT], [PS[sl]])
                                        act(Pf[sl][:, :], PS[sl][:, :], AF.Exp, [PS[sl]], [Pf[sl]], scale=SCALE)
                                        mm(psO[:, :], vmp[:, mt, p, i * 64:i * 64 + 128], Pf[sl][:, :], k == 0, k == 3, [vmp, Pf[sl]], [psO])
                                        mm(psL[:, :], onesAB[:, i * 64:i * 64 + 128], Pf[sl][:, :], k == 0, k == 3, [onesAB, Pf[sl]], [psL])
                                        k += 1
                                recip(rl[:, :], psL[:, :], [psL], [rl])
                                tt("dve", mixM[:, p, q0:q0 + 512], psO[:, :], rl[:, :], ALU.mult, [psO, rl], [mixM])

                    it2 = 0
                    dils = (1, 4, 16)
                    vload(dils[0])
                    for bi, dil in enumerate(dils):
                        nblk = 16 // dil
                        bspan = 128 * dil
                        for r in range(dil):
                            for n in range(nblk):
                                qs = slice(n * bspan + r, (n + 1) * bspan, dil)
                                kc = slice(SEG + n * bspan + r, SEG + (n + 1) * bspan, dil)
                                kp = slice(SEG + (n - 1) * bspan + r, SEG + n * bspan, dil)
                                vt_c = n * dil + r
                                vt_p = (n - 1) * dil + r if n > 0 else 16 + r
                                msk = maskH if n == 0 else maskA
                                for p in range(3):
                                    sl = it2 % 2; it2 += 1
                                    psSi = (PS[sl], PS[4 + sl]); psO = PS[2 + sl]
                                    for kt, kcols in enumerate((kp, kc)):
                                        for i in range(2):
                                            mm(psSi[i][:, kt * 128:(kt + 1) * 128], kT[64 * i:64 * i + 64, p, kcols], qT[64 * i:64 * i + 64, p, qs],
                                               True, True, [kT, qT], [psSi[i]])
                                    for i in range(2):
                                        act(Pf[sl][:, i * 256:(i + 1) * 256], psSi[i][:, 0:256], AF.Exp, [psSi[i]], [Pf[sl]], scale=SCALE)
                                    tt("dve", Pm[sl][:, :], Pf[sl][:, :], msk[:, :], ALU.mult, [Pf[sl], msk], [Pm[sl]])
                                    for part in range(2):
                                        k = 0
                                        for kt, vt in enumerate((vt_p, vt_c)):
                                            for i in range(2):
                                                s = i * 2 + kt
                                                lhs = vbuf[:, vt, p, i * 64:i * 64 + 128] if part == 0 else onesAB[:, i * 64:i * 64 + 128]
                                                mm(psO[:, part * 128:(part + 1) * 128], lhs, Pm[sl][:, s * 128:(s + 1) * 128], k == 0, k == 3,
                                                   [vbuf if part == 0 else onesAB, Pm[sl]], [psO])
                                                k += 1
                                    accv = acc[:, :, p, qs]
                                    pov = psO[:, 0:256].rearrange("p (a q) -> p a q", a=2)
                                    if bi == 0:
                                        cp("dve", accv, pov, [psO], [acc])
                                    else:
                                        tt("dve", accv, pov, accv, ALU.add, [psO, acc], [acc])
                        if bi < 2:
                            vload(dils[bi + 1])
                            mem_attn((0, 512) if bi == 0 else (1024, 1536))
                    for p in range(3):
                        for qi, q0 in enumerate(range(0, SEG, 512)):
                            rlx = rl if qi % 2 == 0 else rl2
                            recip(rlx[:, :], acc[:, 1, p, q0:q0 + 512], [acc], [rlx])
                            tt("pool" if qi % 2 == 0 else "dve", mixB[:, p, q0:q0 + 512], acc[:, 0, p, q0:q0 + 512], rlx[:, :], ALU.mult,
                               [acc, rlx], [mixB])
                    P.barrier()
                    s2a.close()

                    kms = sbuf(s2, "kms", [128, 16, 2, 256], BF16)
                    vms = sbuf(s2, "vms", [128, 16, 2, 2, 192], BF16)
                    dma("pool", "cp", kms[:], cmkT[:, :, :, :], writes=[kms])
                    P.op("pool", lambda h: h.memset(vms[:], 0.0), [], [vms.r])
                    for mt in range(2):
                        for i in range(2):
                            for c in range(2):
                                dma("pool", "cp", vms[:, :, mt, c, i * 128:i * 128 + 64],
                                    cmv[:, :, mt, :].rearrange("m b (c i d) -> m b c i d", c=2, i=2)[:, :, c, i, :], writes=[vms])
                    for b in range(16):
                        for p in range(2):
                            for mt in range(2):
                                for i in range(2):
                                    col = (b * 2 + p) * 2 + mt
                                    mm(PS[i][:, col * 4:col * 4 + 4], kms[64 * i:64 * i + 64, b, p, mt * 128:(mt + 1) * 128],
                                       qmT[64 * i:64 * i + 64, p, SEG + 4 * b:SEG + 4 * b + 4], True, True, [kms, qmT], [PS[i]])
                    for i in range(2):
                        act(Pf[0][:, i * 256:(i + 1) * 256], PS[i][:, 0:256], AF.Exp, [PS[i]], [Pf[0]], scale=SCALE)
                    psO = PS[2]
                    for part in range(2):
                        for b in range(16):
                            for p in range(2):
                                k = 0
                                for mt in range(2):
                                    for i in range(2):
                                        col = i * 64 + (b * 2 + p) * 2 + mt
                                        lhs = vms[:, b, mt, p, i * 64:i * 64 + 128] if part == 0 else onesAB[:, i * 64:i * 64 + 128]
                                        mm(psO[:, part * 128 + p * 64 + 4 * b:part * 128 + p * 64 + 4 * b + 4], lhs, Pf[0][:, col * 4:col * 4 + 4],
                                           k == 0, k == 3, [vms if part == 0 else onesAB, Pf[0]], [psO])
                                        k += 1
                    recip(rl[:, 0:128], psO[:, 128:256], [psO], [rl])
                    tt("dve", mixM[:, :, SEG:SEG + NS], psO[:, 0:128].rearrange("p (c q) -> p c q", c=2),
                       rl[:, 0:128].rearrange("p (c q) -> p c q", c=2), ALU.mult, [psO, rl], [mixM])

                    kws = [sbuf(s2, f"kws{i}", [128, 3, 1152], BF16) for i in range(2)]
                    vws = [sbuf(s2, f"vws{i}", [128, 9, 3, 192], BF16) for i in range(2)]
                    for i in range(2):
                        P.op("pool", lambda h, i=i: h.memset(vws[i][:], 0.0), [], [vws[i].r])
                    onew = sbuf(s2, "onew", [128, 2, 3, 64])
                    psNi = (PS[4], PS[3])
                    for p in range(3):
                        for i in range(2):
                            mm(psNi[i][0:64, p * 64:(p + 1) * 64], kTs[64 * i:64 * i + 64, p, :], qTs[64 * i:64 * i + 64, p, :],
                               True, True, [kTs, qTs], [psNi[i]])
                    for i in range(2):
                        act(Pf[1][0:64, i * 192:(i + 1) * 192], psNi[i][0:64, 0:192], AF.Exp, [psNi[i]], [Pf[1]], scale=SCALE)
                    tt("pool", Pm[1][0:64, 0:384], Pf[1][0:64, 0:384], maskN[:, :], ALU.mult, [Pf[1], maskN], [Pm[1]])
                    psNO = PS[5]
                    for part in range(2):
                        for p in range(3):
                            for i in range(2):
                                lhs = vsp[0:64, p, i * 64:i * 64 + 128] if part == 0 else onesAB[0:64, i * 64:i * 64 + 128]
                                mm(psNO[:, (part * 3 + p) * 64:(part * 3 + p + 1) * 64], lhs, Pm[1][0:64, (i * 3 + p) * 64:(i * 3 + p + 1) * 64],
                                   i == 0, i == 1, [vsp if part == 0 else onesAB, Pm[1]], [psNO])
                    cp("dve", onew[:, :, :, :], psNO[:, 0:384].rearrange("p (a c q) -> p a c q", a=2, c=3), [psNO], [onew])
                    psW = PS[6]
                    for b in range(16):
                        sl = b % 2
                        dma("pool", f"kv{sl}", kws[sl][:], cwkT[b], writes=[kws[sl]])
                        for i in range(2):
                            for c in range(3):
                                dma("pool", f"kv{sl}", vws[sl][:, :, c, i * 128:i * 128 + 64],
                                    cwv[b].rearrange("k t (c i d) -> k t c i d", c=3, i=2)[:, :, c, i, :], writes=[vws[sl]])
                        psSi = (PS[sl], PS[2 + sl])
                        for t9 in range(9):
                            for p in range(3):
                                for i in range(2):
                                    col = t9 * 3 + p
                                    mm(psSi[i][:, col * 4:col * 4 + 4], kws[sl][64 * i:64 * i + 64, p, t9 * 128:(t9 + 1) * 128],
                                       qTs[64 * i:64 * i + 64, p, 4 * b:4 * b + 4], True, True, [kws[sl], qTs], [psSi[i]])
                        for i in range(2):
                            act(Pf[sl][:, i * 108:(i + 1) * 108], psSi[i][:, 0:108], AF.Exp, [psSi[i]], [Pf[sl]], scale=SCALE)
                        tt("pool", Pm[sl][:, 0:216], Pf[sl][:, 0:216], maskS[:, :], ALU.mult, [Pf[sl], maskS], [Pm[sl]])
                        for part in range(2):
                            for p in range(3):
                                k = 0
                                for t9 in range(9):
                                    for i in range(2):
                                        col = i * 27 + t9 * 3 + p
                                        lhs = vws[sl][:, t9, p, i * 64:i * 64 + 128] if part == 0 else onesAB[:, i * 64:i * 64 + 128]
                                        c0 = (part * 3 + p) * 64 + 4 * b
                                        mm(psW[:, c0:c0 + 4], lhs, Pm[sl][:, col * 4:col * 4 + 4], k == 0, k == 17,
                                           [vws[sl] if part == 0 else onesAB, Pm[sl]], [psW])
                                        k += 1
                    tt("dve", onew[:, :, :, :], psW[:, 0:384].rearrange("p (a c q) -> p a c q", a=2, c=3), onew[:, :, :, :], ALU.add,
                       [psW, onew], [onew])
                    recip(rl[:, 0:192], onew[:, 1, :, :].rearrange("p c q -> p (c q)"), [onew], [rl])
                    tt("dve", mixB[:, :, SEG:SEG + NS], onew[:, 0, :, :], rl[:, 0:192].rearrange("p (c q) -> p c q", c=3), ALU.mult,
                       [onew, rl], [mixB])
                P.barrier()
            with contextlib.ExitStack() as s3:
              if KSTOP >= 3:
                w_out_b = sbuf(s3, "w_out_b", [128, 8, D], BF16)
                dma("pool", "cp", w_out_b[:], w_out.rearrange("(c p) n -> p c n", p=128), writes=[w_out_b])
                xt3 = [sbuf(s3, f"xt3_{i}", [128, D]) for i in range(2)]
                x13 = [sbuf(s3, f"x13_{i}", [128, D]) for i in range(2)]
                srcT = [(oaT, 0), (oaT, 1), (oaT, 2), (mixB, 0), (mixB, 1), (mixB, 2), (mixM, 0), (mixM, 1)]
                for j in range(17):
                    nt = 128 if j < 16 else NS
                    par = j % 2
                    src = xp[j * 128:(j + 1) * 128, :] if j < 16 else xs_d[0:NS, :]
                    dma("sp", f"x{par}", xt3[par][0:nt, :], src, writes=[xt3[par]])
                    for hf in range(2):
                        bank = PS[(j * 2 + hf) % 4]
                        for c, (sb_, ci) in enumerate(srcT):
                            mm(bank[0:nt, :], sb_[:, ci, j * 128:j * 128 + nt], w_out_b[:, c, hf * 512:(hf + 1) * 512], c == 0, c == 7,
                               [sb_, w_out_b], [bank])
                        tt("dve", x13[par][0:nt, hf * 512:(hf + 1) * 512], bank[0:nt, :], xt3[par][0:nt, hf * 512:(hf + 1) * 512], ALU.add,
                           [bank, xt3[par]], [x13[par]])
                    dma("sp", f"o{par}", x1scr[j * 128:j * 128 + nt, :], x13[par][0:nt, :], reads=[x13[par]])
                P.barrier()

        NSLOT = 54
        SR = 384
        QT = SR // 128
        NROW = NSLOT * SR
        IO = bass.IndirectOffsetOnAxis
        w_gu2 = w_gu.rearrange("e r n -> (e r) n")
        w_dn2 = w_dn.rearrange("e r n -> (e r) n")

        regs = {}

        def _mkregs(h):
            for name, val in (("tok", NTOK + 127), ("w", NEXP * D - 1), ("row", NROW - 1)):
                r = h.alloc_register("bnd_" + name)
                h.reg_mov(r, val)
                regs[name] = r
        P.ops["pool"].append(_mkregs)

        def gather(sem, out, src, idx, reads, writes, bound):
            P.dmaf("pool", sem, lambda h: h.indirect_dma_start(out=out, out_offset=None, in_=src, in_offset=IO(ap=idx, axis=0),
                                                             bounds_check=regs[bound], oob_is_err=False),
                   [b.r for b in reads], [b.r for b in writes])

        with contextlib.ExitStack() as s4:
          if KSTOP >= 4:
            comb = sbuf(s4, "comb", [128, 17, 32]); msel = sbuf(s4, "msel", [128, 17, 32])
            pos4i = sbuf(s4, "pos4i", [128, 17 * 4], I32); tokid = sbuf(s4, "tokid", [128, 17], I32)
            onehot = sbuf(s4, "onehot", [128, NSLOT, 32])
            idxw = sbuf(s4, "idxw", [128, NSLOT * 8], I32); idxall = sbuf(s4, "idxall", [128, NSLOT * QT], I32)
            bguT = sbuf(s4, "bguT", [128, NEXP, 16])
            mc = sbuf(s4, "mc", [128, 111])
            siota = mc[:, 0:54]; eiota = mc[:, 54:86]; wrow = mc[:, 86:94]
            dma("sp", "c", mc[:], mconst_d[:, :], writes=[mc])
            dma("sp", "c", bguT[:], bguT_d[:, :, :], writes=[bguT])
            ts("dve", bguT[:, :, 8:16], bguT[:, :, 8:16], 1.0, None, ALU.add, None, [bguT], [bguT])
            cp("dve", tokid[:, :], mc[:, 94:111], [mc], [tokid])

            with contextlib.ExitStack() as s4a:
                g2c = sbuf(s4a, "g2c", [128, 8]); g2B = sbuf(s4a, "g2B", [128, D]); wr32 = sbuf(s4a, "wr32", [128, 8, 32])
                brB = sbuf(s4a, "brB", [128, 32]); bdn32 = sbuf(s4a, "bdn32", [32, D])
                triu = sbuf(s4a, "triu", [128, 128]); ones_f = sbuf(s4a, "ones_f", [128, 128])
                zb = sbuf(s4a, "zb", [128, D], BF16); zf = sbuf(s4a, "zf", [128, 32]); fillf = sbuf(s4a, "fillf", [128, NSLOT * QT])
                filli = sbuf(s4a, "filli", [128, NSLOT * QT], I32)
                dma("sp", "c", g2c[:], norm2_g[:, :], writes=[g2c])
                dma("sp", "c", g2B[:], norm2_row.partition_broadcast(128), writes=[g2B])
                dma("sp", "c", wr32[:], w_router.rearrange("(c p) n -> p c n", p=128), writes=[wr32])
                dma("sp", "c", brB[:], b_router.partition_broadcast(128), writes=[brB])
                dma("sp", "c", bdn32[:], b_dn[:, :], writes=[bdn32])
                dma("sp", "c", triu[:], triu_d[:, :], writes=[triu])
                P.op("pool", lambda h: h.memset(ones_f[:], 1.0), [], [ones_f.r])
                P.op("pool", lambda h: h.memset(zb[:], 0.0), [], [zb.r])
                P.op("pool", lambda h: h.memset(zf[:], 0.0), [], [zf.r])
                P.op("pool", lambda h: h.memset(fillf[:], float(NTOK)), [], [fillf.r])
                cp("dve", filli[:, :], fillf[:, :], [fillf], [filli])
                dma("sp", "o0", hn_d[NTOK:NTOK + 128, :], zb[:, :], reads=[zb])
                dma("sp", "o0", comb_d[NTOK:NTOK + 128, :], zf[:, :], reads=[zf])
                Rinv = Res("inv_d")
                P.dma("pool", "g", inv_d.rearrange("(p c) o -> p (c o)", p=128), filli[:, :], [filli.r], [Rinv])

                sm = lambda name, shape, dt=F32: sbuf(s4a, name, shape, dt)

                class TS:
                    pass
                tsets = []
                for gi in range(2):
                    T = TS(); sfx = f"_{gi}"
                    T.junk = sbuf(s4a, "junk4" + sfx, [128, D], BF16); T.hnb = sbuf(s4a, "hnb" + sfx, [128, D], BF16)
                    T.ssq = sbuf(s4a, "ssq4" + sfx, [128, 1]); T.std = sbuf(s4a, "std4" + sfx, [128, 1]); T.rstd = sbuf(s4a, "rstd4" + sfx, [128, 1])
                    T.hn32 = sbuf(s4a, "hn32" + sfx, [128, D]); T.hT32 = sbuf(s4a, "hT32" + sfx, [128, 8, 128])
                    T.lg = sbuf(s4a, "lg" + sfx, [128, 32]); T.m8 = sbuf(s4a, "m8" + sfx, [128, 8]); T.nmx = sbuf(s4a, "nmx" + sfx, [128, 1])
                    T.ex = sbuf(s4a, "ex" + sfx, [128, 32]); T.cw = sbuf(s4a, "cw" + sfx, [128, 32])
                    T.den = sbuf(s4a, "den" + sfx, [128, 1]); T.rden = sbuf(s4a, "rden" + sfx, [128, 1])
                    T.combT = sbuf(s4a, "combT" + sfx, [32, 128])
                    T.ps = [PS[4 * gi + i] for i in range(4)]
                    T.osem = f"o{gi}"
                    tsets.append(T)
                xts = [sbuf(s4a, f"x1t{i}", [128, D]) for i in range(4)]

                def load_x1(j):
                    nt = 128 if j < 16 else NS
                    dma("sp", "m", xts[j % 4][0:nt, :], x1scr[j * 128:j * 128 + nt, :], writes=[xts[j % 4]])

                def setup_tile(j, T):
                    nt = 128 if j < 16 else NS
                    r0 = j * 128
                    xt = xts[j % 4]
                    act(T.junk[0:nt, :], xt[0:nt, :], AF.Square, [xt], [T.junk, T.ssq], accum_out=T.ssq[0:nt, :])
                    yield
                    rstd_from_ssq(T.ssq, T.std, T.rstd, 1, D, nt)
                    yield
                    act(T.hn32[0:nt, :], xt[0:nt, :], AF.Copy, [xt, T.rstd], [T.hn32], scale=T.rstd[0:nt, :])
                    yield
                    tt("pool", T.hnb[0:nt, :], T.hn32[0:nt, :], g2B[0:nt, :], ALU.mult, [T.hn32, g2B], [T.hnb])
                    dma("sp", T.osem, hn_d[r0:r0 + nt, :], T.hnb[0:nt, :], reads=[T.hnb])
                    for c in range(8):
                        bank = T.ps[c // 4]
                        tr(bank[:, (c % 4) * 128:(c % 4) * 128 + nt], T.hn32[0:nt, c * 128:(c + 1) * 128], ident_f[0:nt, 0:nt], [T.hn32, ident_f], [bank])
                    yield
                    for hb in range(2):
                        bank = T.ps[hb]
                        bv = bank[:, :].rearrange("p (c t) -> p c t", c=4)[:, :, 0:nt]
                        gb = g2c[:, hb * 4:hb * 4 + 4].unsqueeze(2).to_broadcast([128, 4, nt])
                        tt("dve", T.hT32[:, hb * 4:hb * 4 + 4, 0:nt], bv, gb, ALU.mult, [bank, g2c], [T.hT32])
                    yield
                    for c in range(8):
                        mm(T.ps[2][0:nt, 0:32], T.hT32[:, c, 0:nt], wr32[:, c, :], c == 0, c == 7, [T.hT32, wr32], [T.ps[2]])
                    yield
                    tt("dve", T.lg[0:nt, :], T.ps[2][0:nt, 0:32], brB[0:nt, :], ALU.add, [T.ps[2], brB], [T.lg])
                    P.op("dve", lambda h: h.max(out=T.m8[0:nt, :], in_=T.lg[0:nt, :]), [T.lg.r], [T.m8.r])
                    ts("dve", T.nmx[0:nt, :], T.m8[0:nt, 0:1], -1.0, None, ALU.mult, None, [T.m8], [T.nmx])
                    yield
                    act(T.ex[0:nt, :], T.lg[0:nt, :], AF.Exp, [T.lg, T.nmx], [T.ex], bias=T.nmx[0:nt, :])
                    yield
                    stt(T.cw[0:nt, :], T.lg[0:nt, :], T.m8[0:nt, 3:4], T.ex[0:nt, :], ALU.is_ge, ALU.mult, [T.lg, T.m8, T.ex], [T.cw, T.den],
                        accum_out=T.den[0:nt, :])
                    recip(T.rden[0:nt, :], T.den[0:nt, :], [T.den], [T.rden])
                    ts("dve", comb[0:nt, j, :], T.cw[0:nt, :], T.rden[0:nt, :], None, ALU.mult, None, [T.cw, T.rden], [comb])
                    ts("dve", msel[0:nt, j, :], T.lg[0:nt, :], T.m8[0:nt, 3:4], None, ALU.is_ge, None, [T.lg, T.m8], [msel])
                    dma("sp", T.osem, comb_d[r0:r0 + nt, :], comb[0:nt, j, :], reads=[comb])
                    tr(T.ps[2][0:32, 128:128 + nt], comb[0:nt, j, :], ident_f[0:nt, 0:nt], [comb, ident_f], [T.ps[2]])
                    yield
                    cp("act", T.combT[:, 0:nt], T.ps[2][0:32, 128:128 + nt], [T.ps[2]], [T.combT])
                    yield
                    for hf in range(2):
                        bank = T.ps[3] if hf == 0 else T.ps[0]
                        mm(bank[0:nt, :], T.combT[:, 0:nt], bdn32[:, hf * 512:(hf + 1) * 512], True, True, [T.combT, bdn32], [bank])
                    yield
                    for hf in range(2):
                        bank = T.ps[3] if hf == 0 else T.ps[0]
                        tt("dve", xt[0:nt, hf * 512:(hf + 1) * 512], bank[0:nt, :], xt[0:nt, hf * 512:(hf + 1) * 512], ALU.add, [bank, xt], [xt])
                    dma("sp", T.osem, x1scr[r0:r0 + nt, :], xt[0:nt, :], reads=[xt])

                load_x1(0); load_x1(1)
                for j0 in range(0, 17, 2):
                    for j in (j0 + 2, j0 + 3):
                        if j < 17:
                            load_x1(j)
                    gens = [setup_tile(j, tsets[j - j0]) for j in (j0, j0 + 1) if j < 17]
                    while gens:
                        for g in list(gens):
                            try:
                                next(g)
                            except StopIteration:
                                gens.remove(g)

                cnt = sm("cnt", [128, 32]); nsl = [sm("nslA", [128, 32]), sm("nslB", [128, 32])]
                cum = [sm("cumA", [128, 32]), sm("cumB", [128, 32])]; st512 = sm("st512", [128, 32])
                tmp3 = sm("tmp3", [128, NSLOT, 32]); eall = sm("eall", [128, NSLOT]); idxwf = sm("idxwf", [128, NSLOT, 8])
                vt = [sm("vtA", [128, 32]), sm("vtB", [128, 32])]; p8 = sm("p8", [128, 8]); p4f = sm("p4f", [128, 4])
                ones_b = sm("ones_b", [128, 128], BF16); triu_b = sm("triu_b", [128, 128], BF16); msel_b = sm("msel_b", [128, 17, 32], BF16)
                cp("act", ones_b[:, :], ones_f[:, :], [ones_f], [ones_b])
                cp("act", triu_b[:, :], triu[:, :], [triu], [triu_b])
                cp("dve", msel_b[:, 0:16, :], msel[:, 0:16, :], [msel], [msel_b])
                cp("dve", msel_b[0:NS, 16, :], msel[0:NS, 16, :], [msel], [msel_b])
                for j in range(17):
                    nt = 128 if j < 16 else NS
                    mm(PS[0][:, 0:32], ones_b[0:nt, :], msel_b[0:nt, j, :], j == 0, j == 16, [ones_b, msel_b], [PS[0]])
                for j in range(17):
                    nt = 128 if j < 16 else NS
                    bank = PS[1] if j < 16 else PS[2]
                    dst = bank[0:nt, (j % 16) * 32:(j % 16) * 32 + 32]
                    for jp in range(j):
                        mm(dst, ones_b[:, 0:nt], msel_b[:, jp, :], jp == 0, False, [ones_b, msel_b], [bank])
                    mm(dst, triu_b[0:nt, 0:nt], msel_b[0:nt, j, :], j == 0, True, [triu_b, msel_b], [bank])
                cp("dve", cnt[:, :], PS[0][:, 0:32], [PS[0]], [cnt])
                ts("dve", nsl[0][:, :], cnt[:, :], 0.0, None, ALU.is_gt, None, [cnt], [nsl[0]])
                a = 0
                for k in range(1, 6):
                    stt(nsl[1 - a][:, :], cnt[:, :], float(SR) * k, nsl[a][:, :], ALU.is_gt, ALU.add, [cnt, nsl[a]], [nsl[1 - a]])
                    a = 1 - a
                nslots = nsl[a]
                cp("dve", cum[0][:, :], nslots[:, :], [nslots], [cum[0]])
                b_ = 0
                for sh in (1, 2, 4, 8, 16):
                    cp("dve", cum[1 - b_][:, 0:sh], cum[b_][:, 0:sh], [cum[b_]], [cum[1 - b_]])
                    tt("dve", cum[1 - b_][:, sh:32], cum[b_][:, sh:32], cum[b_][:, 0:32 - sh], ALU.add, [cum[b_]], [cum[1 - b_]])
                    b_ = 1 - b_
                cumi = cum[b_]
                tt("dve", st512[:, :], cumi[:, :], nslots[:, :], ALU.subtract, [cumi, nslots], [st512])
                ts("dve", st512[:, :], st512[:, :], float(SR), None, ALU.mult, None, [st512], [st512])
                tt("dve", tmp3[:, :, :], cumi[:, :].unsqueeze(1).to_broadcast([128, NSLOT, 32]),
                   siota.unsqueeze(2).to_broadcast([128, NSLOT, 32]), ALU.is_le, [cumi, mc], [tmp3])
                red(eall[:, :], tmp3[:, :, :], [tmp3], [eall])
                tt("dve", onehot[:, :, :], eiota.unsqueeze(1).to_broadcast([128, NSLOT, 32]),
                   eall[:, :].unsqueeze(2).to_broadcast([128, NSLOT, 32]), ALU.is_equal, [mc, eall], [onehot])
                stt(idxwf[:, :, :], eall[:, :].unsqueeze(2).to_broadcast([128, NSLOT, 8]), 1024.0,
                    wrow.unsqueeze(1).to_broadcast([128, NSLOT, 8]), ALU.mult, ALU.add, [eall, mc], [idxwf])
                cp("dve", idxw[:, :], idxwf[:, :, :].rearrange("p s c -> p (s c)"), [idxwf], [idxw])
                for j in range(17):
                    nt = 128 if j < 16 else NS
                    bank = PS[1] if j < 16 else PS[2]
                    rk = bank[0:nt, (j % 16) * 32:(j % 16) * 32 + 32]
                    stt(vt[0][0:nt, :], rk, 1.0, st512[0:nt, :], ALU.add, ALU.add, [bank, st512], [vt[0]])
                    tt("dve", vt[1][0:nt, :], vt[0][0:nt, :], msel[0:nt, j, :], ALU.mult, [vt[0], msel], [vt[1]])
                    P.op("dve", lambda h, nt=nt: h.max(out=p8[0:nt, :], in_=vt[1][0:nt, :]), [vt[1].r], [p8.r])
                    ts("dve", p4f[0:nt, :], p8[0:nt, 0:4], -1.0, None, ALU.add, None, [p8], [p4f])
                    cp("dve", pos4i[0:nt, j * 4:j * 4 + 4], p4f[0:nt, :], [p4f], [pos4i])
                for j in range(17):
                    nt = 128 if j < 16 else NS
                    for k in range(4):
                        P.dmaf("pool", "g", lambda h, nt=nt, j=j, k=k: h.indirect_dma_start(
                            out=inv_d[:, :], out_offset=IO(ap=pos4i[0:nt, j * 4 + k:j * 4 + k + 1], axis=0), in_=tokid[0:nt, j:j + 1], in_offset=None,
                            bounds_check=regs["row"], oob_is_err=False), [pos4i.r, tokid.r, Rinv], [])
                for q6 in range(9):
                    P.dma("sp", "m", idxall[:, q6 * 18:(q6 + 1) * 18],
                          inv_d[q6 * 2304:(q6 + 1) * 2304, :].rearrange("(s p) o -> p (s o)", p=128), [], [idxall.r, Rinv],
                          allow_slow_non_contiguous=True)
            P.barrier()

            with contextlib.ExitStack() as s4b:
                wgu = [sbuf(s4b, f"wgu{i}", [128, 8, 2048], BF16) for i in range(2)]
                wdn = [sbuf(s4b, f"wdn{i}", [128, 8, D], BF16) for i in range(2)]
                xg = [sbuf(s4b, f"xg{i}", [128, QT, D], BF16) for i in range(2)]
                hgT = [sbuf(s4b, f"hgT{i}", [128, 8, SR], BF16) for i in range(2)]
                gcomb = [sbuf(s4b, f"gcomb{i}", [128, QT, 32]) for i in range(2)]
                bias_s = [sbuf(s4b, f"bias_s{i}", [128, 16]) for i in range(2)]
                gate_s = [sbuf(s4b, f"gate_s{i}", [128, QT]) for i in range(2)]
                btmp = sbuf(s4b, "btmp", [128, 16, 32]); gtmp = sbuf(s4b, "gtmp", [128, QT, 32])
                gsb = [sbuf(s4b, f"gsb{i}", [128, SR]) for i in range(2)]
                ssb = [sbuf(s4b, f"ssb{i}", [128, SR]) for i in range(2)]
                usb = [sbuf(s4b, f"usb{i}", [128, SR]) for i in range(2)]
                actT = [sbuf(s4b, f"actT{i}", [128, 8, SR], BF16) for i in range(2)]
                res_sb = [sbuf(s4b, f"res_sb{i}", [128, D]) for i in range(3)]

                order = []
                for i in range(NSLOT // 3):
                    order += [i, 2 * (NSLOT // 3) + i]
                order += list(range(NSLOT // 3, 2 * (NSLOT // 3)))
                assert sorted(order) == list(range(NSLOT))

                def tok_loads(i):
                    s = order[i]
                    par = i % 2
                    for q in range(QT):
                        gather(f"kv{par}", xg[par][:, q, :], hn_d[:, :], idxall[:, s * QT + q:s * QT + q + 1], [idxall], [xg[par]], "tok")
                    for q in range(QT):
                        gather(f"kv{par}", gcomb[par][:, q, :], comb_d[:, :], idxall[:, s * QT + q:s * QT + q + 1], [idxall], [gcomb[par]], "tok")

                def wgu_loads(i):
                    s = order[i]
                    par = i % 2
                    for c in range(8):
                        gather(f"w{par}", wgu[par][:, c, :], w_gu2[:, :], idxw[:, s * 8 + c:s * 8 + c + 1], [idxw], [wgu[par]], "w")

                def wdn_loads(i):
                    s = order[i]
                    par = i % 2
                    for c in range(8):
                        gather(f"wd{par}", wdn[par][:, c, :], w_dn2[:, :], idxw[:, s * 8 + c:s * 8 + c + 1], [idxw], [wdn[par]], "w")

                def slot_transposes(i):
                    par = i % 2
                    for q in range(QT):
                        bank = PS[6 + q % 2]
                        bb = bank[:].bitcast(BF16)
                        for c in range(8):
                            tr(bb[:, c * 128:(c + 1) * 128], xg[par][:, q, c * 128:(c + 1) * 128], ident_b[:, :], [xg[par], ident_b], [bank])
                        cp("act" if q % 2 == 0 else "dve", hgT[par][:, :, q * 128:(q + 1) * 128], bb.rearrange("p (c t) -> p c t", c=8),
                           [bank], [hgT[par]])

                tok_loads(0); wgu_loads(0); wdn_loads(0)
                tok_loads(1) if NSLOT > 1 else None
                wgu_loads(1); wdn_loads(1)
                slot_transposes(0)
                for si in range(NSLOT):
                    s = order[si]
                    par = si % 2
                    tt("dve", btmp[:, :, :], bguT[:, :, :].rearrange("p e c -> p c e"),
                       onehot[:, s, :].unsqueeze(1).to_broadcast([128, 16, 32]), ALU.mult, [bguT, onehot], [btmp])
                    red(bias_s[par][:, :], btmp[:, :, :], [btmp], [bias_s[par]])
                    tt("dve", gtmp[:, :, :], gcomb[par][:, :, :], onehot[:, s, :].unsqueeze(1).to_broadcast([128, QT, 32]), ALU.mult,
                       [gcomb[par], onehot], [gtmp])
                    red(gate_s[par][:, :], gtmp[:, :, :], [gtmp], [gate_s[par]])
                    if si + 2 < NSLOT:
                        tok_loads(si + 2)
                    aT = actT[par]
                    for c in range(8):
                        db = c % 2
                        bg = PS[db * 2]; bu = PS[db * 2 + 1]
                        for k in range(8):
                            mm(bg[:, 0:SR], wgu[par][:, k, c * 128:(c + 1) * 128], hgT[par][:, k, :], k == 0, k == 7, [wgu[par], hgT[par]], [bg])
                        for k in range(8):
                            mm(bu[:, 0:SR], wgu[par][:, k, 1024 + c * 128:1024 + (c + 1) * 128], hgT[par][:, k, :], k == 0, k == 7,
                               [wgu[par], hgT[par]], [bu])
                        ts("dve", gsb[db][:, :], bg[:, 0:SR], bias_s[par][:, c:c + 1], 7.0, ALU.add, ALU.min, [bg, bias_s[par]], [gsb[db]])
                        act(ssb[db][:, :], gsb[db][:, :], AF.Sigmoid, [gsb[db]], [ssb[db]], scale=1.702)
                        ts("dve", usb[db][:, :], bu[:, 0:SR], bias_s[par][:, 8 + c:9 + c], -6.0, ALU.add, ALU.max, [bu, bias_s[par]], [usb[db]])
                        stt(usb[db][:, :], usb[db][:, :], 8.0, gsb[db][:, :], ALU.min, ALU.mult, [usb[db], gsb[db]], [usb[db]])
                        tt("dve", aT[:, c, :], usb[db][:, :], ssb[db][:, :], ALU.mult, [usb[db], ssb[db]], [aT])
                    if si + 2 < NSLOT:
                        wgu_loads(si + 2)
                    if si + 1 < NSLOT:
                        slot_transposes(si + 1)
                    for q in range(QT):
                        ri = (si * QT + q) % 3
                        rs = res_sb[ri]
                        for hf in range(2):
                            bank = PS[4 + hf]
                            for k in range(8):
                                mm(bank[:, :], aT[:, k, q * 128:(q + 1) * 128], wdn[par][:, k, hf * 512:(hf + 1) * 512], k == 0, k == 7,
                                   [aT, wdn[par]], [bank])
                            if hf == 0:
                                act(rs[:, 0:512], bank[:, :], AF.Copy, [bank, gate_s[par]], [rs], scale=gate_s[par][:, q:q + 1])
                            else:
                                ts("dve", rs[:, 512:1024], bank[:, :], gate_s[par][:, q:q + 1], None, ALU.mult, None, [bank, gate_s[par]], [rs])
                        dma("sp", f"r{ri}", res_d[(s * QT + q) * 128:(s * QT + q + 1) * 128, :], rs[:, :], reads=[rs])
                    if si + 2 < NSLOT:
                        wdn_loads(si + 2)
            P.barrier()

            with contextlib.ExitStack() as s4c:
                NCB = 4
                yb = [sbuf(s4c, f"yb{i}", [128, D]) for i in range(NCB)]
                rg = [[sbuf(s4c, f"rg{i}_{k}", [128, D]) for k in range(4)] for i in range(NCB)]
                csem = ("kv0", "kv1", "w0", "w1")
                xsem = ("x0", "x1", "o0", "o1")

                def comb_loads(j):
                    nt = 128 if j < 16 else NS
                    par = j % NCB
                    dma("sp", xsem[par], yb[par][0:nt, :], x1scr[j * 128:j * 128 + nt, :], writes=[yb[par]])
                    for k in range(4):
                        gather(csem[par], rg[par][k][0:nt, :], res_d[:, :], pos4i[0:nt, j * 4 + k:j * 4 + k + 1], [pos4i], [rg[par][k]], "row")
                for j in range(NCB - 1):
                    comb_loads(j)
                for j in range(17):
                    nt = 128 if j < 16 else NS
                    par = j % NCB
                    if j + NCB - 1 < 17:
                        comb_loads(j + NCB - 1)
                    for k in range(4):
                        tt("dve", yb[par][0:nt, :], yb[par][0:nt, :], rg[par][k][0:nt, :], ALU.add, [yb[par], rg[par][k]], [yb[par]])
                    if j < 16:
                        dma("sp", "y", y_p[j * 128:(j + 1) * 128, :], yb[par][:, :], reads=[yb[par]], is_output=True)
                    else:
                        dma("sp", "y", y_s[:, :], yb[par][0:NS, :], reads=[yb[par]], is_output=True)
        P.finish("sp")
        with block_cm as block:
            P.emit(block)
    return nc


def _host_constants():
    k = np.arange(128)[:, None]
    q = np.arange(128)[None, :]
    cur = (k <= q).astype(np.float32)
    prev = (k >= q).astype(np.float32)
    maskA = np.concatenate([prev, cur, prev, cur], axis=1)
    maskH0 = np.concatenate([0 * prev, cur, 0 * prev, cur], axis=1)
    mS = np.zeros((128, 2, 9, 3, 4), np.float32)
    for t9 in range(8):
        mS[:, :, t9, :, t9 % 4] = 1.0
    for t in range(4):
        mS[t:, :, 8, :, t] = 1.0
    maskS = mS.reshape(128, 216)
    w4 = np.zeros((4, 4), np.float32)
    for tk in range(4):
        for t in range(4):
            w4[tk, t] = (1.0 if tk <= t else 0.0) + (2.0 if tk == t else 0.0)
    mN = np.kron(np.eye(16, dtype=np.float32), w4)
    maskN = np.tile(mN, (1, 6))
    tril4 = np.zeros((4, 4), np.float32)
    for s in range(4):
        for t in range(4):
            tril4[s, t] = 1.0 if s <= t else 0.0
    mbd = np.kron(np.eye(16, dtype=np.float32), tril4)
    mask_bd = np.repeat(mbd[:, None, :], 6, axis=1)
    ident = np.eye(128, dtype=np.float32)
    onesAB = np.concatenate([np.ones((128, 64)), np.zeros((128, 64)), np.ones((128, 64))], axis=1).astype(np.float32)
    return dict(maskA=maskA, maskH0=maskH0, maskS=maskS, maskN=maskN, mask_bd=mask_bd, ident=ident, onesAB=onesAB)


def _rope_tables(pos):
    half = 8
    inv_freq = np.power(np.float32(500000.0), -np.arange(half, dtype=np.float32) / np.float32(half)).astype(np.float32)
    ang = pos.astype(np.float32)[:, None] * inv_freq[None, :]
    c = np.cos(ang).astype(np.float32)
    s = np.sin(ang).astype(np.float32)
    return np.concatenate([c, c], axis=1), np.concatenate([-s, s], axis=1)


_NC_CACHE = {}


def kernel(x_prompt, x_sample, mem_prompt, cache_win_k, cache_win_v, cache_mem_k, cache_mem_v,
           norm1_g, w_in, gv_a, w_s, b_s, gq_b, gk_b, gq_m, gk_m, mem_norm_g, w_mem_kv, w_out,
           norm2_g, w_router, b_router, w_gate_up, b_gate_up, w_down, b_down):
    f = lambda a: np.ascontiguousarray(np.asarray(a, dtype=np.float32))
    x_prompt, x_sample, mem_prompt = f(x_prompt), f(x_sample), f(mem_prompt)
    cwk, cwv = f(cache_win_k)[0], f(cache_win_v)[0]
    cmk, cmv = f(cache_mem_k)[0], f(cache_mem_v)[0]
    C = _host_constants()
    gT = lambda g: f(np.asarray(g)[0].reshape(8, 128).T)
    w_s0 = np.asarray(w_s, np.float32)[0]
    b_s0 = np.asarray(b_s, np.float32)[0]
    shared = {
        "norm1_gT": gT(norm1_g), "norm2_gT": gT(norm2_g), "memn_gT": gT(mem_norm_g),
        "w_in": f(w_in)[0], "w_mem_kv": f(w_mem_kv)[0], "w_out": f(w_out)[0],
        "gv_a": f(gv_a), "gqk": f(np.concatenate([np.tile(np.asarray(gq_b)[0], 6), np.tile(np.asarray(gk_b)[0], 6)])[None]),
        "gqm": f(np.tile(np.asarray(gq_m)[0], 4)[None]), "gkm": f(np.tile(np.asarray(gk_m)[0], 4)[None]),
        "w_sT": f(w_s0.transpose(2, 0, 1)),
        "bsB": f(np.repeat(b_s0.T[:, :, None], 64, axis=2).reshape(128, 384)),
        "maskA": C["maskA"], "maskS": C["maskS"], "maskN": C["maskN"], "mask_bd": C["mask_bd"],
        "ident": C["ident"], "onesAB": C["onesAB"],
        "w_router": f(w_router)[0], "b_router": f(b_router),
        "w_gate_up": f(w_gate_up)[0], "w_down": f(w_down)[0],
        "bguT": f(np.asarray(b_gate_up, np.float32)[0].reshape(NEXP, 16, 128).transpose(2, 0, 1)),
        "b_down": f(b_down)[0],
        "norm2_row": f(norm2_g),
    }
    pp = np.arange(128, dtype=np.float32)[:, None]
    shared["mconst"] = f(np.concatenate([np.tile(np.arange(54, dtype=np.float32)[None], (128, 1)),
                                         np.tile(np.arange(32, dtype=np.float32)[None], (128, 1)),
                                         np.arange(8, dtype=np.float32)[None] * 128 + pp,
                                         np.arange(17, dtype=np.float32)[None] * 128 + pp], axis=1))
    shared["triu"] = f(np.triu(np.ones((128, 128), np.float32), 1))
    bd = np.zeros((64, 6, 64), np.float32)
    for b in range(16):
        bd[4 * b:4 * b + 4, :, 4 * b:4 * b + 4] = w_s0[:, :4, :4].transpose(2, 0, 1)
    shared["bd_ws"] = bd
    shared["bsS"] = f(np.tile(np.repeat(b_s0[:, :4].T[:, :, None], 64, axis=2).reshape(4, 384), (16, 1)))
    rows = np.zeros((9, 128), np.int64)
    for t in range(4):
        rows[t] = t + 16 * np.arange(128)
        rows[4 + t] = 1536 + t + 4 * np.arange(128)
    rows[8] = 1920 + np.arange(128)

    in_maps = []
    for c in range(NCORES):
        b, g = c // 4, c % 4
        s0 = g * SEG
        m = dict(shared)
        m["xp"] = f(x_prompt[b, s0:s0 + SEG])
        m["xh"] = f(x_prompt[b, s0 - SEG:s0]) if g > 0 else np.zeros((SEG, D), np.float32)
        m["xs"] = f(x_sample[16 * c:16 * c + 16].reshape(NS, D))
        m["mem"] = f(mem_prompt[b])
        kk = cwk[16 * c:16 * c + 16][:, rows]
        m["cwkT"] = f(kk.reshape(16, 9, 128, 3, 128).transpose(0, 4, 3, 1, 2).reshape(16, 128, 3, 1152))
        vv = cwv[16 * c:16 * c + 16][:, rows]
        m["cwv"] = f(vv.transpose(0, 2, 1, 3, 4).reshape(16, 128, 9, 384))
        mk = cmk[16 * c:16 * c + 16]
        m["cmkT"] = f(mk.reshape(16, 256, 2, 128).transpose(3, 0, 2, 1))
        mv = cmv[16 * c:16 * c + 16]
        m["cmv"] = f(mv.reshape(16, 2, 128, 256).transpose(2, 0, 1, 3))
        pos = np.concatenate([s0 + np.arange(SEG), s0 - SEG + np.arange(SEG), 8192 + (np.arange(NS) % 4)])
        cs, sn = _rope_tables(pos)
        m["rope_cs"], m["rope_sn"] = cs, sn
        m["maskH"] = C["maskA"] if g > 0 else C["maskH0"]
        in_maps.append(m)

    if "nc" not in _NC_CACHE:
        _NC_CACHE["nc"] = build_program()
    nc = _NC_CACHE["nc"]
    res = run_bass_kernel_spmd(nc, in_maps, core_ids=list(range(NCORES)))
    R = res.results
    y_prompt = np.stack([np.concatenate([R[4 * b + g]["y_p"] for g in range(4)], axis=0) for b in range(2)])
    y_sample = np.concatenate([R[c]["y_s"].reshape(16, 4, D) for c in range(NCORES)], axis=0)
    wk = np.stack([R[4 * b + 3]["wk_p"] for b in range(2)]).reshape(1, 2, SEG, 6, 64)
    wv = np.stack([R[4 * b + 3]["wv_p"] for b in range(2)]).reshape(1, 2, SEG, 6, 64)
    mk = np.stack([R[4 * b]["mk_p"] for b in range(2)]).reshape(1, 2, 256, 4, 64)
    mv = np.stack([R[4 * b]["mv_p"] for b in range(2)]).reshape(1, 2, 256, 4, 64)
    wks = np.concatenate([R[c]["wk_s"].reshape(16, 4, 6, 64) for c in range(NCORES)], axis=0)[None]
    wvs = np.concatenate([R[c]["wv_s"].reshape(16, 4, 6, 64) for c in range(NCORES)], axis=0)[None]
    gvs = np.concatenate([R[c]["gv_s"].reshape(16, 4, 384) for c in range(NCORES)], axis=0)[None]
    return (y_prompt.astype(np.float32), y_sample.astype(np.float32), wk.astype(np.float32), wv.astype(np.float32),
            mk.astype(np.float32), mv.astype(np.float32), wks.astype(np.float32), wvs.astype(np.float32), gvs.astype(np.float32))
```
